# Optimizing a Trainium2 kernel written in Bass

```python
import math
import jax, jax.numpy as jnp
from jax import lax
import numpy as np

D_MODEL = 1024
BATCH = 32
SEQ = 2048
DEPTH = 4

N_MIXERS = 2
EPS = 1e-6
BLOCK = 128
RET_HEADS = D_MODEL // 256
RET_DK = 256
RET_DV = 2 * RET_DK
RET_QK = RET_HEADS * RET_DK
RET_V = RET_HEADS * RET_DV
RET_IN = 2 * RET_QK + 2 * RET_V
ROPE_BASE = 10000.0
SB_HEADS = D_MODEL // 64
SB_DH = 64
SB_IN = 3 * SB_HEADS * SB_DH
D_FF = 2816
CONV_W = 3

kernel_name = "hybrid_retention_stickbreaking_convffn"


def _rmsnorm(x, g):
    xf = x.astype(jnp.float32)
    y = xf * lax.rsqrt(jnp.mean(xf * xf, axis=-1, keepdims=True) + EPS)
    return (y * g.astype(jnp.float32)).astype(x.dtype)


def _rotary(x, cos, sin):
    x1, x2 = jnp.split(x, 2, axis=-1)
    c = cos[None, :, None, :]
    s = sin[None, :, None, :]
    return jnp.concatenate([x1 * c - x2 * s, x1 * s + x2 * c], axis=-1)


def _retention(h, w_in, w_out):
    B_, S_, _ = h.shape
    nc = S_ // BLOCK
    proj = h @ w_in
    q, k, v, g = jnp.split(proj, [RET_QK, 2 * RET_QK, 2 * RET_QK + RET_V], axis=-1)
    pos = jnp.arange(S_, dtype=jnp.float32)
    inv_freq = ROPE_BASE ** (-jnp.arange(0, RET_DK, 2, dtype=jnp.float32) / RET_DK)
    ang = pos[:, None] * inv_freq[None, :]
    cos, sin = jnp.cos(ang), jnp.sin(ang)
    q = _rotary(q.reshape(B_, S_, RET_HEADS, RET_DK).astype(jnp.float32), cos, sin)
    k = _rotary(k.reshape(B_, S_, RET_HEADS, RET_DK).astype(jnp.float32), cos, sin) * (RET_DK ** -0.5)
    v = v.reshape(B_, S_, RET_HEADS, RET_DV).astype(jnp.float32)
    qc = q.transpose(0, 2, 1, 3).reshape(B_, RET_HEADS, nc, BLOCK, RET_DK)
    kc = k.transpose(0, 2, 1, 3).reshape(B_, RET_HEADS, nc, BLOCK, RET_DK)
    vc = v.transpose(0, 2, 1, 3).reshape(B_, RET_HEADS, nc, BLOCK, RET_DV)
    log_g = jnp.log(1.0 - 2.0 ** (-5.0 - jnp.arange(RET_HEADS, dtype=jnp.float32)))
    cpos = jnp.arange(BLOCK, dtype=jnp.float32)
    diff = cpos[:, None] - cpos[None, :]
    dmat = jnp.where(diff[None] >= 0,
                     jnp.exp(jnp.maximum(diff, 0.0)[None] * log_g[:, None, None]), 0.0)
    scores = jnp.einsum('bhncd,bhnkd->bhnck', qc, kc) * dmat[None, :, None]
    intra = jnp.einsum('bhnck,bhnke->bhnce', scores, vc)
    q_dec = qc * jnp.exp((cpos + 1.0)[None, :] * log_g[:, None])[None, :, None, :, None]
    k_dec = kc * jnp.exp((BLOCK - 1.0 - cpos)[None, :] * log_g[:, None])[None, :, None, :, None]
    chunk_decay = jnp.exp(BLOCK * log_g)[None, :, None, None]

    def step(state, xs):
        qd, kd, vv = xs
        out = jnp.einsum('bhcd,bhde->bhce', qd, state)
        state = chunk_decay * state + jnp.einsum('bhkd,bhke->bhde', kd, vv)
        return state, out

    state0 = jnp.zeros((B_, RET_HEADS, RET_DK, RET_DV), jnp.float32)
    _, cross = lax.scan(step, state0, (jnp.moveaxis(q_dec, 2, 0), jnp.moveaxis(k_dec, 2, 0),
                                       jnp.moveaxis(vc, 2, 0)))
    o = intra + jnp.moveaxis(cross, 0, 2)
    o = o.reshape(B_, RET_HEADS, S_, RET_DV)
    o = o * lax.rsqrt(jnp.mean(o * o, axis=-1, keepdims=True) + EPS)
    o = o.transpose(0, 2, 1, 3).reshape(B_, S_, RET_V)
    y = jax.nn.silu(g.astype(jnp.float32)) * o
    return y.astype(h.dtype) @ w_out


def _stick_breaking(h, w_in, w_out):
    B_, S_, _ = h.shape
    nb = S_ // BLOCK
    scale = 1.0 / math.sqrt(SB_DH)
    proj = h @ w_in
    q, k, v = jnp.split(proj, 3, axis=-1)
    to_heads = lambda a: a.reshape(B_, S_, SB_HEADS, SB_DH).transpose(0, 2, 1, 3).astype(jnp.float32)
    q, k, v = to_heads(q), to_heads(k), to_heads(v)
    outs = []
    for i in range(nb):
        qb = q[:, :, i * BLOCK:(i + 1) * BLOCK]
        t_pos = i * BLOCK + jnp.arange(BLOCK)
        kp = jnp.moveaxis(k[:, :, :(i + 1) * BLOCK].reshape(B_, SB_HEADS, i + 1, BLOCK, SB_DH), 2, 0)
        vp = jnp.moveaxis(v[:, :, :(i + 1) * BLOCK].reshape(B_, SB_HEADS, i + 1, BLOCK, SB_DH), 2, 0)

        def step(carry, xs, qb=qb, t_pos=t_pos):
            suffix, acc = carry
            kb, vb, j = xs
            z = jnp.einsum('bhqd,bhkd->bhqk', qb, kb) * scale
            s_pos = j * BLOCK + jnp.arange(BLOCK)
            mask = s_pos[None, :] < t_pos[:, None]
            log1m = jnp.where(mask, jax.nn.log_sigmoid(-z), 0.0)
            excl = lax.cumsum(log1m, axis=3, reverse=True) - log1m
            log_a = jax.nn.log_sigmoid(z) + excl + suffix[..., None]
            a = jnp.where(mask, jnp.exp(log_a), 0.0)
            acc = acc + jnp.einsum('bhqk,bhkd->bhqd', a, vb)
            suffix = suffix + jnp.sum(log1m, axis=3)
            return (suffix, acc), None

        init = (jnp.zeros((B_, SB_HEADS, BLOCK), jnp.float32),
                jnp.zeros((B_, SB_HEADS, BLOCK, SB_DH), jnp.float32))
        (_, acc), _ = lax.scan(step, init, (kp, vp, jnp.arange(i + 1)), reverse=True)
        outs.append(acc)
    o = jnp.concatenate(outs, axis=2)
    o = o.transpose(0, 2, 1, 3).reshape(B_, S_, SB_HEADS * SB_DH)
    return o.astype(h.dtype) @ w_out


def _conv_ffn(h, w_up, conv_w, conv_b, w_down):
    u = h @ w_up
    u = lax.conv_general_dilated(u, conv_w[:, None, :].astype(u.dtype), window_strides=(1,),
                                 padding=[(CONV_W - 1, 0)],
                                 dimension_numbers=('NWC', 'WIO', 'NWC'),
                                 feature_group_count=2 * D_FF) + conv_b
    gate, val = jnp.split(u, 2, axis=-1)
    return (jax.nn.silu(gate) * val) @ w_down


def setup_inputs(seed: int = 0) -> dict:
    key = jax.random.key(seed)
    ks = jax.random.split(key, 16)
    n_ret = (DEPTH + 1) // 2
    n_sb = DEPTH // 2
    nrm = lambda k, shape, fan_in: jax.random.normal(k, shape, jnp.float32) * fan_in ** -0.5
    gain = lambda k, shape: 1.0 + 0.02 * jax.random.normal(k, shape, jnp.float32)
    return {
        "x": jax.random.normal(ks[0], (BATCH, SEQ, D_MODEL), jnp.float32),
        "ret_norm": gain(ks[1], (n_ret, D_MODEL)),
        "ret_w_in": nrm(ks[2], (n_ret, D_MODEL, RET_IN), D_MODEL),
        "ret_w_out": nrm(ks[3], (n_ret, RET_V, D_MODEL), RET_V),
        "sb_norm": gain(ks[4], (n_sb, D_MODEL)),
        "sb_w_in": nrm(ks[5], (n_sb, D_MODEL, SB_IN), D_MODEL),
        "sb_w_out": nrm(ks[6], (n_sb, SB_HEADS * SB_DH, D_MODEL), SB_HEADS * SB_DH),
        "ffn_norm": gain(ks[7], (DEPTH, D_MODEL)),
        "ffn_w_up": nrm(ks[8], (DEPTH, D_MODEL, 2 * D_FF), D_MODEL),
        "ffn_conv_w": nrm(ks[9], (DEPTH, CONV_W, 2 * D_FF), CONV_W),
        "ffn_conv_b": 0.02 * jax.random.normal(ks[10], (DEPTH, 2 * D_FF), jnp.float32),
        "ffn_w_down": nrm(ks[11], (DEPTH, D_FF, D_MODEL), D_FF),
        "final_norm": gain(ks[12], (D_MODEL,)),
    }


def reference(x, ret_norm, ret_w_in, ret_w_out, sb_norm, sb_w_in, sb_w_out,
              ffn_norm, ffn_w_up, ffn_conv_w, ffn_conv_b, ffn_w_down, final_norm):
    for i in range(DEPTH):
        j = i // N_MIXERS
        if i % N_MIXERS == 0:
            x = x + _retention(_rmsnorm(x, ret_norm[j]), ret_w_in[j], ret_w_out[j])
        else:
            x = x + _stick_breaking(_rmsnorm(x, sb_norm[j]), sb_w_in[j], sb_w_out[j])
        x = x + _conv_ffn(_rmsnorm(x, ffn_norm[i]), ffn_w_up[i], ffn_conv_w[i],
                          ffn_conv_b[i], ffn_w_down[i])
    return _rmsnorm(x, final_norm)
```

```python
import math
import numpy as np
import ml_dtypes
import concourse.bass as bass
import concourse.mybir as mybir
from concourse.bass_utils import run_bass_kernel_spmd

F32 = mybir.dt.float32
BF16 = mybir.dt.bfloat16
AF = mybir.ActivationFunctionType
ALU = mybir.AluOpType

D = 1024
S = 2048
NCH = 8
NTB = 4
DFF = 2816
NFC = 22
EPS = 1e-6
NCORES = 8


class Res:
    __slots__ = ("w", "r")
    registry = []

    def __init__(self):
        self.w = None
        self.r = {}
        Res.registry.append(self)


class Buf:
    __slots__ = ("ap", "res")

    def __init__(self, ap):
        self.ap = ap
        self.res = Res()


class Eng:
    def __init__(self, name, h, sem, raw):
        self.name = name
        self.h = h
        self.sem = sem
        self.cnt = 0
        self.waited = {}
        self.raw = raw


class Tr:
    def __init__(self, nc):
        self.nc = nc
        self.engs = {}
        for name, h, raw in (("pe", nc.tensor, False), ("act", nc.scalar, True),
                             ("dve", nc.vector, True), ("pool", nc.gpsimd, True),
                             ("sp", nc.sync, False)):
            self.engs[name] = Eng(name, h, nc.alloc_semaphore("sem_" + name), raw)
        self.ring = [nc.alloc_semaphore("dring%d" % i) for i in range(16)]
        self.ring_n = 0
        self.ring_val = [0] * 16

    def _collect(self, e, reads, writes, extra=()):
        deps = {}

        def add(tok, same_ok):
            if tok is None:
                return
            sem, val = tok
            if sem is e.sem and not same_ok:
                return
            k = id(sem)
            if e.waited.get(k, 0) >= val:
                return
            if k not in deps or deps[k][1] < val:
                deps[k] = tok

        for r in reads:
            add(r.w, e.raw)
        for w in writes:
            add(w.w, False)
            for tok in w.r.values():
                add(tok, False)
        for tok in extra:
            add(tok, False)
        return list(deps.values())

    def op(self, en, fn, reads=(), writes=()):
        e = self.engs[en]
        toks = self._collect(e, reads, writes)
        for tok in toks[:-1]:
            e.h.wait_ge(tok[0], tok[1])
        ins = fn()
        if toks:
            ins._wait_ge(toks[-1][0], toks[-1][1])
        for tok in toks:
            e.waited[id(tok[0])] = tok[1]
        e.cnt += 1
        ins.then_inc(e.sem, 1)
        me = (e.sem, e.cnt)
        for r in reads:
            r.r[id(e.sem)] = me
        for w in writes:
            w.w = me
            w.r = {}
        return ins

    def dma(self, out, in_, reads=(), writes=(), en="sp"):
        e = self.engs[en]
        i = self.ring_n % 16
        self.ring_n += 1
        sem = self.ring[i]
        prev = (sem, self.ring_val[i]) if self.ring_val[i] > 0 else None
        self.ring_val[i] += 16
        toks = self._collect(e, reads, writes, extra=(prev,) if prev else ())
        for tok in toks:
            e.h.wait_ge(tok[0], tok[1])
            e.waited[id(tok[0])] = tok[1]
        e.h.dma_start(out=out, in_=in_).then_inc(sem, 16)
        me = (sem, self.ring_val[i])
        for r in reads:
            r.r[id(sem)] = me
        for w in writes:
            w.w = me
            w.r = {}

    def all_tokens(self):
        toks = [(e.sem, e.cnt) for e in self.engs.values() if e.cnt > 0]
        toks += [(self.ring[i], self.ring_val[i]) for i in range(16) if self.ring_val[i] > 0]
        return toks

    def barrier(self, names=("pe", "act", "dve", "pool", "sp")):
        toks = self.all_tokens()
        for n in names:
            e = self.engs[n]
            for sem, val in toks:
                if sem is e.sem:
                    continue
                if e.waited.get(id(sem), 0) >= val:
                    continue
                e.h.wait_ge(sem, val)
                e.waited[id(sem)] = val


class Carver:
    def __init__(self, arena, base, limit):
        self.a = arena
        self.off = base
        self.limit = limit

    def take(self, shape, dt):
        n = 1
        for s_ in shape:
            n *= s_
        nbytes = n * (4 if dt is F32 else 2)
        start = self.off
        self.off += (nbytes + 63) // 64 * 64
        assert self.off <= self.limit, ("SBUF carve overflow", self.off, self.limit)
        ap = self.a[:, start // 2: start // 2 + nbytes // 2]
        if dt is F32:
            ap = ap.bitcast(F32)
        if len(shape) == 2:
            ap = ap.rearrange("p (a b) -> p a b", a=shape[0])
        elif len(shape) == 3:
            ap = ap.rearrange("p (a b c) -> p a b c", a=shape[0], b=shape[1])
        return ap

    def buf(self, shape, dt):
        return Buf(self.take(shape, dt))


ARENA_BYTES = 212000
R_BASE = 65536 + 32768 + 8192


def build_program(nseq, sublayers, do_prep=True):
    nc = bass.Bass("TRN2", target_bir_lowering=False)
    Res.registry = []
    tr = Tr(nc)
    op = tr.op

    def reset_sync():
        tr.barrier()
        nc.all_engine_barrier()
        for e in tr.engs.values():
            e.h.sem_clear(e.sem)
        for sem in tr.ring:
            nc.sync.sem_clear(sem)
        nc.all_engine_barrier()
        for e in tr.engs.values():
            e.cnt = 0
            e.waited = {}
        tr.ring_n = 0
        tr.ring_val = [0] * 16
        for r in Res.registry:
            r.w = None
            r.r = {}

    def din(name, shape, dt=F32):
        return nc.dram_tensor(name, list(shape), dt, kind="ExternalInput").ap()

    x_d = din("x", [nseq, S, D])
    out_d = nc.dram_tensor("out", [nseq, S, D], F32, kind="ExternalOutput").ap()
    used = set(sublayers)
    ret_w_in = {j: din("ret_w_in_%d" % j, [D, 6144]) for j in range(2) if ("ret", j) in used}
    ret_w_out = {j: din("ret_w_out_%d" % j, [2048, D]) for j in range(2) if ("ret", j) in used}
    sb_w_in = {j: din("sb_w_in_%d" % j, [D, 3072]) for j in range(2) if ("sb", j) in used}
    sb_w_out = {j: din("sb_w_out_%d" % j, [D, D]) for j in range(2) if ("sb", j) in used}
    ffn_w_up = {l: din("ffn_w_up_%d" % l, [D, 2 * DFF]) for l in range(4) if ("ffn", l) in used}
    ffn_w_down = {l: din("ffn_w_down_%d" % l, [DFF, D]) for l in range(4) if ("ffn", l) in used}
    c_gains = din("c_gains", [128, 9 * 8])
    c_cw = din("c_cw", [128, 4 * 44 * 3])
    c_cb = din("c_cb", [128, 4 * 44])
    c_identb = din("c_identb", [128, 128], BF16)
    c_identf = din("c_identf", [128, 128])
    c_tri = din("c_tri", [128, 128], BF16)
    c_mask = din("c_mask", [128, 128], BF16)
    c_dm = din("c_dm", [128, 4 * 128])
    c_kdecs = din("c_kdecs", [128, 4])
    c_epsv = din("c_epsv", [128, 4])
    c_cos = din("c_cos", [128, S])
    c_sin = din("c_sin", [128, S])

    def dscr(name, shape):
        return nc.dram_tensor(name, list(shape), BF16).ap()

    s_rq = [dscr("s_rq%d" % j, [4, 128, 2048]) for j in range(2)]
    s_rk = [dscr("s_rk%d" % j, [4, 128, 2048]) for j in range(2)]
    s_rv = [dscr("s_rv%d" % j, [4, 128, 4096]) for j in range(2)]
    s_rg = [dscr("s_rg%d" % j, [4, 128, 4096]) for j in range(2)]
    s_ro = [dscr("s_ro%d" % j, [4, 128, 4096]) for j in range(2)]
    s_sw = [dscr("s_sw%d" % j, [8, 128, 3072]) for j in range(2)]
    s_so = [dscr("s_so%d" % j, [8, 64, 2048]) for j in range(2)]
    s_fu = [dscr("s_fu%d" % l, [22, 128, 2048]) for l in range(4)]
    s_fd = [dscr("s_fd%d" % l, [128, 22 * 1024]) for l in range(4)]

    arena = nc.alloc_sbuf_tensor("arena", [128, ARENA_BYTES // 2], BF16)
    pc = Carver(arena, 0, R_BASE)
    xT = pc.take([NCH, S], F32)
    hT = pc.take([NCH, S], BF16)
    xr = [[Res() for _ in range(NTB)] for _ in range(NCH)]
    hr = [[Res() for _ in range(NTB)] for _ in range(NCH)]
    identb = pc.buf([128], BF16)
    identf = pc.buf([128], F32)
    onesb = pc.buf([128], BF16)
    tri = pc.buf([128], BF16)
    maskb = pc.buf([128], BF16)
    dm = pc.buf([4, 128], F32)
    kdecs = pc.buf([4], F32)
    epsv = pc.buf([4], F32)
    gains = pc.buf([72], F32)
    g32 = pc.buf([72], F32)
    cw = pc.buf([4 * 44 * 3], F32)
    cb = pc.buf([4 * 44], F32)
    epsc = pc.buf([8], F32)

    PS = nc.alloc_psum_tensor("ps", [128, 8, 512], F32)
    psr = [Res() for _ in range(8)]
    pstate = {"i": 0, "allowed": list(range(8))}

    def psalloc():
        a = pstate["allowed"]
        pstate["i"] = (pstate["i"] + 1) % len(a)
        return a[pstate["i"]]

    def psb(b):
        return PS[:, b, :]

    def psb16(b):
        return PS[:, b, :].bitcast(BF16)

    for b_, src in ((identb, c_identb), (identf, c_identf), (tri, c_tri), (maskb, c_mask),
                    (kdecs, c_kdecs), (epsv, c_epsv), (gains, c_gains), (cw, c_cw), (cb, c_cb)):
        tr.dma(out=b_.ap, in_=src, writes=[b_.res])
    tr.dma(out=dm.ap, in_=c_dm.rearrange("p (h c) -> p h c", h=4), writes=[dm.res])
    op("dve", lambda: nc.vector.memset(onesb.ap, 1.0), writes=[onesb.res])
    op("dve", lambda: nc.vector.memset(epsc.ap, 1024.0 * EPS), writes=[epsc.res])
    op("dve", lambda: nc.vector.tensor_scalar(out=g32.ap, in0=gains.ap, scalar1=32.0, scalar2=None,
                                               op0=ALU.mult), reads=[gains.res], writes=[g32.res])

    cast_rr = {"i": 0}

    def cast(out, in_, reads, writes):
        k = cast_rr["i"] % 3
        cast_rr["i"] += 1
        if k == 0:
            op("act", lambda: nc.scalar.copy(out=out, in_=in_), reads=reads, writes=writes)
        elif k == 1:
            op("dve", lambda: nc.vector.tensor_copy(out, in_), reads=reads, writes=writes)
        else:
            op("pool", lambda: nc.gpsimd.tensor_copy(out, in_), reads=reads, writes=writes)

    used = set(sublayers)
    if do_prep:
        rc = Carver(arena, R_BASE, ARENA_BYTES)
        stg = [rc.buf([4096], F32) for _ in range(2)]
        bst = [rc.buf([4096], BF16) for _ in range(2)]
        pi = {"i": 0}

        def piece(loads, n_el, store_fn, cast_views=None, parts=128):
            k = pi["i"] % 2
            pi["i"] += 1
            sb_, bb_ = stg[k], bst[k]
            for (dst_fn, src) in loads:
                tr.dma(out=dst_fn(sb_.ap), in_=src, writes=[sb_.res])
            if cast_views is None:
                cast(bb_.ap[:parts, :n_el], sb_.ap[:parts, :n_el], [sb_.res], [bb_.res])
            else:
                o_, i_ = cast_views(bb_.ap, sb_.ap)
                cast(o_, i_, [sb_.res], [bb_.res])
            store_fn(bb_)

        for j in range(2):
            if ("ret", j) in used:
                w = ret_w_in[j].rearrange("(c p) n -> p c n", p=128)
                for h in range(4):
                    for (scr, c0, ns) in ((s_rq[j], h * 256, 256), (s_rk[j], 1024 + h * 256, 256),
                                          (s_rv[j], 2048 + h * 512, 512), (s_rg[j], 4096 + h * 512, 512)):
                        ne = 8 * ns
                        piece([(lambda a, ns=ns, ne=ne: a[:, :ne].rearrange("p (c n) -> p c n", c=8),
                                w[:, :, c0:c0 + ns])], ne,
                              lambda bb, scr=scr, h=h, ne=ne: tr.dma(out=scr[h], in_=bb.ap[:, :ne], reads=[bb.res]))
                    wo = ret_w_out[j][h * 512:(h + 1) * 512, :].rearrange("(e p) n -> p e n", p=128)
                    piece([(lambda a: a.rearrange("p (e n) -> p e n", e=4), wo)], 4096,
                          lambda bb, j=j, h=h: tr.dma(out=s_ro[j][h], in_=bb.ap, reads=[bb.res]))
            if ("sb", j) in used:
                w = sb_w_in[j].rearrange("(c p) n -> p c n", p=128)
                for p_ in range(8):
                    loads = []
                    for t3 in range(3):
                        loads.append((lambda a, t3=t3: a[:, :3072].rearrange("p (c t n) -> p c t n", c=8, t=3)[:, :, t3, :],
                                      w[:, :, t3 * 1024 + p_ * 128: t3 * 1024 + (p_ + 1) * 128]))
                    piece(loads, 3072,
                          lambda bb, j=j, p_=p_: tr.dma(out=s_sw[j][p_], in_=bb.ap[:, :3072], reads=[bb.res]))
                    wo = sb_w_out[j][p_ * 128:(p_ + 1) * 128, :].rearrange("(hh d) n -> d hh n", d=64)
                    piece([(lambda a: a[:64, :2048].rearrange("p (hh n) -> p hh n", hh=2), wo)], 2048,
                          lambda bb, j=j, p_=p_: tr.dma(out=s_so[j][p_], in_=bb.ap[:64, :2048], reads=[bb.res]),
                          parts=64)
        for l in range(4):
            if ("ffn", l) in used:
                w = ffn_w_up[l].rearrange("(c p) n -> p c n", p=128)
                for g in range(11):
                    loads = []
                    for gv in range(2):
                        loads.append((lambda a, gv=gv: a.rearrange("p (c gv n) -> p c gv n", c=8, gv=2)[:, :, gv, :],
                                      w[:, :, gv * DFF + g * 256: gv * DFF + (g + 1) * 256]))

                    def cviews(bb, sb_):
                        o_ = bb.rearrange("p (pr cg n) -> p pr cg n", pr=2, n=128)
                        i_ = sb_.rearrange("p (cg pr n) -> p pr cg n", pr=2, n=128)
                        return o_, i_

                    def store(bb, l=l, g=g):
                        tr.dma(out=s_fu[l][2 * g:2 * g + 2].rearrange("s p n -> p s n"),
                               in_=bb.ap.rearrange("p (s n) -> p s n", s=2), reads=[bb.res])

                    piece(loads, 4096, store, cast_views=cviews)
                wd = ffn_w_down[l].rearrange("(k p) n -> p k n", p=128)
                for q in range(6):
                    k0 = q * 4
                    nk = min(4, 22 - k0)
                    ne = nk * 1024
                    piece([(lambda a, nk=nk, ne=ne: a[:, :ne].rearrange("p (k n) -> p k n", k=nk), wd[:, k0:k0 + nk, :])], ne,
                          lambda bb, l=l, k0=k0, ne=ne: tr.dma(out=s_fd[l][:, k0 * 1024:k0 * 1024 + ne], in_=bb.ap[:, :ne],
                                                               reads=[bb.res]))
        tr.barrier()

    def tbs(tb):
        return slice(tb * 512, (tb + 1) * 512)

    def norm_block(tb, gi, sqs, rstd, dst_fn, dst_res):
        bank = psalloc()
        for c in range(NCH):
            sqb = sqs[c % len(sqs)]
            op("act", lambda: nc.scalar.activation(out=sqb.ap, in_=xT[:, c, tbs(tb)], func=AF.Square),
               reads=[xr[c][tb]], writes=[sqb.res])
            op("pe", lambda: nc.tensor.matmul(psb(bank), lhsT=onesb.ap, rhs=sqb.ap, start=(c == 0), stop=(c == NCH - 1)),
               reads=[sqb.res, onesb.res], writes=[psr[bank]])
        op("act", lambda: nc.scalar.activation(out=rstd.ap, in_=psb(bank), func=AF.Ln, bias=epsc.ap[:, 0:1]),
           reads=[epsc.res], writes=[psr[bank], rstd.res])
        op("act", lambda: nc.scalar.activation(out=rstd.ap, in_=rstd.ap, func=AF.Exp, scale=-0.5),
           reads=[rstd.res], writes=[rstd.res])
        for c in range(NCH):
            en = "dve"
            eh = nc.vector
            op(en, lambda: eh.scalar_tensor_tensor(out=dst_fn(c), in0=xT[:, c, tbs(tb)],
                                                   scalar=g32.ap[:, gi * 8 + c: gi * 8 + c + 1], in1=rstd.ap,
                                                   op0=ALU.mult, op1=ALU.mult),
               reads=[xr[c][tb], rstd.res, g32.res], writes=[dst_res(c)])

    def resid_add(n, tb, bank):
        op("dve", lambda: nc.vector.tensor_tensor(out=xT[:, n, tbs(tb)], in0=psb(bank), in1=xT[:, n, tbs(tb)], op=ALU.add),
           reads=[xr[n][tb]], writes=[psr[bank], xr[n][tb]])

    def load_x(b):
        tr.barrier()
        rc = Carver(arena, R_BASE, ARENA_BYTES)
        st = [rc.buf([1024], F32) for _ in range(2)]
        pstate["allowed"] = list(range(8))
        for t in range(16):
            sb_ = st[t % 2]
            tr.dma(out=sb_.ap, in_=x_d[b, t * 128:(t + 1) * 128, :], writes=[sb_.res])
            for half in range(2):
                bank = psalloc()
                for cc in range(4):
                    c = half * 4 + cc
                    op("pe", lambda: nc.tensor.transpose(PS[:, bank, cc * 128:(cc + 1) * 128], sb_.ap[:, c * 128:(c + 1) * 128], identf.ap),
                       reads=[sb_.res, identf.res], writes=[psr[bank]])
                tb = t // 4
                dst = xT[:, half * 4:(half + 1) * 4, t * 128:(t + 1) * 128]
                src = PS[:, bank, :].rearrange("p (c n) -> p c n", c=4)
                wr = [psr[bank]] + [xr[half * 4 + cc][tb] for cc in range(4)]
                if half == 0:
                    op("act", lambda: nc.scalar.copy(out=dst, in_=src), writes=wr)
                else:
                    op("dve", lambda: nc.vector.tensor_copy(dst, src), writes=wr)

    def store_out(b):
        tr.barrier()
        rc = Carver(arena, R_BASE, ARENA_BYTES)
        sqs = [rc.buf([512], BF16) for _ in range(3)]
        rstd = rc.buf([512], F32)
        yn = rc.buf([NCH, 512], F32)
        ynr = [Res() for _ in range(NCH)]
        ost = [rc.buf([1024], F32) for _ in range(2)]
        pstate["allowed"] = list(range(8))
        for tb in range(NTB):
            norm_block(tb, 8, sqs, rstd, lambda c: yn.ap[:, c, :], lambda c: ynr[c])
            for tt in range(4):
                t = tb * 4 + tt
                ob = ost[t % 2]
                for half in range(2):
                    bank = psalloc()
                    for cc in range(4):
                        c = half * 4 + cc
                        op("pe", lambda: nc.tensor.transpose(PS[:, bank, cc * 128:(cc + 1) * 128], yn.ap[:, c, tt * 128:(tt + 1) * 128], identf.ap),
                           reads=[ynr[c], identf.res], writes=[psr[bank]])
                    if half == 0:
                        op("act", lambda: nc.scalar.copy(out=ob.ap[:, 0:512], in_=psb(bank)), writes=[psr[bank], ob.res])
                    else:
                        op("dve", lambda: nc.vector.tensor_copy(ob.ap[:, 512:1024], psb(bank)), writes=[psr[bank], ob.res])
                tr.dma(out=out_d[b, t * 128:(t + 1) * 128, :], in_=ob.ap, reads=[ob.res])

    def ffn(l):
        tr.barrier()
        rc = Carver(arena, R_BASE, ARENA_BYTES)
        wd = rc.buf([NFC, 1024], BF16)
        wups = [rc.buf([8, 2, 128], BF16) for _ in range(3)]
        mT = rc.buf([NFC, 512], BF16)
        sqs = [rc.buf([512], BF16) for _ in range(3)]
        rstd = rc.buf([512], F32)
        U = [[rc.buf([514], F32) for _ in range(2)] for _ in range(2)]
        ACC = [[rc.buf([512], F32) for _ in range(2)] for _ in range(2)]
        H = rc.buf([44, 2], F32)
        pstate["allowed"] = list(range(8))
        op("dve", lambda: nc.vector.memset(H.ap, 0.0), writes=[H.res])
        seq = [(tb, i) for tb in range(NTB) for i in range(NFC)]
        issued = {"n": 0}

        def ensure(upto):
            while issued["n"] <= min(upto, len(seq) - 1):
                n = issued["n"]
                tb_, i_ = seq[n]
                wb = wups[n % 3]
                tr.dma(out=wb.ap, in_=s_fu[l][i_].rearrange("p (c g n) -> p c g n", c=8, g=2), writes=[wb.res])
                if n == 1:
                    tr.dma(out=wd.ap, in_=s_fd[l].rearrange("p (k n) -> p k n", k=NFC), writes=[wd.res])
                issued["n"] += 1

        cwl = lambda i, gv, k: cw.ap[:, ((l * 44 + gv * 22 + i) * 3 + k):((l * 44 + gv * 22 + i) * 3 + k + 1)]
        cbl = lambda i, gv: cb.ap[:, (l * 44 + gv * 22 + i):(l * 44 + gv * 22 + i + 1)]
        n = 0
        for tb in range(NTB):
            norm_block(tb, 4 + l, sqs, rstd, lambda c: hT[:, c, tbs(tb)], lambda c: hr[c][tb])
            for i in range(NFC):
                ensure(n + 2)
                wb = wups[n % 3]
                par = n % 2
                n += 1
                banks = []
                for gv in range(2):
                    bank = psalloc()
                    banks.append(bank)
                    for c in range(NCH):
                        op("pe", lambda: nc.tensor.matmul(psb(bank), lhsT=wb.ap[:, c, gv, :], rhs=hT[:, c, tbs(tb)],
                                                          start=(c == 0), stop=(c == NCH - 1)),
                           reads=[wb.res, hr[c][tb]], writes=[psr[bank]])
                for gv in range(2):
                    bank = banks[gv]
                    u = U[gv][par]
                    acc = ACC[gv][par]
                    hcol = H.ap[:, gv * 22 + i, :]
                    op("act", lambda: nc.scalar.copy(out=u.ap[:, 0:2], in_=hcol), reads=[H.res], writes=[u.res])
                    op("act", lambda: nc.scalar.copy(out=u.ap[:, 2:514], in_=psb(bank)), writes=[psr[bank], u.res])
                    op("act", lambda: nc.scalar.copy(out=hcol, in_=u.ap[:, 512:514]), reads=[u.res], writes=[H.res])
                    e1 = "pool"
                    e2 = "dve"
                    h1 = nc.gpsimd
                    h2 = nc.vector
                    op(e1, lambda: h1.tensor_scalar(out=acc.ap, in0=u.ap[:, 2:514], scalar1=cwl(i, gv, 2), scalar2=cbl(i, gv),
                                                    op0=ALU.mult, op1=ALU.add),
                       reads=[u.res, cw.res, cb.res], writes=[acc.res])
                    op(e2, lambda: h2.scalar_tensor_tensor(out=acc.ap, in0=u.ap[:, 1:513], scalar=cwl(i, gv, 1), in1=acc.ap,
                                                           op0=ALU.mult, op1=ALU.add),
                       reads=[u.res, acc.res, cw.res], writes=[acc.res])
                    op(e2, lambda: h2.scalar_tensor_tensor(out=acc.ap, in0=u.ap[:, 0:512], scalar=cwl(i, gv, 0), in1=acc.ap,
                                                           op0=ALU.mult, op1=ALU.add),
                       reads=[u.res, acc.res, cw.res], writes=[acc.res])
                ag, av = ACC[0][par], ACC[1][par]
                op("act", lambda: nc.scalar.activation(out=ag.ap, in_=ag.ap, func=AF.Silu), reads=[ag.res], writes=[ag.res])
                op("dve", lambda: nc.vector.tensor_tensor(out=mT.ap[:, i, :], in0=ag.ap, in1=av.ap, op=ALU.mult),
                   reads=[ag.res, av.res], writes=[mT.res])
            for nn in range(NCH):
                bank = psalloc()
                for k in range(NFC):
                    op("pe", lambda: nc.tensor.matmul(psb(bank), lhsT=wd.ap[:, k, nn * 128:(nn + 1) * 128], rhs=mT.ap[:, k, :],
                                                      start=(k == 0), stop=(k == NFC - 1)),
                       reads=[wd.res, mT.res], writes=[psr[bank]])
                resid_add(nn, tb, bank)

    def retention(j):
        tr.barrier()
        rc = Carver(arena, R_BASE, ARENA_BYTES)
        Wq = [rc.buf([8, 256], BF16) for _ in range(2)]
        Wk = [rc.buf([8, 256], BF16) for _ in range(2)]
        Wv = rc.buf([8, 512], BF16)
        Wg = rc.buf([8, 512], BF16)
        Wo = rc.buf([4, 1024], BF16)
        qT = rc.buf([2, S], BF16)
        kT = rc.buf([2, S], BF16)
        yT = [rc.buf([4, 512], BF16) for _ in range(2)]
        cs = [[rc.buf([512], F32) for _ in range(2)] for _ in range(2)]
        tmp = [rc.buf([512], F32) for _ in range(4)]
        vc = [rc.buf([512], BF16) for _ in range(2)]
        sgc = [rc.buf([512], BF16) for _ in range(2)]
        sT = [rc.buf([128], BF16) for _ in range(2)]
        kdc = [rc.buf([256], BF16) for _ in range(2)]
        S32 = rc.buf([2, 512], F32)
        Sbf = [rc.buf([2, 512], BF16) for _ in range(2)]
        yc = [rc.buf([512], BF16) for _ in range(2)]
        junk = rc.buf([512], BF16)
        st = rc.buf([8], F32)
        sqs = [rc.buf([512], BF16) for _ in range(3)]
        rstd = rc.buf([512], F32)
        pstate["allowed"] = list(range(8))
        gam = [1.0 - 2.0 ** (-5.0 - h) for h in range(4)]

        def load_qk(h):
            tr.dma(out=Wq[h % 2].ap, in_=s_rq[j][h].rearrange("p (c n) -> p c n", c=8), writes=[Wq[h % 2].res])
            tr.dma(out=Wk[h % 2].ap, in_=s_rk[j][h].rearrange("p (c n) -> p c n", c=8), writes=[Wk[h % 2].res])

        load_qk(0)
        for tb in range(NTB):
            norm_block(tb, j, sqs, rstd, lambda c: hT[:, c, tbs(tb)], lambda c: hr[c][tb])
        csn = 0
        for h in range(4):
            tr.dma(out=Wv.ap, in_=s_rv[j][h].rearrange("p (c n) -> p c n", c=8), writes=[Wv.res])
            tr.dma(out=Wg.ap, in_=s_rg[j][h].rearrange("p (c n) -> p c n", c=8), writes=[Wg.res])
            tr.dma(out=Wo.ap, in_=s_ro[j][h].rearrange("p (e n) -> p e n", e=4), writes=[Wo.res])
            if h + 1 < 4:
                load_qk(h + 1)
            for tb in range(NTB):
                cb_, sb_ = cs[csn % 2]
                csn += 1
                tr.dma(out=cb_.ap, in_=c_cos[:, tbs(tb)], writes=[cb_.res])
                tr.dma(out=sb_.ap, in_=c_sin[:, tbs(tb)], writes=[sb_.res])
                for (W, dst) in ((Wq[h % 2], qT), (Wk[h % 2], kT)):
                    b1, b2 = psalloc(), psalloc()
                    for (bank, half) in ((b1, 0), (b2, 1)):
                        for c in range(NCH):
                            op("pe", lambda: nc.tensor.matmul(psb(bank), lhsT=W.ap[:, c, half * 128:(half + 1) * 128],
                                                              rhs=hT[:, c, tbs(tb)], start=(c == 0), stop=(c == NCH - 1)),
                               reads=[W.res, hr[c][tb]], writes=[psr[bank]])
                    t1, t2, t3, t4 = tmp
                    op("dve", lambda: nc.vector.tensor_tensor(out=t1.ap, in0=psb(b1), in1=cb_.ap, op=ALU.mult),
                       reads=[cb_.res], writes=[psr[b1], t1.res])
                    op("dve", lambda: nc.vector.tensor_tensor(out=t3.ap, in0=psb(b1), in1=sb_.ap, op=ALU.mult),
                       reads=[sb_.res], writes=[psr[b1], t3.res])
                    op("dve", lambda: nc.vector.tensor_tensor(out=t2.ap, in0=psb(b2), in1=sb_.ap, op=ALU.mult),
                       reads=[sb_.res], writes=[psr[b2], t2.res])
                    op("dve", lambda: nc.vector.tensor_tensor(out=t4.ap, in0=psb(b2), in1=cb_.ap, op=ALU.mult),
                       reads=[cb_.res], writes=[psr[b2], t4.res])
                    op("pool", lambda: nc.gpsimd.tensor_tensor(out=dst.ap[:, 0, tbs(tb)], in0=t1.ap, in1=t2.ap, op=ALU.subtract),
                       reads=[t1.res, t2.res], writes=[dst.res])
                    op("pool", lambda: nc.gpsimd.tensor_tensor(out=dst.ap[:, 1, tbs(tb)], in0=t3.ap, in1=t4.ap, op=ALU.add),
                       reads=[t3.res, t4.res], writes=[dst.res])
            for c in range(16):
                tb = c // 4
                ts_ = slice(c * 128, (c + 1) * 128)
                v_, sg_, sT_, kd_, y_ = vc[c % 2], sgc[c % 2], sT[c % 2], kdc[c % 2], yc[c % 2]
                bv, bg = psalloc(), psalloc()
                for kc in range(NCH):
                    op("pe", lambda: nc.tensor.matmul(psb(bv), lhsT=hT[:, kc, ts_], rhs=Wv.ap[:, kc, :], start=(kc == 0), stop=(kc == 7)),
                       reads=[hr[kc][tb], Wv.res], writes=[psr[bv]])
                for kc in range(NCH):
                    op("pe", lambda: nc.tensor.matmul(psb(bg), lhsT=hT[:, kc, ts_], rhs=Wg.ap[:, kc, :], start=(kc == 0), stop=(kc == 7)),
                       reads=[hr[kc][tb], Wg.res], writes=[psr[bg]])
                op("act", lambda: nc.scalar.copy(out=v_.ap, in_=psb(bv)), writes=[psr[bv], v_.res])
                op("act", lambda: nc.scalar.activation(out=sg_.ap, in_=psb(bg), func=AF.Silu), writes=[psr[bg], sg_.res])
                bs = psalloc()
                for dc in range(2):
                    op("pe", lambda: nc.tensor.matmul(PS[:, bs, 0:128], lhsT=kT.ap[:, dc, ts_], rhs=qT.ap[:, dc, ts_],
                                                      start=(dc == 0), stop=(dc == 1)),
                       reads=[kT.res, qT.res], writes=[psr[bs]])
                op("dve", lambda: nc.vector.tensor_tensor(out=sT_.ap, in0=PS[:, bs, 0:128], in1=dm.ap[:, h, :], op=ALU.mult),
                   reads=[dm.res], writes=[psr[bs], sT_.res])
                bo = psalloc()
                sb_prev = Sbf[(c + 1) % 2]
                op("pe", lambda: nc.tensor.matmul(psb(bo), lhsT=sT_.ap, rhs=v_.ap, start=True, stop=(c == 0)),
                   reads=[sT_.res, v_.res], writes=[psr[bo]])
                if c > 0:
                    for dc in range(2):
                        op("pe", lambda: nc.tensor.matmul(psb(bo), lhsT=qT.ap[:, dc, ts_], rhs=sb_prev.ap[:, dc, :],
                                                          start=False, stop=(dc == 1)),
                           reads=[qT.res, sb_prev.res], writes=[psr[bo]])
                if c < 15:
                    bt = psalloc()
                    for dc in range(2):
                        op("pe", lambda: nc.tensor.transpose(psb16(bt)[:, dc * 128:(dc + 1) * 128], kT.ap[:, dc, ts_], identb.ap),
                           reads=[kT.res, identb.res], writes=[psr[bt]])
                    op("act", lambda: nc.scalar.activation(out=kd_.ap, in_=psb16(bt)[:, 0:256], func=AF.Copy,
                                                           scale=kdecs.ap[:, h:h + 1]),
                       reads=[kdecs.res], writes=[psr[bt], kd_.res])
                    sb_new = Sbf[c % 2]
                    for dc in range(2):
                        bu = psalloc()
                        op("pe", lambda: nc.tensor.matmul(psb(bu), lhsT=kd_.ap[:, dc * 128:(dc + 1) * 128], rhs=v_.ap, start=True, stop=True),
                           reads=[kd_.res, v_.res], writes=[psr[bu]])
                        if c == 0:
                            op("dve", lambda: nc.vector.tensor_copy(S32.ap[:, dc, :], psb(bu)), writes=[psr[bu], S32.res])
                        else:
                            op("dve", lambda: nc.vector.scalar_tensor_tensor(out=S32.ap[:, dc, :], in0=S32.ap[:, dc, :],
                                                                             scalar=float(gam[h] ** 128), in1=psb(bu),
                                                                             op0=ALU.mult, op1=ALU.add),
                               reads=[S32.res], writes=[psr[bu], S32.res])
                    op("act", lambda: nc.scalar.copy(out=sb_new.ap, in_=S32.ap), reads=[S32.res], writes=[sb_new.res])
                op("act", lambda: nc.scalar.activation(out=junk.ap, in_=psb(bo), func=AF.Square, accum_out=st.ap[:, 0:1]),
                   writes=[psr[bo], junk.res, st.res])
                op("act", lambda: nc.scalar.activation(out=st.ap[:, 1:2], in_=st.ap[:, 0:1], func=AF.Ln, scale=1.0 / 512.0,
                                                       bias=epsv.ap[:, h:h + 1]),
                   reads=[st.res, epsv.res], writes=[st.res])
                op("act", lambda: nc.scalar.activation(out=st.ap[:, 2:3], in_=st.ap[:, 1:2], func=AF.Exp, scale=-0.5),
                   reads=[st.res], writes=[st.res])
                op("dve", lambda: nc.vector.scalar_tensor_tensor(out=y_.ap, in0=psb(bo), scalar=st.ap[:, 2:3], in1=sg_.ap,
                                                                 op0=ALU.mult, op1=ALU.mult),
                   reads=[st.res, sg_.res], writes=[psr[bo], y_.res])
                by = psalloc()
                for e in range(4):
                    op("pe", lambda: nc.tensor.transpose(psb16(by)[:, e * 128:(e + 1) * 128], y_.ap[:, e * 128:(e + 1) * 128], identb.ap),
                       reads=[y_.res, identb.res], writes=[psr[by]])
                yt = yT[tb % 2]
                op("act", lambda: nc.scalar.copy(out=yt.ap[:, :, (c % 4) * 128:(c % 4 + 1) * 128],
                                                 in_=psb16(by)[:, 0:512].rearrange("p (e n) -> p e n", e=4)),
                   writes=[psr[by], yt.res])
                if c % 4 == 3:
                    for nn in range(NCH):
                        bank = psalloc()
                        for e in range(4):
                            op("pe", lambda: nc.tensor.matmul(psb(bank), lhsT=Wo.ap[:, e, nn * 128:(nn + 1) * 128], rhs=yt.ap[:, e, :],
                                                              start=(e == 0), stop=(e == 3)),
                               reads=[Wo.res, yt.res], writes=[psr[bank]])
                        resid_add(nn, tb, bank)

    def stick(j):
        tr.barrier()
        rc = Carver(arena, R_BASE, ARENA_BYTES)
        Wp = [rc.buf([8, 3, 128], BF16) for _ in range(2)]
        Wo = [rc.buf([2, 1024], BF16) for _ in range(2)]
        qsT = rc.buf([S], BF16)
        kT = rc.buf([S], BF16)
        nkT = rc.buf([S], BF16)
        vp = rc.buf([16, 128], BF16)
        oT = rc.buf([2, S], BF16)
        ebuf = [rc.buf([512], F32) for _ in range(2)]
        spb = [rc.buf([512], BF16) for _ in range(4)]
        Rb = [rc.buf([512], BF16) for _ in range(2)]
        aTb = [rc.buf([512], BF16) for _ in range(4)]
        sqs = [rc.buf([512], BF16) for _ in range(3)]
        rstd = rc.buf([512], F32)
        pstate["allowed"] = list(range(8))

        def load_w(p_):
            tr.dma(out=Wp[p_ % 2].ap, in_=s_sw[j][p_].rearrange("p (c t n) -> p c t n", c=8, t=3), writes=[Wp[p_ % 2].res])
            tr.dma(out=Wo[p_ % 2].ap[:64], in_=s_so[j][p_].rearrange("p (h n) -> p h n", h=2), writes=[Wo[p_ % 2].res])

        load_w(0)
        for tb in range(NTB):
            norm_block(tb, 2 + j, sqs, rstd, lambda c: hT[:, c, tbs(tb)], lambda c: hr[c][tb])
        cnt = {"e": 0, "sp": 0, "a": 0}
        for p_ in range(8):
            if p_ + 1 < 8:
                load_w(p_ + 1)
            W = Wp[p_ % 2]
            wo = Wo[p_ % 2]
            pstate["allowed"] = list(range(8))
            for tb in range(NTB):
                bq, bk = psalloc(), psalloc()
                for (bank, t3) in ((bq, 0), (bk, 1)):
                    for c in range(NCH):
                        op("pe", lambda: nc.tensor.matmul(psb(bank), lhsT=W.ap[:, c, t3, :], rhs=hT[:, c, tbs(tb)],
                                                          start=(c == 0), stop=(c == NCH - 1)),
                           reads=[W.res, hr[c][tb]], writes=[psr[bank]])
                op("act", lambda: nc.scalar.activation(out=qsT.ap[:, tbs(tb)], in_=psb(bq), func=AF.Copy, scale=0.125),
                   writes=[psr[bq], qsT.res])
                op("act", lambda: nc.scalar.copy(out=kT.ap[:, tbs(tb)], in_=psb(bk)), writes=[psr[bk], kT.res])
                op("dve", lambda: nc.vector.tensor_scalar(out=nkT.ap[:, tbs(tb)], in0=psb(bk), scalar1=-1.0, scalar2=None, op0=ALU.mult),
                   writes=[psr[bk], nkT.res])
            for t4 in range(4):
                bank = psalloc()
                for tt in range(4):
                    t = t4 * 4 + tt
                    for c in range(NCH):
                        op("pe", lambda: nc.tensor.matmul(PS[:, bank, tt * 128:(tt + 1) * 128], lhsT=hT[:, c, t * 128:(t + 1) * 128],
                                                          rhs=W.ap[:, c, 2, :], start=(c == 0), stop=(c == NCH - 1)),
                           reads=[W.res, hr[c][t // 4]], writes=[psr[bank]])
                op("act", lambda: nc.scalar.copy(out=vp.ap[:, t4 * 4:(t4 + 1) * 4, :],
                                                 in_=PS[:, bank, :].rearrange("p (t n) -> p t n", t=4)),
                   writes=[psr[bank], vp.res])
            pstate["allowed"] = list(range(6))
            for qg in range(4):
                q0 = qg * 512
                for hh in range(2):
                    op("pool", lambda: nc.gpsimd.memset(Rb[hh].ap, 0.0), writes=[Rb[hh].res])
                nsteps = 4 * qg + 4
                zb = {}

                def emit_z(step, hh):
                    kb = 4 * qg + 3 - step
                    c0 = max(0, kb - 4 * qg) * 128
                    bank = psalloc()
                    ps_ = slice(hh * 64, hh * 64 + 64)
                    op("pe", lambda: nc.tensor.matmul(PS[:, bank, c0:512], lhsT=kT.ap[ps_, kb * 128:(kb + 1) * 128],
                                                      rhs=qsT.ap[ps_, q0 + c0:q0 + 512], start=True, stop=True),
                       reads=[kT.res, qsT.res], writes=[psr[bank]])
                    zb[(step, hh)] = bank

                for hh in range(2):
                    emit_z(0, hh)
                for step in range(nsteps):
                    kb = 4 * qg + 3 - step
                    diag = kb >= 4 * qg
                    c0 = max(0, kb - 4 * qg) * 128
                    N = 512 - c0
                    sps, ats, cbs = [], [], []
                    for hh in range(2):
                        bank = zb.pop((step, hh))
                        eb = ebuf[cnt["e"] % 2]
                        cnt["e"] += 1
                        sp_ = spb[cnt["sp"] % 4]
                        cnt["sp"] += 1
                        op("act", lambda: nc.scalar.activation(out=eb.ap[:, c0:512], in_=PS[:, bank, c0:512], func=AF.Exp),
                           writes=[psr[bank], eb.res])
                        op("act", lambda: nc.scalar.activation(out=sp_.ap[:, c0:512], in_=eb.ap[:, c0:512], func=AF.Ln, bias=1.0),
                           reads=[eb.res], writes=[sp_.res])
                        if diag:
                            op("dve", lambda: nc.vector.tensor_tensor(out=sp_.ap[:, c0:c0 + 128], in0=sp_.ap[:, c0:c0 + 128],
                                                                      in1=maskb.ap, op=ALU.mult),
                               reads=[sp_.res, maskb.res], writes=[sp_.res])
                        sps.append(sp_)
                    for hh in range(2):
                        sp_ = sps[hh]
                        bank = psalloc()
                        cbs.append(bank)
                        ps_ = slice(hh * 64, hh * 64 + 64)
                        op("pe", lambda: nc.tensor.matmul(PS[:, bank, c0:512], lhsT=tri.ap, rhs=sp_.ap[:, c0:512], start=True, stop=False),
                           reads=[tri.res, sp_.res], writes=[psr[bank]])
                        if step > 0:
                            op("pe", lambda: nc.tensor.matmul(PS[:, bank, c0:512], lhsT=onesb.ap, rhs=Rb[hh].ap[:, c0:512],
                                                              start=False, stop=False),
                               reads=[onesb.res, Rb[hh].res], writes=[psr[bank]])
                        op("pe", lambda: nc.tensor.matmul(PS[:, bank, c0:512], lhsT=nkT.ap[ps_, kb * 128:(kb + 1) * 128],
                                                          rhs=qsT.ap[ps_, q0 + c0:q0 + 512], start=False, stop=True),
                           reads=[nkT.res, qsT.res], writes=[psr[bank]])
                    if step + 1 < nsteps:
                        for hh in range(2):
                            emit_z(step + 1, hh)
                    for hh in range(2):
                        sp_ = sps[hh]
                        bank = cbs[hh]
                        at = aTb[cnt["a"] % 4]
                        cnt["a"] += 1
                        op("act", lambda: nc.scalar.activation(out=at.ap[:, c0:512], in_=PS[:, bank, c0:512], func=AF.Exp, scale=-1.0),
                           writes=[psr[bank], at.res])
                        if diag:
                            op("dve", lambda: nc.vector.tensor_tensor(out=at.ap[:, c0:c0 + 128], in0=at.ap[:, c0:c0 + 128],
                                                                      in1=maskb.ap, op=ALU.mult),
                               reads=[at.res, maskb.res], writes=[at.res])
                        if step + 1 < nsteps:
                            op("pool", lambda: nc.gpsimd.tensor_tensor(out=Rb[hh].ap[:, c0:512], in0=Rb[hh].ap[:, c0:512],
                                                                       in1=sp_.ap[:, c0:512], op=ALU.add),
                               reads=[Rb[hh].res, sp_.res], writes=[Rb[hh].res])
                        ab = 6 + hh
                        op("pe", lambda: nc.tensor.matmul(PS[0:64, ab, c0:512], lhsT=vp.ap[:, kb, hh * 64:(hh + 1) * 64],
                                                          rhs=at.ap[:, c0:512], start=(step == 0), stop=(step == nsteps - 1),
                                                          skip_group_check=True),
                           reads=[vp.res, at.res], writes=[psr[ab]])
                for hh in range(2):
                    ab = 6 + hh
                    op("act", lambda: nc.scalar.copy(out=oT.ap[0:64, hh, q0:q0 + 512], in_=PS[0:64, ab, :]),
                       writes=[psr[ab], oT.res])
            pstate["allowed"] = list(range(6))
            for tb in range(NTB):
                for nn in range(NCH):
                    bank = psalloc()
                    for hh in range(2):
                        op("pe", lambda: nc.tensor.matmul(psb(bank), lhsT=wo.ap[0:64, hh, nn * 128:(nn + 1) * 128],
                                                          rhs=oT.ap[0:64, hh, tbs(tb)], start=(hh == 0), stop=(hh == 1)),
                           reads=[wo.res, oT.res], writes=[psr[bank]])
                    resid_add(nn, tb, bank)

    def body(b):
        load_x(b)
        for sl in sublayers:
            if sl[0] == "ret":
                retention(sl[1])
            elif sl[0] == "sb":
                stick(sl[1])
            else:
                ffn(sl[1])
        store_out(b)

    reset_sync()
    if nseq == 1:
        body(0)
        reset_sync()
    else:
        with nc.Fori(0, nseq, hint_back_edge=True) as b:
            body(b)
            reset_sync()
    return nc


def make_consts():
    bf = ml_dtypes.bfloat16
    c = {}
    c["c_identb"] = np.eye(128, dtype=np.float32).astype(bf)
    c["c_identf"] = np.eye(128, dtype=np.float32)
    jj = np.arange(128)
    c["c_tri"] = (jj[:, None] >= jj[None, :]).astype(np.float32).astype(bf)
    c["c_mask"] = (jj[:, None] < jj[None, :]).astype(np.float32).astype(bf)
    dm = np.zeros((128, 4, 128), np.float64)
    kd = np.zeros((128, 4), np.float64)
    ev = np.zeros((128, 4), np.float64)
    for h in range(4):
        g = 1.0 - 2.0 ** (-5.0 - h)
        k = jj.astype(np.float64)
        dm[:, h, :] = np.where(jj[None, :] >= jj[:, None], (g ** (-(k + 1.0)))[:, None] / 16.0, 0.0)
        kd[:, h] = g ** (127.0 - k) / 16.0
        ev[:, h] = EPS * g ** (-2.0 * (k + 1.0))
    c["c_dm"] = dm.reshape(128, 512).astype(np.float32)
    c["c_kdecs"] = kd.astype(np.float32)
    c["c_epsv"] = ev.astype(np.float32)
    pos = np.arange(S, dtype=np.float32)
    inv = (np.float32(10000.0) ** (-np.arange(0, 256, 2, dtype=np.float32) / np.float32(256))).astype(np.float32)
    ang = (pos[:, None] * inv[None, :]).astype(np.float32)
    c["c_cos"] = np.ascontiguousarray(np.cos(ang).T.astype(np.float32))
    c["c_sin"] = np.ascontiguousarray(np.sin(ang).T.astype(np.float32))
    return c


def layout_params(inp):
    f = lambda a: np.asarray(a, dtype=np.float32)
    gl = [f(inp["ret_norm"])[0], f(inp["ret_norm"])[1], f(inp["sb_norm"])[0], f(inp["sb_norm"])[1]]
    gl += [f(inp["ffn_norm"])[l] for l in range(4)] + [f(inp["final_norm"])]
    gains = np.stack([g.reshape(8, 128).T for g in gl], axis=1).reshape(128, 72)
    cwv = f(inp["ffn_conv_w"])
    cw = cwv.reshape(4, 3, 44, 128).transpose(3, 0, 2, 1).reshape(128, 4 * 44 * 3)
    cbv = f(inp["ffn_conv_b"])
    cb = cbv.reshape(4, 44, 128).transpose(2, 0, 1).reshape(128, 4 * 44)
    return {"c_gains": np.ascontiguousarray(gains), "c_cw": np.ascontiguousarray(cw), "c_cb": np.ascontiguousarray(cb)}


FULL = [("ret", 0), ("ffn", 0), ("sb", 0), ("ffn", 1), ("ret", 1), ("ffn", 2), ("sb", 1), ("ffn", 3)]


def run(inputs, nseq, sublayers, xs_per_core, trace=False):
    nc = build_program(nseq, sublayers)
    common = {}
    common.update(make_consts())
    common.update(layout_params(inputs))
    for sl in set(sublayers):
        names = {"ret": ("ret_w_in", "ret_w_out"), "sb": ("sb_w_in", "sb_w_out"), "ffn": ("ffn_w_up", "ffn_w_down")}[sl[0]]
        for k in names:
            common["%s_%d" % (k, sl[1])] = np.ascontiguousarray(np.asarray(inputs[k][sl[1]], dtype=np.float32))
    in_maps = []
    for xs in xs_per_core:
        m = dict(common)
        m["x"] = np.ascontiguousarray(xs)
        in_maps.append(m)
    res = run_bass_kernel_spmd(nc, in_maps, core_ids=list(range(len(xs_per_core))), trace=trace)
    return res


def kernel(**inputs):
    x = np.asarray(inputs["x"], dtype=np.float32)
    B = x.shape[0]
    per = B // NCORES
    xs = [x[i * per:(i + 1) * per] for i in range(NCORES)]
    res = run(inputs, per, FULL, xs)
    out = np.concatenate([np.asarray(r["out"], dtype=np.float32) for r in res.results], axis=0)
    return out
```

```python
import math
import numpy as np
import ml_dtypes
import concourse.bass as bass
import concourse.mybir as mybir
from concourse.bass_utils import run_bass_kernel_spmd

F32 = mybir.dt.float32
BF16 = mybir.dt.bfloat16
AF = mybir.ActivationFunctionType
ALU = mybir.AluOpType

D = 1024
S = 2048
NCH = 8
NTB = 4
DFF = 2816
NFC = 22
EPS = 1e-6
NCORES = 8


class Res:
    __slots__ = ("w", "r")
    registry = []

    def __init__(self):
        self.w = None
        self.r = {}
        Res.registry.append(self)


class Buf:
    __slots__ = ("ap", "res")

    def __init__(self, ap):
        self.ap = ap
        self.res = Res()


class Eng:
    def __init__(self, name, h, sem, raw):
        self.name = name
        self.h = h
        self.sem = sem
        self.cnt = 0
        self.waited = {}
        self.raw = raw


class Tr:
    def __init__(self, nc):
        self.nc = nc
        self.engs = {}
        for name, h, raw in (("pe", nc.tensor, False), ("act", nc.scalar, True),
                             ("dve", nc.vector, True), ("pool", nc.gpsimd, True),
                             ("sp", nc.sync, False)):
            self.engs[name] = Eng(name, h, nc.alloc_semaphore("sem_" + name), raw)
        self.ring = [nc.alloc_semaphore("dring%d" % i) for i in range(16)]
        self.ring_n = 0
        self.ring_val = [0] * 16

    def _collect(self, e, reads, writes, extra=()):
        deps = {}

        def add(tok, same_ok):
            if tok is None:
                return
            sem, val = tok
            if sem is e.sem and not same_ok:
                return
            k = id(sem)
            if e.waited.get(k, 0) >= val:
                return
            if k not in deps or deps[k][1] < val:
                deps[k] = tok

        for r in reads:
            add(r.w, e.raw)
        for w in writes:
            add(w.w, False)
            for tok in w.r.values():
                add(tok, False)
        for tok in extra:
            add(tok, False)
        return list(deps.values())

    def op(self, en, fn, reads=(), writes=()):
        e = self.engs[en]
        toks = self._collect(e, reads, writes)
        for tok in toks[:-1]:
            e.h.wait_ge(tok[0], tok[1])
        ins = fn()
        if toks:
            ins._wait_ge(toks[-1][0], toks[-1][1])
        for tok in toks:
            e.waited[id(tok[0])] = tok[1]
        e.cnt += 1
        ins.then_inc(e.sem, 1)
        me = (e.sem, e.cnt)
        for r in reads:
            r.r[id(e.sem)] = me
        for w in writes:
            w.w = me
            w.r = {}
        return ins

    def dma(self, out, in_, reads=(), writes=(), en="sp"):
        e = self.engs[en]
        i = self.ring_n % 16
        self.ring_n += 1
        sem = self.ring[i]
        prev = (sem, self.ring_val[i]) if self.ring_val[i] > 0 else None
        self.ring_val[i] += 16
        toks = self._collect(e, reads, writes, extra=(prev,) if prev else ())
        for tok in toks:
            e.h.wait_ge(tok[0], tok[1])
            e.waited[id(tok[0])] = tok[1]
        e.h.dma_start(out=out, in_=in_).then_inc(sem, 16)
        me = (sem, self.ring_val[i])
        for r in reads:
            r.r[id(sem)] = me
        for w in writes:
            w.w = me
            w.r = {}

    def all_tokens(self):
        toks = [(e.sem, e.cnt) for e in self.engs.values() if e.cnt > 0]
        toks += [(self.ring[i], self.ring_val[i]) for i in range(16) if self.ring_val[i] > 0]
        return toks

    def barrier(self, names=("pe", "act", "dve", "pool", "sp")):
        toks = self.all_tokens()
        for n in names:
            e = self.engs[n]
            for sem, val in toks:
                if sem is e.sem:
                    continue
                if e.waited.get(id(sem), 0) >= val:
                    continue
                e.h.wait_ge(sem, val)
                e.waited[id(sem)] = val


class Carver:
    def __init__(self, arena, base, limit):
        self.a = arena
        self.off = base
        self.limit = limit

    def take(self, shape, dt):
        n = 1
        for s_ in shape:
            n *= s_
        nbytes = n * (4 if dt is F32 else 2)
        start = self.off
        self.off += (nbytes + 63) // 64 * 64
        assert self.off <= self.limit, ("SBUF carve overflow", self.off, self.limit)
        ap = self.a[:, start // 2: start // 2 + nbytes // 2]
        if dt is F32:
            ap = ap.bitcast(F32)
        if len(shape) == 2:
            ap = ap.rearrange("p (a b) -> p a b", a=shape[0])
        elif len(shape) == 3:
            ap = ap.rearrange("p (a b c) -> p a b c", a=shape[0], b=shape[1])
        return ap

    def buf(self, shape, dt):
        return Buf(self.take(shape, dt))


ARENA_BYTES = 212000
R_BASE = 65536 + 32768 + 8192


def build_program(nseq, sublayers, do_prep=True):
    nc = bass.Bass("TRN2", target_bir_lowering=False)
    Res.registry = []
    tr = Tr(nc)
    op = tr.op

    def reset_sync():
        tr.barrier()
        nc.all_engine_barrier()
        for e in tr.engs.values():
            e.h.sem_clear(e.sem)
        for sem in tr.ring:
            nc.sync.sem_clear(sem)
        nc.all_engine_barrier()
        for e in tr.engs.values():
            e.cnt = 0
            e.waited = {}
        tr.ring_n = 0
        tr.ring_val = [0] * 16
        for r in Res.registry:
            r.w = None
            r.r = {}

    def din(name, shape, dt=F32):
        return nc.dram_tensor(name, list(shape), dt, kind="ExternalInput").ap()

    x_d = din("x", [nseq, S, D])
    out_d = nc.dram_tensor("out", [nseq, S, D], F32, kind="ExternalOutput").ap()
    used = set(sublayers)
    ret_w_in = {j: din("ret_w_in_%d" % j, [D, 6144]) for j in range(2) if ("ret", j) in used}
    ret_w_out = {j: din("ret_w_out_%d" % j, [2048, D]) for j in range(2) if ("ret", j) in used}
    sb_w_in = {j: din("sb_w_in_%d" % j, [D, 3072]) for j in range(2) if ("sb", j) in used}
    sb_w_out = {j: din("sb_w_out_%d" % j, [D, D]) for j in range(2) if ("sb", j) in used}
    ffn_w_up = {l: din("ffn_w_up_%d" % l, [D, 2 * DFF]) for l in range(4) if ("ffn", l) in used}
    ffn_w_down = {l: din("ffn_w_down_%d" % l, [DFF, D]) for l in range(4) if ("ffn", l) in used}
    c_gains = din("c_gains", [128, 9 * 8])
    c_cw = din("c_cw", [128, 4 * 44 * 3])
    c_cb = din("c_cb", [128, 4 * 44])
    c_identb = din("c_identb", [128, 128], BF16)
    c_identf = din("c_identf", [128, 128])
    c_tri = din("c_tri", [128, 128], BF16)
    c_mask = din("c_mask", [128, 128], BF16)
    c_dm = din("c_dm", [128, 4 * 128])
    c_kdecs = din("c_kdecs", [128, 4])
    c_epsv = din("c_epsv", [128, 4])
    c_cos = din("c_cos", [128, S])
    c_sin = din("c_sin", [128, S])

    def dscr(name, shape):
        return nc.dram_tensor(name, list(shape), BF16).ap()

    s_rq = [dscr("s_rq%d" % j, [4, 128, 2048]) for j in range(2)]
    s_rk = [dscr("s_rk%d" % j, [4, 128, 2048]) for j in range(2)]
    s_rv = [dscr("s_rv%d" % j, [4, 128, 4096]) for j in range(2)]
    s_rg = [dscr("s_rg%d" % j, [4, 128, 4096]) for j in range(2)]
    s_ro = [dscr("s_ro%d" % j, [4, 128, 4096]) for j in range(2)]
    s_sw = [dscr("s_sw%d" % j, [8, 128, 3072]) for j in range(2)]
    s_so = [dscr("s_so%d" % j, [8, 64, 2048]) for j in range(2)]
    s_fu = [dscr("s_fu%d" % l, [22, 128, 2048]) for l in range(4)]
    s_fd = [dscr("s_fd%d" % l, [128, 22 * 1024]) for l in range(4)]

    arena = nc.alloc_sbuf_tensor("arena", [128, ARENA_BYTES // 2], BF16)
    pc = Carver(arena, 0, R_BASE)
    xT = pc.take([NCH, S], F32)
    hT = pc.take([NCH, S], BF16)
    xr = [[Res() for _ in range(NTB)] for _ in range(NCH)]
    hr = [[Res() for _ in range(NTB)] for _ in range(NCH)]
    identb = pc.buf([128], BF16)
    identf = pc.buf([128], F32)
    onesb = pc.buf([128], BF16)
    tri = pc.buf([128], BF16)
    maskb = pc.buf([128], BF16)
    dm = pc.buf([4, 128], F32)
    kdecs = pc.buf([4], F32)
    epsv = pc.buf([4], F32)
    gains = pc.buf([72], F32)
    g32 = pc.buf([72], F32)
    cw = pc.buf([4 * 44 * 3], F32)
    cb = pc.buf([4 * 44], F32)
    epsc = pc.buf([8], F32)

    PS = nc.alloc_psum_tensor("ps", [128, 8, 512], F32)
    psr = [Res() for _ in range(8)]
    pstate = {"i": 0, "allowed": list(range(8))}

    def psalloc():
        a = pstate["allowed"]
        pstate["i"] = (pstate["i"] + 1) % len(a)
        return a[pstate["i"]]

    def psb(b):
        return PS[:, b, :]

    def psb16(b):
        return PS[:, b, :].bitcast(BF16)

    for b_, src in ((identb, c_identb), (identf, c_identf), (tri, c_tri), (maskb, c_mask),
                    (kdecs, c_kdecs), (epsv, c_epsv), (gains, c_gains), (cw, c_cw), (cb, c_cb)):
        tr.dma(out=b_.ap, in_=src, writes=[b_.res])
    tr.dma(out=dm.ap, in_=c_dm.rearrange("p (h c) -> p h c", h=4), writes=[dm.res])
    op("dve", lambda: nc.vector.memset(onesb.ap, 1.0), writes=[onesb.res])
    op("dve", lambda: nc.vector.memset(epsc.ap, 1024.0 * EPS), writes=[epsc.res])
    op("dve", lambda: nc.vector.tensor_scalar(out=g32.ap, in0=gains.ap, scalar1=32.0, scalar2=None,
                                               op0=ALU.mult), reads=[gains.res], writes=[g32.res])

    cast_rr = {"i": 0}

    def cast(out, in_, reads, writes):
        k = cast_rr["i"] % 3
        cast_rr["i"] += 1
        if k == 0:
            op("act", lambda: nc.scalar.copy(out=out, in_=in_), reads=reads, writes=writes)
        else:
            op("dve", lambda: nc.vector.tensor_copy(out, in_), reads=reads, writes=writes)

    used = set(sublayers)
    if do_prep:
        rc = Carver(arena, R_BASE, ARENA_BYTES)
        stg = [rc.buf([4096], F32) for _ in range(3)]
        bst = [rc.buf([4096], BF16) for _ in range(3)]
        pi = {"i": 0}

        def piece(loads, n_el, store_fn, cast_views=None, parts=128):
            k = pi["i"] % 3
            pi["i"] += 1
            sb_, bb_ = stg[k], bst[k]
            for (dst_fn, src) in loads:
                tr.dma(out=dst_fn(sb_.ap), in_=src, writes=[sb_.res])
            if cast_views is None:
                cast(bb_.ap[:parts, :n_el], sb_.ap[:parts, :n_el], [sb_.res], [bb_.res])
            else:
                o_, i_ = cast_views(bb_.ap, sb_.ap)
                cast(o_, i_, [sb_.res], [bb_.res])
            store_fn(bb_)

        for j in range(2):
            if ("ret", j) in used:
                w = ret_w_in[j].rearrange("(c p) n -> p c n", p=128)
                for h in range(4):
                    for (scr, c0, ns) in ((s_rq[j], h * 256, 256), (s_rk[j], 1024 + h * 256, 256),
                                          (s_rv[j], 2048 + h * 512, 512), (s_rg[j], 4096 + h * 512, 512)):
                        ne = 8 * ns
                        piece([(lambda a, ns=ns, ne=ne: a[:, :ne].rearrange("p (c n) -> p c n", c=8),
                                w[:, :, c0:c0 + ns])], ne,
                              lambda bb, scr=scr, h=h, ne=ne: tr.dma(out=scr[h], in_=bb.ap[:, :ne], reads=[bb.res]))
                    wo = ret_w_out[j][h * 512:(h + 1) * 512, :].rearrange("(e p) n -> p e n", p=128)
                    piece([(lambda a: a.rearrange("p (e n) -> p e n", e=4), wo)], 4096,
                          lambda bb, j=j, h=h: tr.dma(out=s_ro[j][h], in_=bb.ap, reads=[bb.res]))
            if ("sb", j) in used:
                w = sb_w_in[j].rearrange("(c p) n -> p c n", p=128)
                for p_ in range(8):
                    loads = []
                    for t3 in range(3):
                        loads.append((lambda a, t3=t3: a[:, :3072].rearrange("p (c t n) -> p c t n", c=8, t=3)[:, :, t3, :],
                                      w[:, :, t3 * 1024 + p_ * 128: t3 * 1024 + (p_ + 1) * 128]))
                    piece(loads, 3072,
                          lambda bb, j=j, p_=p_: tr.dma(out=s_sw[j][p_], in_=bb.ap[:, :3072], reads=[bb.res]))
                    wo = sb_w_out[j][p_ * 128:(p_ + 1) * 128, :].rearrange("(hh d) n -> d hh n", d=64)
                    piece([(lambda a: a[:64, :2048].rearrange("p (hh n) -> p hh n", hh=2), wo)], 2048,
                          lambda bb, j=j, p_=p_: tr.dma(out=s_so[j][p_], in_=bb.ap[:64, :2048], reads=[bb.res]),
                          parts=64)
        for l in range(4):
            if ("ffn", l) in used:
                w = ffn_w_up[l].rearrange("(c p) n -> p c n", p=128)
                for g in range(11):
                    loads = []
                    for gv in range(2):
                        loads.append((lambda a, gv=gv: a.rearrange("p (c gv n) -> p c gv n", c=8, gv=2)[:, :, gv, :],
                                      w[:, :, gv * DFF + g * 256: gv * DFF + (g + 1) * 256]))

                    def cviews(bb, sb_):
                        o_ = bb.rearrange("p (pr cg n) -> p pr cg n", pr=2, n=128)
                        i_ = sb_.rearrange("p (cg pr n) -> p pr cg n", pr=2, n=128)
                        return o_, i_

                    def store(bb, l=l, g=g):
                        tr.dma(out=s_fu[l][2 * g:2 * g + 2].rearrange("s p n -> p s n"),
                               in_=bb.ap.rearrange("p (s n) -> p s n", s=2), reads=[bb.res])

                    piece(loads, 4096, store, cast_views=cviews)
                wd = ffn_w_down[l].rearrange("(k p) n -> p k n", p=128)
                for q in range(6):
                    k0 = q * 4
                    nk = min(4, 22 - k0)
                    ne = nk * 1024
                    piece([(lambda a, nk=nk, ne=ne: a[:, :ne].rearrange("p (k n) -> p k n", k=nk), wd[:, k0:k0 + nk, :])], ne,
                          lambda bb, l=l, k0=k0, ne=ne: tr.dma(out=s_fd[l][:, k0 * 1024:k0 * 1024 + ne], in_=bb.ap[:, :ne],
                                                               reads=[bb.res]))
        tr.barrier()

    def tbs(tb):
        return slice(tb * 512, (tb + 1) * 512)

    def norm_block(tb, gi, sqs, rstd, dst_fn, dst_res):
        bank = psalloc()
        for c in range(NCH):
            sqb = sqs[c % len(sqs)]
            op("act", lambda: nc.scalar.activation(out=sqb.ap, in_=xT[:, c, tbs(tb)], func=AF.Square),
               reads=[xr[c][tb]], writes=[sqb.res])
            op("pe", lambda: nc.tensor.matmul(psb(bank), lhsT=onesb.ap, rhs=sqb.ap, start=(c == 0), stop=(c == NCH - 1)),
               reads=[sqb.res, onesb.res], writes=[psr[bank]])
        op("act", lambda: nc.scalar.activation(out=rstd.ap, in_=psb(bank), func=AF.Ln, bias=epsc.ap[:, 0:1]),
           reads=[epsc.res], writes=[psr[bank], rstd.res])
        op("act", lambda: nc.scalar.activation(out=rstd.ap, in_=rstd.ap, func=AF.Exp, scale=-0.5),
           reads=[rstd.res], writes=[rstd.res])
        for c in range(NCH):
            en = "dve"
            eh = nc.vector
            op(en, lambda: eh.scalar_tensor_tensor(out=dst_fn(c), in0=xT[:, c, tbs(tb)],
                                                   scalar=g32.ap[:, gi * 8 + c: gi * 8 + c + 1], in1=rstd.ap,
                                                   op0=ALU.mult, op1=ALU.mult),
               reads=[xr[c][tb], rstd.res, g32.res], writes=[dst_res(c)])

    def resid_add(n, tb, bank):
        op("dve", lambda: nc.vector.tensor_tensor(out=xT[:, n, tbs(tb)], in0=psb(bank), in1=xT[:, n, tbs(tb)], op=ALU.add),
           reads=[xr[n][tb]], writes=[psr[bank], xr[n][tb]])

    def load_x(b):
        tr.barrier()
        rc = Carver(arena, R_BASE, ARENA_BYTES)
        st = [rc.buf([1024], F32) for _ in range(2)]
        pstate["allowed"] = list(range(8))
        for t in range(16):
            sb_ = st[t % 2]
            tr.dma(out=sb_.ap, in_=x_d[b, t * 128:(t + 1) * 128, :], writes=[sb_.res])
            for half in range(2):
                bank = psalloc()
                for cc in range(4):
                    c = half * 4 + cc
                    op("pe", lambda: nc.tensor.transpose(PS[:, bank, cc * 128:(cc + 1) * 128], sb_.ap[:, c * 128:(c + 1) * 128], identf.ap),
                       reads=[sb_.res, identf.res], writes=[psr[bank]])
                tb = t // 4
                dst = xT[:, half * 4:(half + 1) * 4, t * 128:(t + 1) * 128]
                src = PS[:, bank, :].rearrange("p (c n) -> p c n", c=4)
                wr = [psr[bank]] + [xr[half * 4 + cc][tb] for cc in range(4)]
                if half == 0:
                    op("act", lambda: nc.scalar.copy(out=dst, in_=src), writes=wr)
                else:
                    op("dve", lambda: nc.vector.tensor_copy(dst, src), writes=wr)

    def store_out(b):
        tr.barrier()
        rc = Carver(arena, R_BASE, ARENA_BYTES)
        sqs = [rc.buf([512], BF16) for _ in range(3)]
        rstd = rc.buf([512], F32)
        yn = rc.buf([NCH, 512], F32)
        ynr = [Res() for _ in range(NCH)]
        ost = [rc.buf([1024], F32) for _ in range(2)]
        pstate["allowed"] = list(range(8))
        for tb in range(NTB):
            norm_block(tb, 8, sqs, rstd, lambda c: yn.ap[:, c, :], lambda c: ynr[c])
            for tt in range(4):
                t = tb * 4 + tt
                ob = ost[t % 2]
                for half in range(2):
                    bank = psalloc()
                    for cc in range(4):
                        c = half * 4 + cc
                        op("pe", lambda: nc.tensor.transpose(PS[:, bank, cc * 128:(cc + 1) * 128], yn.ap[:, c, tt * 128:(tt + 1) * 128], identf.ap),
                           reads=[ynr[c], identf.res], writes=[psr[bank]])
                    if half == 0:
                        op("act", lambda: nc.scalar.copy(out=ob.ap[:, 0:512], in_=psb(bank)), writes=[psr[bank], ob.res])
                    else:
                        op("dve", lambda: nc.vector.tensor_copy(ob.ap[:, 512:1024], psb(bank)), writes=[psr[bank], ob.res])
                tr.dma(out=out_d[b, t * 128:(t + 1) * 128, :], in_=ob.ap, reads=[ob.res])

    def ffn(l):
        tr.barrier()
        rc = Carver(arena, R_BASE, ARENA_BYTES)
        wd = rc.buf([NFC, 1024], BF16)
        wups = [rc.buf([8, 2, 128], BF16) for _ in range(3)]
        mT = rc.buf([NFC, 512], BF16)
        sqs = [rc.buf([512], BF16) for _ in range(3)]
        rstd = rc.buf([512], F32)
        U = [[rc.buf([514], F32) for _ in range(2)] for _ in range(2)]
        ACC = [[rc.buf([512], F32) for _ in range(2)] for _ in range(2)]
        H = rc.buf([44, 2], F32)
        pstate["allowed"] = list(range(8))
        op("dve", lambda: nc.vector.memset(H.ap, 0.0), writes=[H.res])
        seq = [(tb, i) for tb in range(NTB) for i in range(NFC)]
        issued = {"n": 0}

        def ensure(upto):
            while issued["n"] <= min(upto, len(seq) - 1):
                n = issued["n"]
                tb_, i_ = seq[n]
                wb = wups[n % 3]
                tr.dma(out=wb.ap, in_=s_fu[l][i_].rearrange("p (c g n) -> p c g n", c=8, g=2), writes=[wb.res])
                if n == 1:
                    tr.dma(out=wd.ap, in_=s_fd[l].rearrange("p (k n) -> p k n", k=NFC), writes=[wd.res])
                issued["n"] += 1

        cwl = lambda i, gv, k: cw.ap[:, ((l * 44 + gv * 22 + i) * 3 + k):((l * 44 + gv * 22 + i) * 3 + k + 1)]
        cbl = lambda i, gv: cb.ap[:, (l * 44 + gv * 22 + i):(l * 44 + gv * 22 + i + 1)]
        n = 0
        norm_block(0, 4 + l, sqs, rstd, lambda c: hT[:, c, tbs(0)], lambda c: hr[c][0])
        for tb in range(NTB):
            for i in range(NFC):
                ensure(n + 2)
                wb = wups[n % 3]
                par = n % 2
                n += 1
                banks = []
                for gv in range(2):
                    bank = psalloc()
                    banks.append(bank)
                    for c in range(NCH):
                        op("pe", lambda: nc.tensor.matmul(psb(bank), lhsT=wb.ap[:, c, gv, :], rhs=hT[:, c, tbs(tb)],
                                                          start=(c == 0), stop=(c == NCH - 1)),
                           reads=[wb.res, hr[c][tb]], writes=[psr[bank]])
                for gv in range(2):
                    bank = banks[gv]
                    u = U[gv][par]
                    acc = ACC[gv][par]
                    hcol = H.ap[:, gv * 22 + i, :]
                    op("act", lambda: nc.scalar.copy(out=u.ap[:, 0:2], in_=hcol), reads=[H.res], writes=[u.res])
                    op("act", lambda: nc.scalar.copy(out=u.ap[:, 2:514], in_=psb(bank)), writes=[psr[bank], u.res])
                    op("act", lambda: nc.scalar.copy(out=hcol, in_=u.ap[:, 512:514]), reads=[u.res], writes=[H.res])
                    e1 = "pool"
                    e2 = "dve"
                    h1 = nc.gpsimd
                    h2 = nc.vector
                    op(e1, lambda: h1.tensor_scalar(out=acc.ap, in0=u.ap[:, 2:514], scalar1=cwl(i, gv, 2), scalar2=cbl(i, gv),
                                                    op0=ALU.mult, op1=ALU.add),
                       reads=[u.res, cw.res, cb.res], writes=[acc.res])
                    op(e2, lambda: h2.scalar_tensor_tensor(out=acc.ap, in0=u.ap[:, 1:513], scalar=cwl(i, gv, 1), in1=acc.ap,
                                                           op0=ALU.mult, op1=ALU.add),
                       reads=[u.res, acc.res, cw.res], writes=[acc.res])
                    op(e2, lambda: h2.scalar_tensor_tensor(out=acc.ap, in0=u.ap[:, 0:512], scalar=cwl(i, gv, 0), in1=acc.ap,
                                                           op0=ALU.mult, op1=ALU.add),
                       reads=[u.res, acc.res, cw.res], writes=[acc.res])
                ag, av = ACC[0][par], ACC[1][par]
                op("act", lambda: nc.scalar.activation(out=ag.ap, in_=ag.ap, func=AF.Silu), reads=[ag.res], writes=[ag.res])
                op("dve", lambda: nc.vector.tensor_tensor(out=mT.ap[:, i, :], in0=ag.ap, in1=av.ap, op=ALU.mult),
                   reads=[ag.res, av.res], writes=[mT.res])
            if tb + 1 < NTB:
                norm_block(tb + 1, 4 + l, sqs, rstd, lambda c: hT[:, c, tbs(tb + 1)], lambda c: hr[c][tb + 1])
            for nn in range(NCH):
                bank = psalloc()
                for k in range(NFC):
                    op("pe", lambda: nc.tensor.matmul(psb(bank), lhsT=wd.ap[:, k, nn * 128:(nn + 1) * 128], rhs=mT.ap[:, k, :],
                                                      start=(k == 0), stop=(k == NFC - 1)),
                       reads=[wd.res, mT.res], writes=[psr[bank]])
                resid_add(nn, tb, bank)

    def retention(j):
        tr.barrier()
        rc = Carver(arena, R_BASE, ARENA_BYTES)
        Wq = [rc.buf([8, 256], BF16) for _ in range(2)]
        Wk = [rc.buf([8, 256], BF16) for _ in range(2)]
        Wv = rc.buf([8, 512], BF16)
        Wg = rc.buf([8, 512], BF16)
        Wo = rc.buf([4, 1024], BF16)
        qT = rc.buf([2, S], BF16)
        kT = rc.buf([2, S], BF16)
        yT = [rc.buf([4, 512], BF16) for _ in range(2)]
        cs = [[rc.buf([512], F32) for _ in range(2)] for _ in range(2)]
        tmp = [rc.buf([512], F32) for _ in range(4)]
        vc = [rc.buf([512], BF16) for _ in range(2)]
        sgc = [rc.buf([512], BF16) for _ in range(2)]
        sT = [rc.buf([128], BF16) for _ in range(2)]
        kdc = [rc.buf([256], BF16) for _ in range(2)]
        S32 = rc.buf([2, 512], F32)
        Sbf = [rc.buf([2, 512], BF16) for _ in range(2)]
        yc = [rc.buf([512], BF16) for _ in range(2)]
        junk = rc.buf([512], BF16)
        st = rc.buf([8], F32)
        sqs = [rc.buf([512], BF16) for _ in range(2)]
        rstd = rc.buf([512], F32)
        pstate["allowed"] = list(range(8))
        gam = [1.0 - 2.0 ** (-5.0 - h) for h in range(4)]

        def load_qk(h):
            tr.dma(out=Wq[h % 2].ap, in_=s_rq[j][h].rearrange("p (c n) -> p c n", c=8), writes=[Wq[h % 2].res])
            tr.dma(out=Wk[h % 2].ap, in_=s_rk[j][h].rearrange("p (c n) -> p c n", c=8), writes=[Wk[h % 2].res])

        load_qk(0)
        for tb in range(NTB):
            norm_block(tb, j, sqs, rstd, lambda c: hT[:, c, tbs(tb)], lambda c: hr[c][tb])
        csn = 0
        for h in range(4):
            tr.dma(out=Wv.ap, in_=s_rv[j][h].rearrange("p (c n) -> p c n", c=8), writes=[Wv.res])
            tr.dma(out=Wg.ap, in_=s_rg[j][h].rearrange("p (c n) -> p c n", c=8), writes=[Wg.res])
            tr.dma(out=Wo.ap, in_=s_ro[j][h].rearrange("p (e n) -> p e n", e=4), writes=[Wo.res])
            if h + 1 < 4:
                load_qk(h + 1)
            for tb in range(NTB):
                cb_, sb_ = cs[csn % 2]
                csn += 1
                tr.dma(out=cb_.ap, in_=c_cos[:, tbs(tb)], writes=[cb_.res])
                tr.dma(out=sb_.ap, in_=c_sin[:, tbs(tb)], writes=[sb_.res])
                for (W, dst) in ((Wq[h % 2], qT), (Wk[h % 2], kT)):
                    b1, b2 = psalloc(), psalloc()
                    for (bank, half) in ((b1, 0), (b2, 1)):
                        for c in range(NCH):
                            op("pe", lambda: nc.tensor.matmul(psb(bank), lhsT=W.ap[:, c, half * 128:(half + 1) * 128],
                                                              rhs=hT[:, c, tbs(tb)], start=(c == 0), stop=(c == NCH - 1)),
                               reads=[W.res, hr[c][tb]], writes=[psr[bank]])
                    t1, t2, t3, t4 = tmp
                    op("dve", lambda: nc.vector.tensor_tensor(out=t1.ap, in0=psb(b1), in1=cb_.ap, op=ALU.mult),
                       reads=[cb_.res], writes=[psr[b1], t1.res])
                    op("dve", lambda: nc.vector.tensor_tensor(out=t3.ap, in0=psb(b1), in1=sb_.ap, op=ALU.mult),
                       reads=[sb_.res], writes=[psr[b1], t3.res])
                    op("dve", lambda: nc.vector.tensor_tensor(out=t2.ap, in0=psb(b2), in1=sb_.ap, op=ALU.mult),
                       reads=[sb_.res], writes=[psr[b2], t2.res])
                    op("dve", lambda: nc.vector.tensor_tensor(out=t4.ap, in0=psb(b2), in1=cb_.ap, op=ALU.mult),
                       reads=[cb_.res], writes=[psr[b2], t4.res])
                    op("pool", lambda: nc.gpsimd.tensor_tensor(out=dst.ap[:, 0, tbs(tb)], in0=t1.ap, in1=t2.ap, op=ALU.subtract),
                       reads=[t1.res, t2.res], writes=[dst.res])
                    op("pool", lambda: nc.gpsimd.tensor_tensor(out=dst.ap[:, 1, tbs(tb)], in0=t3.ap, in1=t4.ap, op=ALU.add),
                       reads=[t3.res, t4.res], writes=[dst.res])
            def emit_A(c):
                tb = c // 4
                ts_ = slice(c * 128, (c + 1) * 128)
                v_, sg_ = vc[c % 2], sgc[c % 2]
                bv, bg = psalloc(), psalloc()
                for kc in range(NCH):
                    op("pe", lambda: nc.tensor.matmul(psb(bv), lhsT=hT[:, kc, ts_], rhs=Wv.ap[:, kc, :], start=(kc == 0), stop=(kc == 7)),
                       reads=[hr[kc][tb], Wv.res], writes=[psr[bv]])
                for kc in range(NCH):
                    op("pe", lambda: nc.tensor.matmul(psb(bg), lhsT=hT[:, kc, ts_], rhs=Wg.ap[:, kc, :], start=(kc == 0), stop=(kc == 7)),
                       reads=[hr[kc][tb], Wg.res], writes=[psr[bg]])
                op("act", lambda: nc.scalar.copy(out=v_.ap, in_=psb(bv)), writes=[psr[bv], v_.res])
                op("act", lambda: nc.scalar.activation(out=sg_.ap, in_=psb(bg), func=AF.Silu), writes=[psr[bg], sg_.res])

            emit_A(0)
            for c in range(16):
                tb = c // 4
                ts_ = slice(c * 128, (c + 1) * 128)
                v_, sg_, sT_, kd_, y_ = vc[c % 2], sgc[c % 2], sT[c % 2], kdc[c % 2], yc[c % 2]
                bs = psalloc()
                for dc in range(2):
                    op("pe", lambda: nc.tensor.matmul(PS[:, bs, 0:128], lhsT=kT.ap[:, dc, ts_], rhs=qT.ap[:, dc, ts_],
                                                      start=(dc == 0), stop=(dc == 1)),
                       reads=[kT.res, qT.res], writes=[psr[bs]])
                op("dve", lambda: nc.vector.tensor_tensor(out=sT_.ap, in0=PS[:, bs, 0:128], in1=dm.ap[:, h, :], op=ALU.mult),
                   reads=[dm.res], writes=[psr[bs], sT_.res])
                bo = psalloc()
                sb_prev = Sbf[(c + 1) % 2]
                op("pe", lambda: nc.tensor.matmul(psb(bo), lhsT=sT_.ap, rhs=v_.ap, start=True, stop=(c == 0)),
                   reads=[sT_.res, v_.res], writes=[psr[bo]])
                if c > 0:
                    for dc in range(2):
                        op("pe", lambda: nc.tensor.matmul(psb(bo), lhsT=qT.ap[:, dc, ts_], rhs=sb_prev.ap[:, dc, :],
                                                          start=False, stop=(dc == 1)),
                           reads=[qT.res, sb_prev.res], writes=[psr[bo]])
                if c < 15:
                    bt = psalloc()
                    for dc in range(2):
                        op("pe", lambda: nc.tensor.transpose(psb16(bt)[:, dc * 128:(dc + 1) * 128], kT.ap[:, dc, ts_], identb.ap),
                           reads=[kT.res, identb.res], writes=[psr[bt]])
                    op("act", lambda: nc.scalar.activation(out=kd_.ap, in_=psb16(bt)[:, 0:256], func=AF.Copy,
                                                           scale=kdecs.ap[:, h:h + 1]),
                       reads=[kdecs.res], writes=[psr[bt], kd_.res])
                    emit_A(c + 1)
                    sb_new = Sbf[c % 2]
                    for dc in range(2):
                        bu = psalloc()
                        op("pe", lambda: nc.tensor.matmul(psb(bu), lhsT=kd_.ap[:, dc * 128:(dc + 1) * 128], rhs=v_.ap, start=True, stop=True),
                           reads=[kd_.res, v_.res], writes=[psr[bu]])
                        if c == 0:
                            op("dve", lambda: nc.vector.tensor_copy(S32.ap[:, dc, :], psb(bu)), writes=[psr[bu], S32.res])
                        else:
                            op("dve", lambda: nc.vector.scalar_tensor_tensor(out=S32.ap[:, dc, :], in0=S32.ap[:, dc, :],
                                                                             scalar=float(gam[h] ** 128), in1=psb(bu),
                                                                             op0=ALU.mult, op1=ALU.add),
                               reads=[S32.res], writes=[psr[bu], S32.res])
                    op("act", lambda: nc.scalar.copy(out=sb_new.ap, in_=S32.ap), reads=[S32.res], writes=[sb_new.res])
                op("act", lambda: nc.scalar.activation(out=junk.ap, in_=psb(bo), func=AF.Square, accum_out=st.ap[:, 0:1]),
                   writes=[psr[bo], junk.res, st.res])
                op("act", lambda: nc.scalar.activation(out=st.ap[:, 1:2], in_=st.ap[:, 0:1], func=AF.Ln, scale=1.0 / 512.0,
                                                       bias=epsv.ap[:, h:h + 1]),
                   reads=[st.res, epsv.res], writes=[st.res])
                op("act", lambda: nc.scalar.activation(out=st.ap[:, 2:3], in_=st.ap[:, 1:2], func=AF.Exp, scale=-0.5),
                   reads=[st.res], writes=[st.res])
                op("dve", lambda: nc.vector.scalar_tensor_tensor(out=y_.ap, in0=psb(bo), scalar=st.ap[:, 2:3], in1=sg_.ap,
                                                                 op0=ALU.mult, op1=ALU.mult),
                   reads=[st.res, sg_.res], writes=[psr[bo], y_.res])
                by = psalloc()
                for e in range(4):
                    op("pe", lambda: nc.tensor.transpose(psb16(by)[:, e * 128:(e + 1) * 128], y_.ap[:, e * 128:(e + 1) * 128], identb.ap),
                       reads=[y_.res, identb.res], writes=[psr[by]])
                yt = yT[tb % 2]
                op("act", lambda: nc.scalar.copy(out=yt.ap[:, :, (c % 4) * 128:(c % 4 + 1) * 128],
                                                 in_=psb16(by)[:, 0:512].rearrange("p (e n) -> p e n", e=4)),
                   writes=[psr[by], yt.res])
                if c % 4 == 3:
                    for nn in range(NCH):
                        bank = psalloc()
                        for e in range(4):
                            op("pe", lambda: nc.tensor.matmul(psb(bank), lhsT=Wo.ap[:, e, nn * 128:(nn + 1) * 128], rhs=yt.ap[:, e, :],
                                                              start=(e == 0), stop=(e == 3)),
                               reads=[Wo.res, yt.res], writes=[psr[bank]])
                        resid_add(nn, tb, bank)

    def stick(j):
        tr.barrier()
        rc = Carver(arena, R_BASE, ARENA_BYTES)
        Wp = [rc.buf([8, 3, 128], BF16) for _ in range(2)]
        Wo = [rc.buf([2, 1024], BF16) for _ in range(2)]
        qsT = rc.buf([S], BF16)
        kT = rc.buf([S], BF16)
        vp = rc.buf([16, 128], BF16)
        oT = rc.buf([2, S], BF16)
        ebuf = [rc.buf([512], F32) for _ in range(4)]
        ecb = [rc.buf([512], F32) for _ in range(2)]
        spb = [rc.buf([512], BF16) for _ in range(4)]
        Rb = [rc.buf([512], BF16) for _ in range(2)]
        aTb = [rc.buf([512], BF16) for _ in range(4)]
        sqs = [rc.buf([512], BF16) for _ in range(3)]
        rstd = rc.buf([512], F32)
        pstate["allowed"] = list(range(8))

        def load_w(p_):
            tr.dma(out=Wp[p_ % 2].ap, in_=s_sw[j][p_].rearrange("p (c t n) -> p c t n", c=8, t=3), writes=[Wp[p_ % 2].res])
            tr.dma(out=Wo[p_ % 2].ap[:64], in_=s_so[j][p_].rearrange("p (h n) -> p h n", h=2), writes=[Wo[p_ % 2].res])

        load_w(0)
        for tb in range(NTB):
            norm_block(tb, 2 + j, sqs, rstd, lambda c: hT[:, c, tbs(tb)], lambda c: hr[c][tb])
        cnt = {"e": 0, "sp": 0, "a": 0}
        for p_ in range(8):
            if p_ + 1 < 8:
                load_w(p_ + 1)
            W = Wp[p_ % 2]
            wo = Wo[p_ % 2]
            pstate["allowed"] = list(range(8))
            for tb in range(NTB):
                bq, bk = psalloc(), psalloc()
                for (bank, t3) in ((bq, 0), (bk, 1)):
                    for c in range(NCH):
                        op("pe", lambda: nc.tensor.matmul(psb(bank), lhsT=W.ap[:, c, t3, :], rhs=hT[:, c, tbs(tb)],
                                                          start=(c == 0), stop=(c == NCH - 1)),
                           reads=[W.res, hr[c][tb]], writes=[psr[bank]])
                op("act", lambda: nc.scalar.activation(out=qsT.ap[:, tbs(tb)], in_=psb(bq), func=AF.Copy, scale=0.125),
                   writes=[psr[bq], qsT.res])
                op("act", lambda: nc.scalar.copy(out=kT.ap[:, tbs(tb)], in_=psb(bk)), writes=[psr[bk], kT.res])
            for t4 in range(4):
                bank = psalloc()
                for tt in range(4):
                    t = t4 * 4 + tt
                    for c in range(NCH):
                        op("pe", lambda: nc.tensor.matmul(PS[:, bank, tt * 128:(tt + 1) * 128], lhsT=hT[:, c, t * 128:(t + 1) * 128],
                                                          rhs=W.ap[:, c, 2, :], start=(c == 0), stop=(c == NCH - 1)),
                           reads=[W.res, hr[c][t // 4]], writes=[psr[bank]])
                op("act", lambda: nc.scalar.copy(out=vp.ap[:, t4 * 4:(t4 + 1) * 4, :],
                                                 in_=PS[:, bank, :].rearrange("p (t n) -> p t n", t=4)),
                   writes=[psr[bank], vp.res])
            pstate["allowed"] = list(range(6))
            for qg in range(4):
                q0 = qg * 512
                for hh in range(2):
                    op("pool", lambda: nc.gpsimd.memset(Rb[hh].ap, 0.0), writes=[Rb[hh].res])
                nsteps = 4 * qg + 4
                zb = {}

                def emit_z(step, hh):
                    kb = 4 * qg + 3 - step
                    c0 = max(0, kb - 4 * qg) * 128
                    bank = psalloc()
                    ps_ = slice(hh * 64, hh * 64 + 64)
                    op("pe", lambda: nc.tensor.matmul(PS[:, bank, c0:512], lhsT=kT.ap[ps_, kb * 128:(kb + 1) * 128],
                                                      rhs=qsT.ap[ps_, q0 + c0:q0 + 512], start=True, stop=True),
                       reads=[kT.res, qsT.res], writes=[psr[bank]])
                    zb[(step, hh)] = bank

                for hh in range(2):
                    emit_z(0, hh)
                for step in range(nsteps):
                    kb = 4 * qg + 3 - step
                    diag = kb >= 4 * qg
                    c0 = max(0, kb - 4 * qg) * 128
                    N = 512 - c0
                    sps, ats, cbs, ebs = [], [], [], []
                    for hh in range(2):
                        bank = zb.pop((step, hh))
                        eb = ebuf[cnt["e"] % 4]
                        cnt["e"] += 1
                        ebs.append(eb)
                        sp_ = spb[cnt["sp"] % 4]
                        cnt["sp"] += 1
                        op("act", lambda: nc.scalar.activation(out=eb.ap[:, c0:512], in_=PS[:, bank, c0:512], func=AF.Exp),
                           writes=[psr[bank], eb.res])
                        op("act", lambda: nc.scalar.activation(out=sp_.ap[:, c0:512], in_=eb.ap[:, c0:512], func=AF.Ln, bias=1.0),
                           reads=[eb.res], writes=[sp_.res])
                        if diag:
                            op("dve", lambda: nc.vector.tensor_tensor(out=sp_.ap[:, c0:c0 + 128], in0=sp_.ap[:, c0:c0 + 128],
                                                                      in1=maskb.ap, op=ALU.mult),
                               reads=[sp_.res, maskb.res], writes=[sp_.res])
                        sps.append(sp_)
                    for hh in range(2):
                        sp_ = sps[hh]
                        bank = psalloc()
                        cbs.append(bank)
                        ps_ = slice(hh * 64, hh * 64 + 64)
                        op("pe", lambda: nc.tensor.matmul(PS[:, bank, c0:512], lhsT=tri.ap, rhs=sp_.ap[:, c0:512], start=True, stop=(step == 0)),
                           reads=[tri.res, sp_.res], writes=[psr[bank]])
                        if step > 0:
                            op("pe", lambda: nc.tensor.matmul(PS[:, bank, c0:512], lhsT=onesb.ap, rhs=Rb[hh].ap[:, c0:512],
                                                              start=False, stop=True),
                               reads=[onesb.res, Rb[hh].res], writes=[psr[bank]])
                    if step + 1 < nsteps:
                        for hh in range(2):
                            emit_z(step + 1, hh)
                    for hh in range(2):
                        sp_ = sps[hh]
                        bank = cbs[hh]
                        at = aTb[cnt["a"] % 4]
                        cnt["a"] += 1
                        ec = ecb[hh]
                        eb = ebs[hh]
                        op("act", lambda: nc.scalar.activation(out=ec.ap[:, c0:512], in_=PS[:, bank, c0:512], func=AF.Exp, scale=-1.0),
                           writes=[psr[bank], ec.res])
                        op("dve", lambda: nc.vector.tensor_tensor(out=at.ap[:, c0:512], in0=eb.ap[:, c0:512], in1=ec.ap[:, c0:512], op=ALU.mult),
                           reads=[eb.res, ec.res], writes=[at.res])
                        if diag:
                            op("dve", lambda: nc.vector.tensor_tensor(out=at.ap[:, c0:c0 + 128], in0=at.ap[:, c0:c0 + 128],
                                                                      in1=maskb.ap, op=ALU.mult),
                               reads=[at.res, maskb.res], writes=[at.res])
                        if step + 1 < nsteps:
                            op("pool", lambda: nc.gpsimd.tensor_tensor(out=Rb[hh].ap[:, c0:512], in0=Rb[hh].ap[:, c0:512],
                                                                       in1=sp_.ap[:, c0:512], op=ALU.add),
                               reads=[Rb[hh].res, sp_.res], writes=[Rb[hh].res])
                        ab = 6 + hh
                        op("pe", lambda: nc.tensor.matmul(PS[0:64, ab, c0:512], lhsT=vp.ap[:, kb, hh * 64:(hh + 1) * 64],
                                                          rhs=at.ap[:, c0:512], start=(step == 0), stop=(step == nsteps - 1),
                                                          skip_group_check=True),
                           reads=[vp.res, at.res], writes=[psr[ab]])
                for hh in range(2):
                    ab = 6 + hh
                    op("act", lambda: nc.scalar.copy(out=oT.ap[0:64, hh, q0:q0 + 512], in_=PS[0:64, ab, :]),
                       writes=[psr[ab], oT.res])
            pstate["allowed"] = list(range(6))
            for tb in range(NTB):
                for nn in range(NCH):
                    bank = psalloc()
                    for hh in range(2):
                        op("pe", lambda: nc.tensor.matmul(psb(bank), lhsT=wo.ap[0:64, hh, nn * 128:(nn + 1) * 128],
                                                          rhs=oT.ap[0:64, hh, tbs(tb)], start=(hh == 0), stop=(hh == 1)),
                           reads=[wo.res, oT.res], writes=[psr[bank]])
                    resid_add(nn, tb, bank)

    def body(b):
        load_x(b)
        for sl in sublayers:
            if sl[0] == "ret":
                retention(sl[1])
            elif sl[0] == "sb":
                stick(sl[1])
            else:
                ffn(sl[1])
        store_out(b)

    reset_sync()
    if nseq == 1:
        body(0)
        reset_sync()
    else:
        with nc.Fori(0, nseq, hint_back_edge=True) as b:
            body(b)
            reset_sync()
    return nc


def make_consts():
    bf = ml_dtypes.bfloat16
    c = {}
    c["c_identb"] = np.eye(128, dtype=np.float32).astype(bf)
    c["c_identf"] = np.eye(128, dtype=np.float32)
    jj = np.arange(128)
    c["c_tri"] = (jj[:, None] >= jj[None, :]).astype(np.float32).astype(bf)
    c["c_mask"] = (jj[:, None] < jj[None, :]).astype(np.float32).astype(bf)
    dm = np.zeros((128, 4, 128), np.float64)
    kd = np.zeros((128, 4), np.float64)
    ev = np.zeros((128, 4), np.float64)
    for h in range(4):
        g = 1.0 - 2.0 ** (-5.0 - h)
        k = jj.astype(np.float64)
        dm[:, h, :] = np.where(jj[None, :] >= jj[:, None], (g ** (-(k + 1.0)))[:, None] / 16.0, 0.0)
        kd[:, h] = g ** (127.0 - k) / 16.0
        ev[:, h] = EPS * g ** (-2.0 * (k + 1.0))
    c["c_dm"] = dm.reshape(128, 512).astype(np.float32)
    c["c_kdecs"] = kd.astype(np.float32)
    c["c_epsv"] = ev.astype(np.float32)
    pos = np.arange(S, dtype=np.float32)
    inv = (np.float32(10000.0) ** (-np.arange(0, 256, 2, dtype=np.float32) / np.float32(256))).astype(np.float32)
    ang = (pos[:, None] * inv[None, :]).astype(np.float32)
    c["c_cos"] = np.ascontiguousarray(np.cos(ang).T.astype(np.float32))
    c["c_sin"] = np.ascontiguousarray(np.sin(ang).T.astype(np.float32))
    return c


def layout_params(inp):
    f = lambda a: np.asarray(a, dtype=np.float32)
    gl = [f(inp["ret_norm"])[0], f(inp["ret_norm"])[1], f(inp["sb_norm"])[0], f(inp["sb_norm"])[1]]
    gl += [f(inp["ffn_norm"])[l] for l in range(4)] + [f(inp["final_norm"])]
    gains = np.stack([g.reshape(8, 128).T for g in gl], axis=1).reshape(128, 72)
    cwv = f(inp["ffn_conv_w"])
    cw = cwv.reshape(4, 3, 44, 128).transpose(3, 0, 2, 1).reshape(128, 4 * 44 * 3)
    cbv = f(inp["ffn_conv_b"])
    cb = cbv.reshape(4, 44, 128).transpose(2, 0, 1).reshape(128, 4 * 44)
    return {"c_gains": np.ascontiguousarray(gains), "c_cw": np.ascontiguousarray(cw), "c_cb": np.ascontiguousarray(cb)}


FULL = [("ret", 0), ("ffn", 0), ("sb", 0), ("ffn", 1), ("ret", 1), ("ffn", 2), ("sb", 1), ("ffn", 3)]


def run(inputs, nseq, sublayers, xs_per_core, trace=False):
    nc = build_program(nseq, sublayers)
    common = {}
    common.update(make_consts())
    common.update(layout_params(inputs))
    for sl in set(sublayers):
        names = {"ret": ("ret_w_in", "ret_w_out"), "sb": ("sb_w_in", "sb_w_out"), "ffn": ("ffn_w_up", "ffn_w_down")}[sl[0]]
        for k in names:
            common["%s_%d" % (k, sl[1])] = np.ascontiguousarray(np.asarray(inputs[k][sl[1]], dtype=np.float32))
    in_maps = []
    for xs in xs_per_core:
        m = dict(common)
        m["x"] = np.ascontiguousarray(xs)
        in_maps.append(m)
    res = run_bass_kernel_spmd(nc, in_maps, core_ids=list(range(len(xs_per_core))), trace=trace)
    return res


def kernel(**inputs):
    x = np.asarray(inputs["x"], dtype=np.float32)
    B = x.shape[0]
    per = B // NCORES
    xs = [x[i * per:(i + 1) * per] for i in range(NCORES)]
    res = run(inputs, per, FULL, xs)
    out = np.concatenate([np.asarray(r["out"], dtype=np.float32) for r in res.results], axis=0)
    return out
```

```python
import math
import numpy as np
import ml_dtypes
import concourse.bass as bass
import concourse.mybir as mybir
from concourse.bass_utils import run_bass_kernel_spmd

F32 = mybir.dt.float32
BF16 = mybir.dt.bfloat16
AF = mybir.ActivationFunctionType
ALU = mybir.AluOpType

D = 1024
S = 2048
NCH = 8
NTB = 4
DFF = 2816
NFC = 22
EPS = 1e-6
NCORES = 8
import os
FILL = int(os.environ.get('SB_FILL', '1'))
FILLN = int(os.environ.get('SB_FILLN', '256'))
RFILL = int(os.environ.get('RET_FILL', '0'))


class Res:
    __slots__ = ("w", "r")
    registry = []

    def __init__(self):
        self.w = None
        self.r = {}
        Res.registry.append(self)


class Buf:
    __slots__ = ("ap", "res")

    def __init__(self, ap):
        self.ap = ap
        self.res = Res()


class Eng:
    def __init__(self, name, h, sem, raw):
        self.name = name
        self.h = h
        self.sem = sem
        self.cnt = 0
        self.waited = {}
        self.raw = raw


class Tr:
    def __init__(self, nc):
        self.nc = nc
        self.engs = {}
        for name, h, raw in (("pe", nc.tensor, False), ("act", nc.scalar, True),
                             ("dve", nc.vector, True), ("pool", nc.gpsimd, True),
                             ("sp", nc.sync, False)):
            self.engs[name] = Eng(name, h, nc.alloc_semaphore("sem_" + name), raw)
        self.ring = [nc.alloc_semaphore("dring%d" % i) for i in range(16)]
        self.ring_n = 0
        self.ring_val = [0] * 16

    def _collect(self, e, reads, writes, extra=()):
        deps = {}

        def add(tok, same_ok):
            if tok is None:
                return
            sem, val = tok
            if sem is e.sem and not same_ok:
                return
            k = id(sem)
            if e.waited.get(k, 0) >= val:
                return
            if k not in deps or deps[k][1] < val:
                deps[k] = tok

        for r in reads:
            add(r.w, e.raw)
        for w in writes:
            add(w.w, False)
            for tok in w.r.values():
                add(tok, False)
        for tok in extra:
            add(tok, False)
        return list(deps.values())

    def op(self, en, fn, reads=(), writes=()):
        e = self.engs[en]
        toks = self._collect(e, reads, writes)
        for tok in toks[:-1]:
            e.h.wait_ge(tok[0], tok[1])
        ins = fn()
        if toks:
            ins._wait_ge(toks[-1][0], toks[-1][1])
        for tok in toks:
            e.waited[id(tok[0])] = tok[1]
        e.cnt += 1
        ins.then_inc(e.sem, 1)
        me = (e.sem, e.cnt)
        for r in reads:
            r.r[id(e.sem)] = me
        for w in writes:
            w.w = me
            w.r = {}
        return ins

    def dma(self, out, in_, reads=(), writes=(), en="sp"):
        e = self.engs[en]
        i = self.ring_n % 16
        self.ring_n += 1
        sem = self.ring[i]
        prev = (sem, self.ring_val[i]) if self.ring_val[i] > 0 else None
        self.ring_val[i] += 16
        toks = self._collect(e, reads, writes, extra=(prev,) if prev else ())
        for tok in toks:
            e.h.wait_ge(tok[0], tok[1])
            e.waited[id(tok[0])] = tok[1]
        e.h.dma_start(out=out, in_=in_).then_inc(sem, 16)
        me = (sem, self.ring_val[i])
        for r in reads:
            r.r[id(sem)] = me
        for w in writes:
            w.w = me
            w.r = {}

    def all_tokens(self):
        toks = [(e.sem, e.cnt) for e in self.engs.values() if e.cnt > 0]
        toks += [(self.ring[i], self.ring_val[i]) for i in range(16) if self.ring_val[i] > 0]
        return toks

    def barrier(self, names=("pe", "act", "dve", "pool", "sp")):
        toks = self.all_tokens()
        for n in names:
            e = self.engs[n]
            for sem, val in toks:
                if sem is e.sem:
                    continue
                if e.waited.get(id(sem), 0) >= val:
                    continue
                e.h.wait_ge(sem, val)
                e.waited[id(sem)] = val


class Carver:
    def __init__(self, arena, base, limit):
        self.a = arena
        self.off = base
        self.limit = limit

    def take(self, shape, dt):
        n = 1
        for s_ in shape:
            n *= s_
        nbytes = n * (4 if dt is F32 else 2)
        start = self.off
        self.off += (nbytes + 63) // 64 * 64
        assert self.off <= self.limit, ("SBUF carve overflow", self.off, self.limit)
        ap = self.a[:, start // 2: start // 2 + nbytes // 2]
        if dt is F32:
            ap = ap.bitcast(F32)
        if len(shape) == 2:
            ap = ap.rearrange("p (a b) -> p a b", a=shape[0])
        elif len(shape) == 3:
            ap = ap.rearrange("p (a b c) -> p a b c", a=shape[0], b=shape[1])
        return ap

    def buf(self, shape, dt):
        return Buf(self.take(shape, dt))


ARENA_BYTES = 212000
R_BASE = 65536 + 32768 + 8192


def build_program(nseq, sublayers, do_prep=True):
    nc = bass.Bass("TRN2", target_bir_lowering=False)
    Res.registry = []
    tr = Tr(nc)
    op = tr.op

    def reset_sync():
        tr.barrier()
        nc.all_engine_barrier()
        for e in tr.engs.values():
            e.h.sem_clear(e.sem)
        for sem in tr.ring:
            nc.sync.sem_clear(sem)
        nc.all_engine_barrier()
        for e in tr.engs.values():
            e.cnt = 0
            e.waited = {}
        tr.ring_n = 0
        tr.ring_val = [0] * 16
        for r in Res.registry:
            r.w = None
            r.r = {}

    def din(name, shape, dt=F32):
        return nc.dram_tensor(name, list(shape), dt, kind="ExternalInput").ap()

    x_d = din("x", [nseq, S, D])
    out_d = nc.dram_tensor("out", [nseq, S, D], F32, kind="ExternalOutput").ap()
    used = set(sublayers)
    ret_w_in = {j: din("ret_w_in_%d" % j, [D, 6144]) for j in range(2) if ("ret", j) in used}
    ret_w_out = {j: din("ret_w_out_%d" % j, [2048, D]) for j in range(2) if ("ret", j) in used}
    sb_w_in = {j: din("sb_w_in_%d" % j, [D, 3072]) for j in range(2) if ("sb", j) in used}
    sb_w_out = {j: din("sb_w_out_%d" % j, [D, D]) for j in range(2) if ("sb", j) in used}
    ffn_w_up = {l: din("ffn_w_up_%d" % l, [D, 2 * DFF]) for l in range(4) if ("ffn", l) in used}
    ffn_w_down = {l: din("ffn_w_down_%d" % l, [DFF, D]) for l in range(4) if ("ffn", l) in used}
    c_gains = din("c_gains", [128, 9 * 8])
    c_cw = din("c_cw", [128, 4 * 44 * 3])
    c_cb = din("c_cb", [128, 4 * 44])
    c_identb = din("c_identb", [128, 128], BF16)
    c_identf = din("c_identf", [128, 128])
    c_tri = din("c_tri", [128, 128], BF16)
    c_mask = din("c_mask", [128, 128], BF16)
    c_dm = din("c_dm", [128, 4 * 128])
    c_kdecs = din("c_kdecs", [128, 4])
    c_epsv = din("c_epsv", [128, 4])
    c_cos = din("c_cos", [128, S])
    c_sin = din("c_sin", [128, S])

    def dscr(name, shape):
        return nc.dram_tensor(name, list(shape), BF16).ap()

    s_rq = [dscr("s_rq%d" % j, [4, 128, 2048]) for j in range(2)]
    s_rk = [dscr("s_rk%d" % j, [4, 128, 2048]) for j in range(2)]
    s_rv = [dscr("s_rv%d" % j, [4, 128, 4096]) for j in range(2)]
    s_rg = [dscr("s_rg%d" % j, [4, 128, 4096]) for j in range(2)]
    s_ro = [dscr("s_ro%d" % j, [4, 128, 4096]) for j in range(2)]
    s_sw = [dscr("s_sw%d" % j, [8, 128, 3072]) for j in range(2)]
    s_so = [dscr("s_so%d" % j, [8, 64, 2048]) for j in range(2)]
    s_fu = [dscr("s_fu%d" % l, [22, 128, 2048]) for l in range(4)]
    s_fd = [dscr("s_fd%d" % l, [128, 22 * 1024]) for l in range(4)]

    arena = nc.alloc_sbuf_tensor("arena", [128, ARENA_BYTES // 2], BF16)
    pc = Carver(arena, 0, R_BASE)
    xT = pc.take([NCH, S], F32)
    hT = pc.take([NCH, S], BF16)
    xr = [[Res() for _ in range(NTB)] for _ in range(NCH)]
    hr = [[Res() for _ in range(NTB)] for _ in range(NCH)]
    identb = pc.buf([128], BF16)
    identf = pc.buf([128], F32)
    onesb = pc.buf([128], BF16)
    tri = pc.buf([128], BF16)
    maskb = pc.buf([128], BF16)
    dm = pc.buf([4, 128], F32)
    kdecs = pc.buf([4], F32)
    epsv = pc.buf([4], F32)
    gains = pc.buf([72], F32)
    g32 = pc.buf([72], F32)
    cw = pc.buf([4 * 44 * 3], F32)
    cb = pc.buf([4 * 44], F32)
    epsc = pc.buf([8], F32)

    PS = nc.alloc_psum_tensor("ps", [128, 8, 512], F32)
    psr = [Res() for _ in range(8)]
    pstate = {"i": 0, "allowed": list(range(8))}

    def psalloc():
        a = pstate["allowed"]
        pstate["i"] = (pstate["i"] + 1) % len(a)
        return a[pstate["i"]]

    def psb(b):
        return PS[:, b, :]

    def psb16(b):
        return PS[:, b, :].bitcast(BF16)

    for b_, src in ((identb, c_identb), (identf, c_identf), (tri, c_tri), (maskb, c_mask),
                    (kdecs, c_kdecs), (epsv, c_epsv), (gains, c_gains), (cw, c_cw), (cb, c_cb)):
        tr.dma(out=b_.ap, in_=src, writes=[b_.res])
    tr.dma(out=dm.ap, in_=c_dm.rearrange("p (h c) -> p h c", h=4), writes=[dm.res])
    op("dve", lambda: nc.vector.memset(onesb.ap, 1.0), writes=[onesb.res])
    op("dve", lambda: nc.vector.memset(epsc.ap, 1024.0 * EPS), writes=[epsc.res])
    op("dve", lambda: nc.vector.tensor_scalar(out=g32.ap, in0=gains.ap, scalar1=32.0, scalar2=None,
                                               op0=ALU.mult), reads=[gains.res], writes=[g32.res])

    cast_rr = {"i": 0}

    def cast(out, in_, reads, writes):
        k = cast_rr["i"] % 3
        cast_rr["i"] += 1
        if k == 0:
            op("act", lambda: nc.scalar.copy(out=out, in_=in_), reads=reads, writes=writes)
        else:
            op("dve", lambda: nc.vector.tensor_copy(out, in_), reads=reads, writes=writes)

    used = set(sublayers)
    if do_prep:
        rc = Carver(arena, R_BASE, ARENA_BYTES)
        stg = [rc.buf([4096], F32) for _ in range(3)]
        bst = [rc.buf([4096], BF16) for _ in range(3)]
        pi = {"i": 0}

        def piece(loads, n_el, store_fn, cast_views=None, parts=128):
            k = pi["i"] % 3
            pi["i"] += 1
            sb_, bb_ = stg[k], bst[k]
            for (dst_fn, src) in loads:
                tr.dma(out=dst_fn(sb_.ap), in_=src, writes=[sb_.res])
            if cast_views is None:
                cast(bb_.ap[:parts, :n_el], sb_.ap[:parts, :n_el], [sb_.res], [bb_.res])
            else:
                o_, i_ = cast_views(bb_.ap, sb_.ap)
                cast(o_, i_, [sb_.res], [bb_.res])
            store_fn(bb_)

        for j in range(2):
            if ("ret", j) in used:
                w = ret_w_in[j].rearrange("(c p) n -> p c n", p=128)
                for h in range(4):
                    for (scr, c0, ns) in ((s_rq[j], h * 256, 256), (s_rk[j], 1024 + h * 256, 256),
                                          (s_rv[j], 2048 + h * 512, 512), (s_rg[j], 4096 + h * 512, 512)):
                        ne = 8 * ns
                        piece([(lambda a, ns=ns, ne=ne: a[:, :ne].rearrange("p (c n) -> p c n", c=8),
                                w[:, :, c0:c0 + ns])], ne,
                              lambda bb, scr=scr, h=h, ne=ne: tr.dma(out=scr[h], in_=bb.ap[:, :ne], reads=[bb.res]))
                    wo = ret_w_out[j][h * 512:(h + 1) * 512, :].rearrange("(e p) n -> p e n", p=128)
                    piece([(lambda a: a.rearrange("p (e n) -> p e n", e=4), wo)], 4096,
                          lambda bb, j=j, h=h: tr.dma(out=s_ro[j][h], in_=bb.ap, reads=[bb.res]))
            if ("sb", j) in used:
                w = sb_w_in[j].rearrange("(c p) n -> p c n", p=128)
                for p_ in range(8):
                    loads = []
                    for t3 in range(3):
                        loads.append((lambda a, t3=t3: a[:, :3072].rearrange("p (c t n) -> p c t n", c=8, t=3)[:, :, t3, :],
                                      w[:, :, t3 * 1024 + p_ * 128: t3 * 1024 + (p_ + 1) * 128]))
                    piece(loads, 3072,
                          lambda bb, j=j, p_=p_: tr.dma(out=s_sw[j][p_], in_=bb.ap[:, :3072], reads=[bb.res]))
                    wo = sb_w_out[j][p_ * 128:(p_ + 1) * 128, :].rearrange("(hh d) n -> d hh n", d=64)
                    piece([(lambda a: a[:64, :2048].rearrange("p (hh n) -> p hh n", hh=2), wo)], 2048,
                          lambda bb, j=j, p_=p_: tr.dma(out=s_so[j][p_], in_=bb.ap[:64, :2048], reads=[bb.res]),
                          parts=64)
        for l in range(4):
            if ("ffn", l) in used:
                w = ffn_w_up[l].rearrange("(c p) n -> p c n", p=128)
                for g in range(11):
                    loads = []
                    for gv in range(2):
                        loads.append((lambda a, gv=gv: a.rearrange("p (c gv n) -> p c gv n", c=8, gv=2)[:, :, gv, :],
                                      w[:, :, gv * DFF + g * 256: gv * DFF + (g + 1) * 256]))

                    def cviews(bb, sb_):
                        o_ = bb.rearrange("p (pr cg n) -> p pr cg n", pr=2, n=128)
                        i_ = sb_.rearrange("p (cg pr n) -> p pr cg n", pr=2, n=128)
                        return o_, i_

                    def store(bb, l=l, g=g):
                        tr.dma(out=s_fu[l][2 * g:2 * g + 2].rearrange("s p n -> p s n"),
                               in_=bb.ap.rearrange("p (s n) -> p s n", s=2), reads=[bb.res])

                    piece(loads, 4096, store, cast_views=cviews)
                wd = ffn_w_down[l].rearrange("(k p) n -> p k n", p=128)
                for q in range(6):
                    k0 = q * 4
                    nk = min(4, 22 - k0)
                    ne = nk * 1024
                    piece([(lambda a, nk=nk, ne=ne: a[:, :ne].rearrange("p (k n) -> p k n", k=nk), wd[:, k0:k0 + nk, :])], ne,
                          lambda bb, l=l, k0=k0, ne=ne: tr.dma(out=s_fd[l][:, k0 * 1024:k0 * 1024 + ne], in_=bb.ap[:, :ne],
                                                               reads=[bb.res]))
        tr.barrier()

    def tbs(tb):
        return slice(tb * 512, (tb + 1) * 512)

    def norm_block(tb, gi, sqs, rstd, dst_fn, dst_res):
        bank = psalloc()
        for c in range(NCH):
            sqb = sqs[c % len(sqs)]
            op("act", lambda: nc.scalar.activation(out=sqb.ap, in_=xT[:, c, tbs(tb)], func=AF.Square),
               reads=[xr[c][tb]], writes=[sqb.res])
            op("pe", lambda: nc.tensor.matmul(psb(bank), lhsT=onesb.ap, rhs=sqb.ap, start=(c == 0), stop=(c == NCH - 1)),
               reads=[sqb.res, onesb.res], writes=[psr[bank]])
        op("act", lambda: nc.scalar.activation(out=rstd.ap, in_=psb(bank), func=AF.Ln, bias=epsc.ap[:, 0:1]),
           reads=[epsc.res], writes=[psr[bank], rstd.res])
        op("act", lambda: nc.scalar.activation(out=rstd.ap, in_=rstd.ap, func=AF.Exp, scale=-0.5),
           reads=[rstd.res], writes=[rstd.res])
        for c in range(NCH):
            en = "dve"
            eh = nc.vector
            op(en, lambda: eh.scalar_tensor_tensor(out=dst_fn(c), in0=xT[:, c, tbs(tb)],
                                                   scalar=g32.ap[:, gi * 8 + c: gi * 8 + c + 1], in1=rstd.ap,
                                                   op0=ALU.mult, op1=ALU.mult),
               reads=[xr[c][tb], rstd.res, g32.res], writes=[dst_res(c)])

    def resid_add(n, tb, bank):
        op("dve", lambda: nc.vector.tensor_tensor(out=xT[:, n, tbs(tb)], in0=psb(bank), in1=xT[:, n, tbs(tb)], op=ALU.add),
           reads=[xr[n][tb]], writes=[psr[bank], xr[n][tb]])

    def load_x(b):
        tr.barrier()
        rc = Carver(arena, R_BASE, ARENA_BYTES)
        st = [rc.buf([1024], F32) for _ in range(2)]
        pstate["allowed"] = list(range(8))
        for t in range(16):
            sb_ = st[t % 2]
            tr.dma(out=sb_.ap, in_=x_d[b, t * 128:(t + 1) * 128, :], writes=[sb_.res])
            for half in range(2):
                bank = psalloc()
                for cc in range(4):
                    c = half * 4 + cc
                    op("pe", lambda: nc.tensor.transpose(PS[:, bank, cc * 128:(cc + 1) * 128], sb_.ap[:, c * 128:(c + 1) * 128], identf.ap),
                       reads=[sb_.res, identf.res], writes=[psr[bank]])
                tb = t // 4
                dst = xT[:, half * 4:(half + 1) * 4, t * 128:(t + 1) * 128]
                src = PS[:, bank, :].rearrange("p (c n) -> p c n", c=4)
                wr = [psr[bank]] + [xr[half * 4 + cc][tb] for cc in range(4)]
                if half == 0:
                    op("act", lambda: nc.scalar.copy(out=dst, in_=src), writes=wr)
                else:
                    op("dve", lambda: nc.vector.tensor_copy(dst, src), writes=wr)

    def store_out(b):
        tr.barrier()
        rc = Carver(arena, R_BASE, ARENA_BYTES)
        sqs = [rc.buf([512], BF16) for _ in range(3)]
        rstd = rc.buf([512], F32)
        yn = rc.buf([NCH, 512], F32)
        ynr = [Res() for _ in range(NCH)]
        ost = [rc.buf([1024], F32) for _ in range(2)]
        pstate["allowed"] = list(range(8))
        for tb in range(NTB):
            norm_block(tb, 8, sqs, rstd, lambda c: yn.ap[:, c, :], lambda c: ynr[c])
            for tt in range(4):
                t = tb * 4 + tt
                ob = ost[t % 2]
                for half in range(2):
                    bank = psalloc()
                    for cc in range(4):
                        c = half * 4 + cc
                        op("pe", lambda: nc.tensor.transpose(PS[:, bank, cc * 128:(cc + 1) * 128], yn.ap[:, c, tt * 128:(tt + 1) * 128], identf.ap),
                           reads=[ynr[c], identf.res], writes=[psr[bank]])
                    if half == 0:
                        op("act", lambda: nc.scalar.copy(out=ob.ap[:, 0:512], in_=psb(bank)), writes=[psr[bank], ob.res])
                    else:
                        op("dve", lambda: nc.vector.tensor_copy(ob.ap[:, 512:1024], psb(bank)), writes=[psr[bank], ob.res])
                tr.dma(out=out_d[b, t * 128:(t + 1) * 128, :], in_=ob.ap, reads=[ob.res])

    def ffn(l):
        tr.barrier()
        rc = Carver(arena, R_BASE, ARENA_BYTES)
        wd = rc.buf([NFC, 1024], BF16)
        wups = [rc.buf([8, 2, 128], BF16) for _ in range(3)]
        mT = rc.buf([NFC, 512], BF16)
        sqs = [rc.buf([512], BF16) for _ in range(3)]
        rstd = rc.buf([512], F32)
        U = [[rc.buf([514], F32) for _ in range(2)] for _ in range(2)]
        ACC = [[rc.buf([512], F32) for _ in range(2)] for _ in range(2)]
        H = rc.buf([44, 2], F32)
        pstate["allowed"] = list(range(8))
        op("dve", lambda: nc.vector.memset(H.ap, 0.0), writes=[H.res])
        seq = [(tb, i) for tb in range(NTB) for i in range(NFC)]
        issued = {"n": 0}

        def ensure(upto):
            while issued["n"] <= min(upto, len(seq) - 1):
                n = issued["n"]
                tb_, i_ = seq[n]
                wb = wups[n % 3]
                tr.dma(out=wb.ap, in_=s_fu[l][i_].rearrange("p (c g n) -> p c g n", c=8, g=2), writes=[wb.res])
                if n == 1:
                    tr.dma(out=wd.ap, in_=s_fd[l].rearrange("p (k n) -> p k n", k=NFC), writes=[wd.res])
                issued["n"] += 1

        cwl = lambda i, gv, k: cw.ap[:, ((l * 44 + gv * 22 + i) * 3 + k):((l * 44 + gv * 22 + i) * 3 + k + 1)]
        cbl = lambda i, gv: cb.ap[:, (l * 44 + gv * 22 + i):(l * 44 + gv * 22 + i + 1)]
        n = 0
        norm_block(0, 4 + l, sqs, rstd, lambda c: hT[:, c, tbs(0)], lambda c: hr[c][0])
        for tb in range(NTB):
            for i in range(NFC):
                ensure(n + 2)
                wb = wups[n % 3]
                par = n % 2
                n += 1
                banks = []
                for gv in range(2):
                    bank = psalloc()
                    banks.append(bank)
                    for c in range(NCH):
                        op("pe", lambda: nc.tensor.matmul(psb(bank), lhsT=wb.ap[:, c, gv, :], rhs=hT[:, c, tbs(tb)],
                                                          start=(c == 0), stop=(c == NCH - 1)),
                           reads=[wb.res, hr[c][tb]], writes=[psr[bank]])
                for gv in range(2):
                    bank = banks[gv]
                    u = U[gv][par]
                    acc = ACC[gv][par]
                    hcol = H.ap[:, gv * 22 + i, :]
                    op("act", lambda: nc.scalar.copy(out=u.ap[:, 0:2], in_=hcol), reads=[H.res], writes=[u.res])
                    op("act", lambda: nc.scalar.copy(out=u.ap[:, 2:514], in_=psb(bank)), writes=[psr[bank], u.res])
                    op("act", lambda: nc.scalar.copy(out=hcol, in_=u.ap[:, 512:514]), reads=[u.res], writes=[H.res])
                    e1 = "pool"
                    e2 = "dve"
                    h1 = nc.gpsimd
                    h2 = nc.vector
                    op(e1, lambda: h1.tensor_scalar(out=acc.ap, in0=u.ap[:, 2:514], scalar1=cwl(i, gv, 2), scalar2=cbl(i, gv),
                                                    op0=ALU.mult, op1=ALU.add),
                       reads=[u.res, cw.res, cb.res], writes=[acc.res])
                    op(e2, lambda: h2.scalar_tensor_tensor(out=acc.ap, in0=u.ap[:, 1:513], scalar=cwl(i, gv, 1), in1=acc.ap,
                                                           op0=ALU.mult, op1=ALU.add),
                       reads=[u.res, acc.res, cw.res], writes=[acc.res])
                    op(e2, lambda: h2.scalar_tensor_tensor(out=acc.ap, in0=u.ap[:, 0:512], scalar=cwl(i, gv, 0), in1=acc.ap,
                                                           op0=ALU.mult, op1=ALU.add),
                       reads=[u.res, acc.res, cw.res], writes=[acc.res])
                ag, av = ACC[0][par], ACC[1][par]
                op("act", lambda: nc.scalar.activation(out=ag.ap, in_=ag.ap, func=AF.Silu), reads=[ag.res], writes=[ag.res])
                op("dve", lambda: nc.vector.tensor_tensor(out=mT.ap[:, i, :], in0=ag.ap, in1=av.ap, op=ALU.mult),
                   reads=[ag.res, av.res], writes=[mT.res])
            if tb + 1 < NTB:
                norm_block(tb + 1, 4 + l, sqs, rstd, lambda c: hT[:, c, tbs(tb + 1)], lambda c: hr[c][tb + 1])
            for nn in range(NCH):
                bank = psalloc()
                for k in range(NFC):
                    op("pe", lambda: nc.tensor.matmul(psb(bank), lhsT=wd.ap[:, k, nn * 128:(nn + 1) * 128], rhs=mT.ap[:, k, :],
                                                      start=(k == 0), stop=(k == NFC - 1)),
                       reads=[wd.res, mT.res], writes=[psr[bank]])
                resid_add(nn, tb, bank)

    def retention(j):
        tr.barrier()
        rc = Carver(arena, R_BASE, ARENA_BYTES)
        Wq = [rc.buf([8, 256], BF16) for _ in range(2)]
        Wk = [rc.buf([8, 256], BF16) for _ in range(2)]
        Wv = rc.buf([8, 512], BF16)
        Wg = rc.buf([8, 512], BF16)
        Wo = rc.buf([4, 1024], BF16)
        qT = rc.buf([2, S], BF16)
        kT = rc.buf([2, S], BF16)
        yT = [rc.buf([4, 512], BF16) for _ in range(2)]
        cs = [[rc.buf([512], F32) for _ in range(2)] for _ in range(2)]
        tmp = [rc.buf([512], F32) for _ in range(4)]
        vc = [rc.buf([512], BF16) for _ in range(2)]
        sgc = [rc.buf([512], BF16) for _ in range(2)]
        sT = [rc.buf([128], BF16) for _ in range(2)]
        kdc = [rc.buf([256], BF16) for _ in range(2)]
        S32 = rc.buf([2, 512], F32)
        Sbf = [rc.buf([2, 512], BF16) for _ in range(2)]
        yc = [rc.buf([512], BF16) for _ in range(2)]
        junk = rc.buf([512], BF16)
        egb = rc.buf([512], F32)
        st = rc.buf([8], F32)
        sqs = [rc.buf([512], BF16) for _ in range(2)]
        rstd = rc.buf([512], F32)
        pstate["allowed"] = list(range(7))
        gam = [1.0 - 2.0 ** (-5.0 - h) for h in range(4)]

        def filler(n=1):
            for _ in range(n * RFILL):
                nc.tensor.matmul(PS[:, 7, 0:FILLN], lhsT=onesb.ap, rhs=hT[:, 0, 0:FILLN], start=True, stop=True, skip_group_check=True)

        def load_qk(h):
            tr.dma(out=Wq[h % 2].ap, in_=s_rq[j][h].rearrange("p (c n) -> p c n", c=8), writes=[Wq[h % 2].res])
            tr.dma(out=Wk[h % 2].ap, in_=s_rk[j][h].rearrange("p (c n) -> p c n", c=8), writes=[Wk[h % 2].res])

        load_qk(0)
        for tb in range(NTB):
            norm_block(tb, j, sqs, rstd, lambda c: hT[:, c, tbs(tb)], lambda c: hr[c][tb])
        csn = 0
        for h in range(4):
            tr.dma(out=Wv.ap, in_=s_rv[j][h].rearrange("p (c n) -> p c n", c=8), writes=[Wv.res])
            tr.dma(out=Wg.ap, in_=s_rg[j][h].rearrange("p (c n) -> p c n", c=8), writes=[Wg.res])
            tr.dma(out=Wo.ap, in_=s_ro[j][h].rearrange("p (e n) -> p e n", e=4), writes=[Wo.res])
            if h + 1 < 4:
                load_qk(h + 1)
            for tb in range(NTB):
                cb_, sb_ = cs[csn % 2]
                csn += 1
                tr.dma(out=cb_.ap, in_=c_cos[:, tbs(tb)], writes=[cb_.res])
                tr.dma(out=sb_.ap, in_=c_sin[:, tbs(tb)], writes=[sb_.res])
                for (W, dst) in ((Wq[h % 2], qT), (Wk[h % 2], kT)):
                    b1, b2 = psalloc(), psalloc()
                    for (bank, half) in ((b1, 0), (b2, 1)):
                        for c in range(NCH):
                            op("pe", lambda: nc.tensor.matmul(psb(bank), lhsT=W.ap[:, c, half * 128:(half + 1) * 128],
                                                              rhs=hT[:, c, tbs(tb)], start=(c == 0), stop=(c == NCH - 1)),
                               reads=[W.res, hr[c][tb]], writes=[psr[bank]])
                    t1, t2, t3, t4 = tmp
                    op("dve", lambda: nc.vector.tensor_tensor(out=t1.ap, in0=psb(b1), in1=cb_.ap, op=ALU.mult),
                       reads=[cb_.res], writes=[psr[b1], t1.res])
                    op("dve", lambda: nc.vector.tensor_tensor(out=t3.ap, in0=psb(b1), in1=sb_.ap, op=ALU.mult),
                       reads=[sb_.res], writes=[psr[b1], t3.res])
                    op("dve", lambda: nc.vector.tensor_tensor(out=t2.ap, in0=psb(b2), in1=sb_.ap, op=ALU.mult),
                       reads=[sb_.res], writes=[psr[b2], t2.res])
                    op("dve", lambda: nc.vector.tensor_tensor(out=t4.ap, in0=psb(b2), in1=cb_.ap, op=ALU.mult),
                       reads=[cb_.res], writes=[psr[b2], t4.res])
                    op("pool", lambda: nc.gpsimd.tensor_tensor(out=dst.ap[:, 0, tbs(tb)], in0=t1.ap, in1=t2.ap, op=ALU.subtract),
                       reads=[t1.res, t2.res], writes=[dst.res])
                    op("pool", lambda: nc.gpsimd.tensor_tensor(out=dst.ap[:, 1, tbs(tb)], in0=t3.ap, in1=t4.ap, op=ALU.add),
                       reads=[t3.res, t4.res], writes=[dst.res])
            def emit_A(c):
                tb = c // 4
                ts_ = slice(c * 128, (c + 1) * 128)
                v_, sg_ = vc[c % 2], sgc[c % 2]
                bv, bg = psalloc(), psalloc()
                for kc in range(NCH):
                    op("pe", lambda: nc.tensor.matmul(psb(bv), lhsT=hT[:, kc, ts_], rhs=Wv.ap[:, kc, :], start=(kc == 0), stop=(kc == 7)),
                       reads=[hr[kc][tb], Wv.res], writes=[psr[bv]])
                for kc in range(NCH):
                    op("pe", lambda: nc.tensor.matmul(psb(bg), lhsT=hT[:, kc, ts_], rhs=Wg.ap[:, kc, :], start=(kc == 0), stop=(kc == 7)),
                       reads=[hr[kc][tb], Wg.res], writes=[psr[bg]])
                op("act", lambda: nc.scalar.copy(out=v_.ap, in_=psb(bv)), writes=[psr[bv], v_.res])
                op("act", lambda: nc.scalar.activation(out=egb.ap, in_=psb(bg), func=AF.Exp, scale=-1.0), writes=[psr[bg], egb.res])
                op("act", lambda: nc.scalar.activation(out=egb.ap, in_=egb.ap, func=AF.Ln, bias=1.0), reads=[egb.res], writes=[egb.res])
                op("act", lambda: nc.scalar.activation(out=egb.ap, in_=egb.ap, func=AF.Exp, scale=-1.0), reads=[egb.res], writes=[egb.res])
                op("dve", lambda: nc.vector.tensor_tensor(out=sg_.ap, in0=psb(bg), in1=egb.ap, op=ALU.mult),
                   reads=[egb.res], writes=[psr[bg], sg_.res])

            emit_A(0)
            for c in range(16):
                tb = c // 4
                ts_ = slice(c * 128, (c + 1) * 128)
                v_, sg_, sT_, kd_, y_ = vc[c % 2], sgc[c % 2], sT[c % 2], kdc[c % 2], yc[c % 2]
                bs = psalloc()
                for dc in range(2):
                    op("pe", lambda: nc.tensor.matmul(PS[:, bs, 0:128], lhsT=kT.ap[:, dc, ts_], rhs=qT.ap[:, dc, ts_],
                                                      start=(dc == 0), stop=(dc == 1)),
                       reads=[kT.res, qT.res], writes=[psr[bs]])
                filler(2)
                op("dve", lambda: nc.vector.tensor_tensor(out=sT_.ap, in0=PS[:, bs, 0:128], in1=dm.ap[:, h, :], op=ALU.mult),
                   reads=[dm.res], writes=[psr[bs], sT_.res])
                bo = psalloc()
                sb_prev = Sbf[(c + 1) % 2]
                op("pe", lambda: nc.tensor.matmul(psb(bo), lhsT=sT_.ap, rhs=v_.ap, start=True, stop=(c == 0)),
                   reads=[sT_.res, v_.res], writes=[psr[bo]])
                if c > 0:
                    for dc in range(2):
                        op("pe", lambda: nc.tensor.matmul(psb(bo), lhsT=qT.ap[:, dc, ts_], rhs=sb_prev.ap[:, dc, :],
                                                          start=False, stop=(dc == 1)),
                           reads=[qT.res, sb_prev.res], writes=[psr[bo]])
                filler(2)
                if c < 15:
                    bt = psalloc()
                    for dc in range(2):
                        op("pe", lambda: nc.tensor.transpose(psb16(bt)[:, dc * 128:(dc + 1) * 128], kT.ap[:, dc, ts_], identb.ap),
                           reads=[kT.res, identb.res], writes=[psr[bt]])
                    op("act", lambda: nc.scalar.activation(out=kd_.ap, in_=psb16(bt)[:, 0:256], func=AF.Copy,
                                                           scale=kdecs.ap[:, h:h + 1]),
                       reads=[kdecs.res], writes=[psr[bt], kd_.res])
                    emit_A(c + 1)
                    sb_new = Sbf[c % 2]
                    for dc in range(2):
                        bu = psalloc()
                        op("pe", lambda: nc.tensor.matmul(psb(bu), lhsT=kd_.ap[:, dc * 128:(dc + 1) * 128], rhs=v_.ap, start=True, stop=True),
                           reads=[kd_.res, v_.res], writes=[psr[bu]])
                        if c == 0:
                            op("dve", lambda: nc.vector.tensor_copy(S32.ap[:, dc, :], psb(bu)), writes=[psr[bu], S32.res])
                        else:
                            op("dve", lambda: nc.vector.scalar_tensor_tensor(out=S32.ap[:, dc, :], in0=S32.ap[:, dc, :],
                                                                             scalar=float(gam[h] ** 128), in1=psb(bu),
                                                                             op0=ALU.mult, op1=ALU.add),
                               reads=[S32.res], writes=[psr[bu], S32.res])
                    op("pool", lambda: nc.gpsimd.tensor_copy(sb_new.ap, S32.ap), reads=[S32.res], writes=[sb_new.res])
                filler(2)
                op("act", lambda: nc.scalar.activation(out=junk.ap, in_=psb(bo), func=AF.Square, accum_out=st.ap[:, 0:1]),
                   writes=[psr[bo], junk.res, st.res])
                op("act", lambda: nc.scalar.activation(out=st.ap[:, 1:2], in_=st.ap[:, 0:1], func=AF.Ln, scale=1.0 / 512.0,
                                                       bias=epsv.ap[:, h:h + 1]),
                   reads=[st.res, epsv.res], writes=[st.res])
                op("act", lambda: nc.scalar.activation(out=st.ap[:, 2:3], in_=st.ap[:, 1:2], func=AF.Exp, scale=-0.5),
                   reads=[st.res], writes=[st.res])
                op("dve", lambda: nc.vector.scalar_tensor_tensor(out=y_.ap, in0=psb(bo), scalar=st.ap[:, 2:3], in1=sg_.ap,
                                                                 op0=ALU.mult, op1=ALU.mult),
                   reads=[st.res, sg_.res], writes=[psr[bo], y_.res])
                by = psalloc()
                for e in range(4):
                    op("pe", lambda: nc.tensor.transpose(psb16(by)[:, e * 128:(e + 1) * 128], y_.ap[:, e * 128:(e + 1) * 128], identb.ap),
                       reads=[y_.res, identb.res], writes=[psr[by]])
                filler(2)
                yt = yT[tb % 2]
                op("act", lambda: nc.scalar.copy(out=yt.ap[:, :, (c % 4) * 128:(c % 4 + 1) * 128],
                                                 in_=psb16(by)[:, 0:512].rearrange("p (e n) -> p e n", e=4)),
                   writes=[psr[by], yt.res])
                if c % 4 == 3:
                    for nn in range(NCH):
                        bank = psalloc()
                        for e in range(4):
                            op("pe", lambda: nc.tensor.matmul(psb(bank), lhsT=Wo.ap[:, e, nn * 128:(nn + 1) * 128], rhs=yt.ap[:, e, :],
                                                              start=(e == 0), stop=(e == 3)),
                               reads=[Wo.res, yt.res], writes=[psr[bank]])
                        resid_add(nn, tb, bank)

    def stick(j):
        tr.barrier()
        rc = Carver(arena, R_BASE, ARENA_BYTES)
        Wp = [rc.buf([8, 3, 128], BF16) for _ in range(2)]
        Wo = [rc.buf([2, 1024], BF16) for _ in range(2)]
        qsT = rc.buf([S], BF16)
        kT = rc.buf([S], BF16)
        vp = rc.buf([16, 128], BF16)
        oT = rc.buf([2, S], BF16)
        ebuf = [rc.buf([512], F32) for _ in range(4)]
        ecb = [rc.buf([512], F32) for _ in range(2)]
        spb = [rc.buf([512], BF16) for _ in range(4)]
        Rb = [rc.buf([512], BF16) for _ in range(2)]
        aTb = [rc.buf([512], BF16) for _ in range(4)]
        sqs = [rc.buf([512], BF16) for _ in range(3)]
        rstd = rc.buf([512], F32)
        pstate["allowed"] = list(range(8))

        def filler(n=1):
            for _ in range(n):
                nc.tensor.matmul(PS[:, 5, 0:FILLN], lhsT=onesb.ap, rhs=hT[:, 0, 0:FILLN], start=True, stop=True, skip_group_check=True)

        def load_w(p_):
            tr.dma(out=Wp[p_ % 2].ap, in_=s_sw[j][p_].rearrange("p (c t n) -> p c t n", c=8, t=3), writes=[Wp[p_ % 2].res])
            tr.dma(out=Wo[p_ % 2].ap[:64], in_=s_so[j][p_].rearrange("p (h n) -> p h n", h=2), writes=[Wo[p_ % 2].res])

        load_w(0)
        for tb in range(NTB):
            norm_block(tb, 2 + j, sqs, rstd, lambda c: hT[:, c, tbs(tb)], lambda c: hr[c][tb])
        cnt = {"e": 0, "sp": 0, "a": 0}
        for p_ in range(8):
            if p_ + 1 < 8:
                load_w(p_ + 1)
            W = Wp[p_ % 2]
            wo = Wo[p_ % 2]
            pstate["allowed"] = list(range(8))
            for tb in range(NTB):
                bq, bk = psalloc(), psalloc()
                for (bank, t3) in ((bq, 0), (bk, 1)):
                    for c in range(NCH):
                        op("pe", lambda: nc.tensor.matmul(psb(bank), lhsT=W.ap[:, c, t3, :], rhs=hT[:, c, tbs(tb)],
                                                          start=(c == 0), stop=(c == NCH - 1)),
                           reads=[W.res, hr[c][tb]], writes=[psr[bank]])
                op("act", lambda: nc.scalar.activation(out=qsT.ap[:, tbs(tb)], in_=psb(bq), func=AF.Copy, scale=0.125),
                   writes=[psr[bq], qsT.res])
                op("act", lambda: nc.scalar.copy(out=kT.ap[:, tbs(tb)], in_=psb(bk)), writes=[psr[bk], kT.res])
            for t4 in range(4):
                bank = psalloc()
                for tt in range(4):
                    t = t4 * 4 + tt
                    for c in range(NCH):
                        op("pe", lambda: nc.tensor.matmul(PS[:, bank, tt * 128:(tt + 1) * 128], lhsT=hT[:, c, t * 128:(t + 1) * 128],
                                                          rhs=W.ap[:, c, 2, :], start=(c == 0), stop=(c == NCH - 1)),
                           reads=[W.res, hr[c][t // 4]], writes=[psr[bank]])
                op("act", lambda: nc.scalar.copy(out=vp.ap[:, t4 * 4:(t4 + 1) * 4, :],
                                                 in_=PS[:, bank, :].rearrange("p (t n) -> p t n", t=4)),
                   writes=[psr[bank], vp.res])
            pstate["allowed"] = list(range(5))
            op("pe", lambda: nc.tensor.matmul(PS[:, 5, 0:FILLN], lhsT=onesb.ap, rhs=hT[:, 0, 0:FILLN], start=True, stop=True,
                                              skip_group_check=True),
               reads=[onesb.res], writes=[psr[5]])
            for qg in range(4):
                q0 = qg * 512
                for hh in range(2):
                    op("pool", lambda: nc.gpsimd.memset(Rb[hh].ap, 0.0), writes=[Rb[hh].res])
                nsteps = 4 * qg + 4
                zb = {}

                def emit_z(step, hh):
                    kb = 4 * qg + 3 - step
                    c0 = max(0, kb - 4 * qg) * 128
                    bank = psalloc()
                    ps_ = slice(hh * 64, hh * 64 + 64)
                    op("pe", lambda: nc.tensor.matmul(PS[:, bank, c0:512], lhsT=kT.ap[ps_, kb * 128:(kb + 1) * 128],
                                                      rhs=qsT.ap[ps_, q0 + c0:q0 + 512], start=True, stop=True),
                       reads=[kT.res, qsT.res], writes=[psr[bank]])
                    filler(FILL)
                    zb[(step, hh)] = bank

                for hh in range(2):
                    emit_z(0, hh)
                for step in range(nsteps):
                    kb = 4 * qg + 3 - step
                    diag = kb >= 4 * qg
                    c0 = max(0, kb - 4 * qg) * 128
                    N = 512 - c0
                    sps, ats, cbs, ebs = [], [], [], []
                    for hh in range(2):
                        bank = zb.pop((step, hh))
                        eb = ebuf[cnt["e"] % 4]
                        cnt["e"] += 1
                        ebs.append(eb)
                        sp_ = spb[cnt["sp"] % 4]
                        cnt["sp"] += 1
                        op("act", lambda: nc.scalar.activation(out=eb.ap[:, c0:512], in_=PS[:, bank, c0:512], func=AF.Exp),
                           writes=[psr[bank], eb.res])
                        op("act", lambda: nc.scalar.activation(out=sp_.ap[:, c0:512], in_=eb.ap[:, c0:512], func=AF.Ln, bias=1.0),
                           reads=[eb.res], writes=[sp_.res])
                        if diag:
                            op("dve", lambda: nc.vector.tensor_tensor(out=sp_.ap[:, c0:c0 + 128], in0=sp_.ap[:, c0:c0 + 128],
                                                                      in1=maskb.ap, op=ALU.mult),
                               reads=[sp_.res, maskb.res], writes=[sp_.res])
                        sps.append(sp_)
                    for hh in range(2):
                        sp_ = sps[hh]
                        bank = psalloc()
                        cbs.append(bank)
                        ps_ = slice(hh * 64, hh * 64 + 64)
                        op("pe", lambda: nc.tensor.matmul(PS[:, bank, c0:512], lhsT=tri.ap, rhs=sp_.ap[:, c0:512], start=True, stop=(step == 0)),
                           reads=[tri.res, sp_.res], writes=[psr[bank]])
                        filler(FILL)
                        if step > 0:
                            op("pe", lambda: nc.tensor.matmul(PS[:, bank, c0:512], lhsT=onesb.ap, rhs=Rb[hh].ap[:, c0:512],
                                                              start=False, stop=True),
                               reads=[onesb.res, Rb[hh].res], writes=[psr[bank]])
                            filler(FILL)
                    if step + 1 < nsteps:
                        for hh in range(2):
                            emit_z(step + 1, hh)
                    for hh in range(2):
                        sp_ = sps[hh]
                        bank = cbs[hh]
                        at = aTb[cnt["a"] % 4]
                        cnt["a"] += 1
                        ec = ecb[hh]
                        eb = ebs[hh]
                        op("act", lambda: nc.scalar.activation(out=ec.ap[:, c0:512], in_=PS[:, bank, c0:512], func=AF.Exp, scale=-1.0),
                           writes=[psr[bank], ec.res])
                        op("dve", lambda: nc.vector.tensor_tensor(out=at.ap[:, c0:512], in0=eb.ap[:, c0:512], in1=ec.ap[:, c0:512], op=ALU.mult),
                           reads=[eb.res, ec.res], writes=[at.res])
                        if diag:
                            op("dve", lambda: nc.vector.tensor_tensor(out=at.ap[:, c0:c0 + 128], in0=at.ap[:, c0:c0 + 128],
                                                                      in1=maskb.ap, op=ALU.mult),
                               reads=[at.res, maskb.res], writes=[at.res])
                        if step + 1 < nsteps:
                            op("pool", lambda: nc.gpsimd.tensor_tensor(out=Rb[hh].ap[:, c0:512], in0=Rb[hh].ap[:, c0:512],
                                                                       in1=sp_.ap[:, c0:512], op=ALU.add),
                               reads=[Rb[hh].res, sp_.res], writes=[Rb[hh].res])
                        ab = 6 + hh
                        op("pe", lambda: nc.tensor.matmul(PS[0:64, ab, c0:512], lhsT=vp.ap[:, kb, hh * 64:(hh + 1) * 64],
                                                          rhs=at.ap[:, c0:512], start=(step == 0), stop=(step == nsteps - 1),
                                                          skip_group_check=True),
                           reads=[vp.res, at.res], writes=[psr[ab]])
                        filler(FILL)
                for hh in range(2):
                    ab = 6 + hh
                    op("act", lambda: nc.scalar.copy(out=oT.ap[0:64, hh, q0:q0 + 512], in_=PS[0:64, ab, :]),
                       writes=[psr[ab], oT.res])
            pstate["allowed"] = list(range(6))
            for tb in range(NTB):
                for nn in range(NCH):
                    bank = psalloc()
                    for hh in range(2):
                        op("pe", lambda: nc.tensor.matmul(psb(bank), lhsT=wo.ap[0:64, hh, nn * 128:(nn + 1) * 128],
                                                          rhs=oT.ap[0:64, hh, tbs(tb)], start=(hh == 0), stop=(hh == 1)),
                           reads=[wo.res, oT.res], writes=[psr[bank]])
                    resid_add(nn, tb, bank)

    def body(b):
        load_x(b)
        for sl in sublayers:
            if sl[0] == "ret":
                retention(sl[1])
            elif sl[0] == "sb":
                stick(sl[1])
            else:
                ffn(sl[1])
        store_out(b)

    reset_sync()
    if nseq == 1:
        body(0)
        reset_sync()
    else:
        with nc.Fori(0, nseq, hint_back_edge=True) as b:
            body(b)
            reset_sync()
    return nc


def make_consts():
    bf = ml_dtypes.bfloat16
    c = {}
    c["c_identb"] = np.eye(128, dtype=np.float32).astype(bf)
    c["c_identf"] = np.eye(128, dtype=np.float32)
    jj = np.arange(128)
    c["c_tri"] = (jj[:, None] >= jj[None, :]).astype(np.float32).astype(bf)
    c["c_mask"] = (jj[:, None] < jj[None, :]).astype(np.float32).astype(bf)
    dm = np.zeros((128, 4, 128), np.float64)
    kd = np.zeros((128, 4), np.float64)
    ev = np.zeros((128, 4), np.float64)
    for h in range(4):
        g = 1.0 - 2.0 ** (-5.0 - h)
        k = jj.astype(np.float64)
        dm[:, h, :] = np.where(jj[None, :] >= jj[:, None], (g ** (-(k + 1.0)))[:, None] / 16.0, 0.0)
        kd[:, h] = g ** (127.0 - k) / 16.0
        ev[:, h] = EPS * g ** (-2.0 * (k + 1.0))
    c["c_dm"] = dm.reshape(128, 512).astype(np.float32)
    c["c_kdecs"] = kd.astype(np.float32)
    c["c_epsv"] = ev.astype(np.float32)
    pos = np.arange(S, dtype=np.float32)
    inv = (np.float32(10000.0) ** (-np.arange(0, 256, 2, dtype=np.float32) / np.float32(256))).astype(np.float32)
    ang = (pos[:, None] * inv[None, :]).astype(np.float32)
    c["c_cos"] = np.ascontiguousarray(np.cos(ang).T.astype(np.float32))
    c["c_sin"] = np.ascontiguousarray(np.sin(ang).T.astype(np.float32))
    return c


def layout_params(inp):
    f = lambda a: np.asarray(a, dtype=np.float32)
    gl = [f(inp["ret_norm"])[0], f(inp["ret_norm"])[1], f(inp["sb_norm"])[0], f(inp["sb_norm"])[1]]
    gl += [f(inp["ffn_norm"])[l] for l in range(4)] + [f(inp["final_norm"])]
    gains = np.stack([g.reshape(8, 128).T for g in gl], axis=1).reshape(128, 72)
    cwv = f(inp["ffn_conv_w"])
    cw = cwv.reshape(4, 3, 44, 128).transpose(3, 0, 2, 1).reshape(128, 4 * 44 * 3)
    cbv = f(inp["ffn_conv_b"])
    cb = cbv.reshape(4, 44, 128).transpose(2, 0, 1).reshape(128, 4 * 44)
    return {"c_gains": np.ascontiguousarray(gains), "c_cw": np.ascontiguousarray(cw), "c_cb": np.ascontiguousarray(cb)}


FULL = [("ret", 0), ("ffn", 0), ("sb", 0), ("ffn", 1), ("ret", 1), ("ffn", 2), ("sb", 1), ("ffn", 3)]


def run(inputs, nseq, sublayers, xs_per_core, trace=False):
    nc = build_program(nseq, sublayers)
    common = {}
    common.update(make_consts())
    common.update(layout_params(inputs))
    for sl in set(sublayers):
        names = {"ret": ("ret_w_in", "ret_w_out"), "sb": ("sb_w_in", "sb_w_out"), "ffn": ("ffn_w_up", "ffn_w_down")}[sl[0]]
        for k in names:
            common["%s_%d" % (k, sl[1])] = np.ascontiguousarray(np.asarray(inputs[k][sl[1]], dtype=np.float32))
    in_maps = []
    for xs in xs_per_core:
        m = dict(common)
        m["x"] = np.ascontiguousarray(xs)
        in_maps.append(m)
    res = run_bass_kernel_spmd(nc, in_maps, core_ids=list(range(len(xs_per_core))), trace=trace)
    return res


def kernel(**inputs):
    x = np.asarray(inputs["x"], dtype=np.float32)
    B = x.shape[0]
    per = B // NCORES
    xs = [x[i * per:(i + 1) * per] for i in range(NCORES)]
    res = run(inputs, per, FULL, xs)
    out = np.concatenate([np.asarray(r["out"], dtype=np.float32) for r in res.results], axis=0)
    return out
```

```python
import math
import numpy as np
import ml_dtypes
import concourse.bass as bass
import concourse.mybir as mybir
from concourse.bass_utils import run_bass_kernel_spmd

F32 = mybir.dt.float32
BF16 = mybir.dt.bfloat16
AF = mybir.ActivationFunctionType
ALU = mybir.AluOpType

D = 1024
S = 2048
NCH = 8
NTB = 4
DFF = 2816
NFC = 22
EPS = 1e-6
NCORES = 8
import os
FILL = int(os.environ.get('SB_FILL', '1'))
FILLN = int(os.environ.get('SB_FILLN', '256'))
RFILL = int(os.environ.get('RET_FILL', '0'))


class Res:
    __slots__ = ("w", "r")
    registry = []

    def __init__(self):
        self.w = None
        self.r = {}
        Res.registry.append(self)


class Buf:
    __slots__ = ("ap", "res")

    def __init__(self, ap):
        self.ap = ap
        self.res = Res()


class Eng:
    def __init__(self, name, h, sem, raw):
        self.name = name
        self.h = h
        self.sem = sem
        self.cnt = 0
        self.waited = {}
        self.raw = raw


class Tr:
    def __init__(self, nc):
        self.nc = nc
        self.engs = {}
        for name, h, raw in (("pe", nc.tensor, False), ("act", nc.scalar, True),
                             ("dve", nc.vector, True), ("pool", nc.gpsimd, True),
                             ("sp", nc.sync, False)):
            self.engs[name] = Eng(name, h, nc.alloc_semaphore("sem_" + name), raw)
        self.ring = [nc.alloc_semaphore("dring%d" % i) for i in range(16)]
        self.ring_n = 0
        self.ring_val = [0] * 16

    def _collect(self, e, reads, writes, extra=()):
        deps = {}

        def add(tok, same_ok):
            if tok is None:
                return
            sem, val = tok
            if sem is e.sem and not same_ok:
                return
            k = id(sem)
            if e.waited.get(k, 0) >= val:
                return
            if k not in deps or deps[k][1] < val:
                deps[k] = tok

        for r in reads:
            add(r.w, e.raw)
        for w in writes:
            add(w.w, False)
            for tok in w.r.values():
                add(tok, False)
        for tok in extra:
            add(tok, False)
        return list(deps.values())

    def op(self, en, fn, reads=(), writes=()):
        e = self.engs[en]
        toks = self._collect(e, reads, writes)
        for tok in toks[:-1]:
            e.h.wait_ge(tok[0], tok[1])
        ins = fn()
        if toks:
            ins._wait_ge(toks[-1][0], toks[-1][1])
        for tok in toks:
            e.waited[id(tok[0])] = tok[1]
        e.cnt += 1
        ins.then_inc(e.sem, 1)
        me = (e.sem, e.cnt)
        for r in reads:
            r.r[id(e.sem)] = me
        for w in writes:
            w.w = me
            w.r = {}
        return ins

    def dma(self, out, in_, reads=(), writes=(), en="sp"):
        e = self.engs[en]
        i = self.ring_n % 16
        self.ring_n += 1
        sem = self.ring[i]
        prev = (sem, self.ring_val[i]) if self.ring_val[i] > 0 else None
        self.ring_val[i] += 16
        toks = self._collect(e, reads, writes, extra=(prev,) if prev else ())
        for tok in toks:
            e.h.wait_ge(tok[0], tok[1])
            e.waited[id(tok[0])] = tok[1]
        e.h.dma_start(out=out, in_=in_).then_inc(sem, 16)
        me = (sem, self.ring_val[i])
        for r in reads:
            r.r[id(sem)] = me
        for w in writes:
            w.w = me
            w.r = {}

    def all_tokens(self):
        toks = [(e.sem, e.cnt) for e in self.engs.values() if e.cnt > 0]
        toks += [(self.ring[i], self.ring_val[i]) for i in range(16) if self.ring_val[i] > 0]
        return toks

    def barrier(self, names=("pe", "act", "dve", "pool", "sp")):
        toks = self.all_tokens()
        for n in names:
            e = self.engs[n]
            for sem, val in toks:
                if sem is e.sem:
                    continue
                if e.waited.get(id(sem), 0) >= val:
                    continue
                e.h.wait_ge(sem, val)
                e.waited[id(sem)] = val


class Carver:
    def __init__(self, arena, base, limit):
        self.a = arena
        self.off = base
        self.limit = limit

    def take(self, shape, dt):
        n = 1
        for s_ in shape:
            n *= s_
        nbytes = n * (4 if dt is F32 else 2)
        start = self.off
        self.off += (nbytes + 63) // 64 * 64
        assert self.off <= self.limit, ("SBUF carve overflow", self.off, self.limit)
        ap = self.a[:, start // 2: start // 2 + nbytes // 2]
        if dt is F32:
            ap = ap.bitcast(F32)
        if len(shape) == 2:
            ap = ap.rearrange("p (a b) -> p a b", a=shape[0])
        elif len(shape) == 3:
            ap = ap.rearrange("p (a b c) -> p a b c", a=shape[0], b=shape[1])
        return ap

    def buf(self, shape, dt):
        return Buf(self.take(shape, dt))


ARENA_BYTES = 212000
R_BASE = 65536 + 32768 + 8192


def build_program(nseq, sublayers, do_prep=True):
    nc = bass.Bass("TRN2", target_bir_lowering=False)
    Res.registry = []
    tr = Tr(nc)
    op = tr.op

    def reset_sync():
        tr.barrier()
        nc.all_engine_barrier()
        for e in tr.engs.values():
            e.h.sem_clear(e.sem)
        for sem in tr.ring:
            nc.sync.sem_clear(sem)
        nc.all_engine_barrier()
        for e in tr.engs.values():
            e.cnt = 0
            e.waited = {}
        tr.ring_n = 0
        tr.ring_val = [0] * 16
        for r in Res.registry:
            r.w = None
            r.r = {}

    def din(name, shape, dt=F32):
        return nc.dram_tensor(name, list(shape), dt, kind="ExternalInput").ap()

    x_d = din("x", [nseq, S, D])
    out_d = nc.dram_tensor("out", [nseq, S, D], F32, kind="ExternalOutput").ap()
    used = set(sublayers)
    ret_w_in = {j: din("ret_w_in_%d" % j, [D, 6144]) for j in range(2) if ("ret", j) in used}
    ret_w_out = {j: din("ret_w_out_%d" % j, [2048, D]) for j in range(2) if ("ret", j) in used}
    sb_w_in = {j: din("sb_w_in_%d" % j, [D, 3072]) for j in range(2) if ("sb", j) in used}
    sb_w_out = {j: din("sb_w_out_%d" % j, [D, D]) for j in range(2) if ("sb", j) in used}
    ffn_w_up = {l: din("ffn_w_up_%d" % l, [D, 2 * DFF]) for l in range(4) if ("ffn", l) in used}
    ffn_w_down = {l: din("ffn_w_down_%d" % l, [DFF, D]) for l in range(4) if ("ffn", l) in used}
    c_gains = din("c_gains", [128, 9 * 8])
    c_cw = din("c_cw", [128, 4 * 44 * 3])
    c_cb = din("c_cb", [128, 4 * 44])
    c_identb = din("c_identb", [128, 128], BF16)
    c_identf = din("c_identf", [128, 128])
    c_tri = din("c_tri", [128, 128], BF16)
    c_mask = din("c_mask", [128, 128], BF16)
    c_dm = din("c_dm", [128, 4 * 128])
    c_kdecs = din("c_kdecs", [128, 4])
    c_epsv = din("c_epsv", [128, 4])
    c_cos = din("c_cos", [128, S])
    c_sin = din("c_sin", [128, S])

    def dscr(name, shape):
        return nc.dram_tensor(name, list(shape), BF16).ap()

    s_rq = [dscr("s_rq%d" % j, [4, 128, 2048]) for j in range(2)]
    s_rk = [dscr("s_rk%d" % j, [4, 128, 2048]) for j in range(2)]
    s_rv = [dscr("s_rv%d" % j, [4, 128, 4096]) for j in range(2)]
    s_rg = [dscr("s_rg%d" % j, [4, 128, 4096]) for j in range(2)]
    s_ro = [dscr("s_ro%d" % j, [4, 128, 4096]) for j in range(2)]
    s_sw = [dscr("s_sw%d" % j, [8, 128, 3072]) for j in range(2)]
    s_so = [dscr("s_so%d" % j, [8, 64, 2048]) for j in range(2)]
    s_fu = [dscr("s_fu%d" % l, [22, 128, 2048]) for l in range(4)]
    s_fd = [dscr("s_fd%d" % l, [128, 22 * 1024]) for l in range(4)]

    arena = nc.alloc_sbuf_tensor("arena", [128, ARENA_BYTES // 2], BF16)
    pc = Carver(arena, 0, R_BASE)
    xT = pc.take([NCH, S], F32)
    hT = pc.take([NCH, S], BF16)
    xr = [[Res() for _ in range(NTB)] for _ in range(NCH)]
    hr = [[Res() for _ in range(NTB)] for _ in range(NCH)]
    identb = pc.buf([128], BF16)
    identf = pc.buf([128], F32)
    onesb = pc.buf([128], BF16)
    tri = pc.buf([128], BF16)
    maskb = pc.buf([128], BF16)
    dm = pc.buf([4, 128], F32)
    kdecs = pc.buf([4], F32)
    epsv = pc.buf([4], F32)
    gains = pc.buf([72], F32)
    g32 = pc.buf([72], F32)
    cw = pc.buf([4 * 44 * 3], F32)
    cb = pc.buf([4 * 44], F32)
    epsc = pc.buf([8], F32)

    PS = nc.alloc_psum_tensor("ps", [128, 8, 512], F32)
    psr = [Res() for _ in range(8)]
    pstate = {"i": 0, "allowed": list(range(8))}

    def psalloc():
        a = pstate["allowed"]
        pstate["i"] = (pstate["i"] + 1) % len(a)
        return a[pstate["i"]]

    def psb(b):
        return PS[:, b, :]

    def psb16(b):
        return PS[:, b, :].bitcast(BF16)

    for b_, src in ((identb, c_identb), (identf, c_identf), (tri, c_tri), (maskb, c_mask),
                    (kdecs, c_kdecs), (epsv, c_epsv), (gains, c_gains), (cw, c_cw), (cb, c_cb)):
        tr.dma(out=b_.ap, in_=src, writes=[b_.res])
    tr.dma(out=dm.ap, in_=c_dm.rearrange("p (h c) -> p h c", h=4), writes=[dm.res])
    op("dve", lambda: nc.vector.memset(onesb.ap, 1.0), writes=[onesb.res])
    op("dve", lambda: nc.vector.memset(epsc.ap, 1024.0 * EPS), writes=[epsc.res])
    op("dve", lambda: nc.vector.tensor_scalar(out=g32.ap, in0=gains.ap, scalar1=32.0, scalar2=None,
                                               op0=ALU.mult), reads=[gains.res], writes=[g32.res])

    cast_rr = {"i": 0}

    def cast(out, in_, reads, writes):
        k = cast_rr["i"] % 3
        cast_rr["i"] += 1
        if k == 0:
            op("act", lambda: nc.scalar.copy(out=out, in_=in_), reads=reads, writes=writes)
        else:
            op("dve", lambda: nc.vector.tensor_copy(out, in_), reads=reads, writes=writes)

    used = set(sublayers)
    if do_prep:
        rc = Carver(arena, R_BASE, ARENA_BYTES)
        stg = [rc.buf([4096], F32) for _ in range(3)]
        bst = [rc.buf([4096], BF16) for _ in range(3)]
        pi = {"i": 0}

        def piece(loads, n_el, store_fn, cast_views=None, parts=128):
            k = pi["i"] % 3
            pi["i"] += 1
            sb_, bb_ = stg[k], bst[k]
            for (dst_fn, src) in loads:
                tr.dma(out=dst_fn(sb_.ap), in_=src, writes=[sb_.res])
            if cast_views is None:
                cast(bb_.ap[:parts, :n_el], sb_.ap[:parts, :n_el], [sb_.res], [bb_.res])
            else:
                o_, i_ = cast_views(bb_.ap, sb_.ap)
                cast(o_, i_, [sb_.res], [bb_.res])
            store_fn(bb_)

        for j in range(2):
            if ("ret", j) in used:
                w = ret_w_in[j].rearrange("(c p) n -> p c n", p=128)
                for h in range(4):
                    for (scr, c0, ns) in ((s_rq[j], h * 256, 256), (s_rk[j], 1024 + h * 256, 256),
                                          (s_rv[j], 2048 + h * 512, 512), (s_rg[j], 4096 + h * 512, 512)):
                        ne = 8 * ns
                        piece([(lambda a, ns=ns, ne=ne: a[:, :ne].rearrange("p (c n) -> p c n", c=8),
                                w[:, :, c0:c0 + ns])], ne,
                              lambda bb, scr=scr, h=h, ne=ne: tr.dma(out=scr[h], in_=bb.ap[:, :ne], reads=[bb.res]))
                    wo = ret_w_out[j][h * 512:(h + 1) * 512, :].rearrange("(e p) n -> p e n", p=128)
                    piece([(lambda a: a.rearrange("p (e n) -> p e n", e=4), wo)], 4096,
                          lambda bb, j=j, h=h: tr.dma(out=s_ro[j][h], in_=bb.ap, reads=[bb.res]))
            if ("sb", j) in used:
                w = sb_w_in[j].rearrange("(c p) n -> p c n", p=128)
                for p_ in range(8):
                    loads = []
                    for t3 in range(3):
                        loads.append((lambda a, t3=t3: a[:, :3072].rearrange("p (c t n) -> p c t n", c=8, t=3)[:, :, t3, :],
                                      w[:, :, t3 * 1024 + p_ * 128: t3 * 1024 + (p_ + 1) * 128]))
                    piece(loads, 3072,
                          lambda bb, j=j, p_=p_: tr.dma(out=s_sw[j][p_], in_=bb.ap[:, :3072], reads=[bb.res]))
                    wo = sb_w_out[j][p_ * 128:(p_ + 1) * 128, :].rearrange("(hh d) n -> d hh n", d=64)
                    piece([(lambda a: a[:64, :2048].rearrange("p (hh n) -> p hh n", hh=2), wo)], 2048,
                          lambda bb, j=j, p_=p_: tr.dma(out=s_so[j][p_], in_=bb.ap[:64, :2048], reads=[bb.res]),
                          parts=64)
        for l in range(4):
            if ("ffn", l) in used:
                w = ffn_w_up[l].rearrange("(c p) n -> p c n", p=128)
                for g in range(11):
                    loads = []
                    for gv in range(2):
                        loads.append((lambda a, gv=gv: a.rearrange("p (c gv n) -> p c gv n", c=8, gv=2)[:, :, gv, :],
                                      w[:, :, gv * DFF + g * 256: gv * DFF + (g + 1) * 256]))

                    def cviews(bb, sb_):
                        o_ = bb.rearrange("p (pr cg n) -> p pr cg n", pr=2, n=128)
                        i_ = sb_.rearrange("p (cg pr n) -> p pr cg n", pr=2, n=128)
                        return o_, i_

                    def store(bb, l=l, g=g):
                        tr.dma(out=s_fu[l][2 * g:2 * g + 2].rearrange("s p n -> p s n"),
                               in_=bb.ap.rearrange("p (s n) -> p s n", s=2), reads=[bb.res])

                    piece(loads, 4096, store, cast_views=cviews)
                wd = ffn_w_down[l].rearrange("(k p) n -> p k n", p=128)
                for q in range(6):
                    k0 = q * 4
                    nk = min(4, 22 - k0)
                    ne = nk * 1024
                    piece([(lambda a, nk=nk, ne=ne: a[:, :ne].rearrange("p (k n) -> p k n", k=nk), wd[:, k0:k0 + nk, :])], ne,
                          lambda bb, l=l, k0=k0, ne=ne: tr.dma(out=s_fd[l][:, k0 * 1024:k0 * 1024 + ne], in_=bb.ap[:, :ne],
                                                               reads=[bb.res]))
        tr.barrier()

    def tbs(tb):
        return slice(tb * 512, (tb + 1) * 512)

    def norm_block(tb, gi, sqs, rstd, dst_fn, dst_res):
        bank = psalloc()
        for c in range(NCH):
            sqb = sqs[c % len(sqs)]
            op("act", lambda: nc.scalar.activation(out=sqb.ap, in_=xT[:, c, tbs(tb)], func=AF.Square),
               reads=[xr[c][tb]], writes=[sqb.res])
            op("pe", lambda: nc.tensor.matmul(psb(bank), lhsT=onesb.ap, rhs=sqb.ap, start=(c == 0), stop=(c == NCH - 1)),
               reads=[sqb.res, onesb.res], writes=[psr[bank]])
        op("act", lambda: nc.scalar.activation(out=rstd.ap, in_=psb(bank), func=AF.Ln, bias=epsc.ap[:, 0:1]),
           reads=[epsc.res], writes=[psr[bank], rstd.res])
        op("act", lambda: nc.scalar.activation(out=rstd.ap, in_=rstd.ap, func=AF.Exp, scale=-0.5),
           reads=[rstd.res], writes=[rstd.res])
        for c in range(NCH):
            en = "dve"
            eh = nc.vector
            op(en, lambda: eh.scalar_tensor_tensor(out=dst_fn(c), in0=xT[:, c, tbs(tb)],
                                                   scalar=g32.ap[:, gi * 8 + c: gi * 8 + c + 1], in1=rstd.ap,
                                                   op0=ALU.mult, op1=ALU.mult),
               reads=[xr[c][tb], rstd.res, g32.res], writes=[dst_res(c)])

    def resid_add(n, tb, bank):
        op("dve", lambda: nc.vector.tensor_tensor(out=xT[:, n, tbs(tb)], in0=psb(bank), in1=xT[:, n, tbs(tb)], op=ALU.add),
           reads=[xr[n][tb]], writes=[psr[bank], xr[n][tb]])

    def load_x(b):
        tr.barrier()
        rc = Carver(arena, R_BASE, ARENA_BYTES)
        st = [rc.buf([1024], F32) for _ in range(2)]
        pstate["allowed"] = list(range(8))
        for t in range(16):
            sb_ = st[t % 2]
            tr.dma(out=sb_.ap, in_=x_d[b, t * 128:(t + 1) * 128, :], writes=[sb_.res])
            for half in range(2):
                bank = psalloc()
                for cc in range(4):
                    c = half * 4 + cc
                    op("pe", lambda: nc.tensor.transpose(PS[:, bank, cc * 128:(cc + 1) * 128], sb_.ap[:, c * 128:(c + 1) * 128], identf.ap),
                       reads=[sb_.res, identf.res], writes=[psr[bank]])
                tb = t // 4
                dst = xT[:, half * 4:(half + 1) * 4, t * 128:(t + 1) * 128]
                src = PS[:, bank, :].rearrange("p (c n) -> p c n", c=4)
                wr = [psr[bank]] + [xr[half * 4 + cc][tb] for cc in range(4)]
                if half == 0:
                    op("act", lambda: nc.scalar.copy(out=dst, in_=src), writes=wr)
                else:
                    op("dve", lambda: nc.vector.tensor_copy(dst, src), writes=wr)

    def store_out(b):
        tr.barrier()
        rc = Carver(arena, R_BASE, ARENA_BYTES)
        sqs = [rc.buf([512], BF16) for _ in range(3)]
        rstd = rc.buf([512], F32)
        yn = rc.buf([NCH, 512], F32)
        ynr = [Res() for _ in range(NCH)]
        ost = [rc.buf([1024], F32) for _ in range(2)]
        pstate["allowed"] = list(range(8))
        for tb in range(NTB):
            norm_block(tb, 8, sqs, rstd, lambda c: yn.ap[:, c, :], lambda c: ynr[c])
            for tt in range(4):
                t = tb * 4 + tt
                ob = ost[t % 2]
                for half in range(2):
                    bank = psalloc()
                    for cc in range(4):
                        c = half * 4 + cc
                        op("pe", lambda: nc.tensor.transpose(PS[:, bank, cc * 128:(cc + 1) * 128], yn.ap[:, c, tt * 128:(tt + 1) * 128], identf.ap),
                           reads=[ynr[c], identf.res], writes=[psr[bank]])
                    if half == 0:
                        op("act", lambda: nc.scalar.copy(out=ob.ap[:, 0:512], in_=psb(bank)), writes=[psr[bank], ob.res])
                    else:
                        op("dve", lambda: nc.vector.tensor_copy(ob.ap[:, 512:1024], psb(bank)), writes=[psr[bank], ob.res])
                tr.dma(out=out_d[b, t * 128:(t + 1) * 128, :], in_=ob.ap, reads=[ob.res])

    def ffn(l):
        tr.barrier()
        rc = Carver(arena, R_BASE, ARENA_BYTES)
        wd = rc.buf([NFC, 1024], BF16)
        wups = [rc.buf([8, 2, 128], BF16) for _ in range(3)]
        mT = rc.buf([NFC, 512], BF16)
        sqs = [rc.buf([512], BF16) for _ in range(3)]
        rstd = rc.buf([512], F32)
        U = [[rc.buf([514], F32) for _ in range(2)] for _ in range(2)]
        ACC = [[rc.buf([512], F32) for _ in range(2)] for _ in range(2)]
        H = rc.buf([44, 2], F32)
        pstate["allowed"] = list(range(8))
        op("dve", lambda: nc.vector.memset(H.ap, 0.0), writes=[H.res])
        seq = [(tb, i) for tb in range(NTB) for i in range(NFC)]
        issued = {"n": 0}

        def ensure(upto):
            while issued["n"] <= min(upto, len(seq) - 1):
                n = issued["n"]
                tb_, i_ = seq[n]
                wb = wups[n % 3]
                tr.dma(out=wb.ap, in_=s_fu[l][i_].rearrange("p (c g n) -> p c g n", c=8, g=2), writes=[wb.res])
                if n == 1:
                    tr.dma(out=wd.ap, in_=s_fd[l].rearrange("p (k n) -> p k n", k=NFC), writes=[wd.res])
                issued["n"] += 1

        cwl = lambda i, gv, k: cw.ap[:, ((l * 44 + gv * 22 + i) * 3 + k):((l * 44 + gv * 22 + i) * 3 + k + 1)]
        cbl = lambda i, gv: cb.ap[:, (l * 44 + gv * 22 + i):(l * 44 + gv * 22 + i + 1)]
        n = 0
        norm_block(0, 4 + l, sqs, rstd, lambda c: hT[:, c, tbs(0)], lambda c: hr[c][0])
        for tb in range(NTB):
            for i in range(NFC):
                ensure(n + 2)
                wb = wups[n % 3]
                par = n % 2
                n += 1
                banks = []
                for gv in range(2):
                    bank = psalloc()
                    banks.append(bank)
                    for c in range(NCH):
                        op("pe", lambda: nc.tensor.matmul(psb(bank), lhsT=wb.ap[:, c, gv, :], rhs=hT[:, c, tbs(tb)],
                                                          start=(c == 0), stop=(c == NCH - 1)),
                           reads=[wb.res, hr[c][tb]], writes=[psr[bank]])
                for gv in range(2):
                    bank = banks[gv]
                    u = U[gv][par]
                    acc = ACC[gv][par]
                    hcol = H.ap[:, gv * 22 + i, :]
                    op("act", lambda: nc.scalar.copy(out=u.ap[:, 0:2], in_=hcol), reads=[H.res], writes=[u.res])
                    op("act", lambda: nc.scalar.copy(out=u.ap[:, 2:514], in_=psb(bank)), writes=[psr[bank], u.res])
                    op("act", lambda: nc.scalar.copy(out=hcol, in_=u.ap[:, 512:514]), reads=[u.res], writes=[H.res])
                    e2 = "dve"
                    h2 = nc.vector
                    op("act", lambda: nc.scalar.activation(out=acc.ap, in_=psb(bank), func=AF.Identity,
                                                           scale=cwl(i, gv, 2), bias=cbl(i, gv)),
                       reads=[cw.res, cb.res], writes=[psr[bank], acc.res])
                    op(e2, lambda: h2.scalar_tensor_tensor(out=acc.ap, in0=u.ap[:, 1:513], scalar=cwl(i, gv, 1), in1=acc.ap,
                                                           op0=ALU.mult, op1=ALU.add),
                       reads=[u.res, acc.res, cw.res], writes=[acc.res])
                    op(e2, lambda: h2.scalar_tensor_tensor(out=acc.ap, in0=u.ap[:, 0:512], scalar=cwl(i, gv, 0), in1=acc.ap,
                                                           op0=ALU.mult, op1=ALU.add),
                       reads=[u.res, acc.res, cw.res], writes=[acc.res])
                ag, av = ACC[0][par], ACC[1][par]
                op("act", lambda: nc.scalar.activation(out=ag.ap, in_=ag.ap, func=AF.Silu), reads=[ag.res], writes=[ag.res])
                op("pool", lambda: nc.gpsimd.tensor_tensor(out=mT.ap[:, i, :], in0=ag.ap, in1=av.ap, op=ALU.mult),
                   reads=[ag.res, av.res], writes=[mT.res])
            if tb + 1 < NTB:
                norm_block(tb + 1, 4 + l, sqs, rstd, lambda c: hT[:, c, tbs(tb + 1)], lambda c: hr[c][tb + 1])
            for nn in range(NCH):
                bank = psalloc()
                for k in range(NFC):
                    op("pe", lambda: nc.tensor.matmul(psb(bank), lhsT=wd.ap[:, k, nn * 128:(nn + 1) * 128], rhs=mT.ap[:, k, :],
                                                      start=(k == 0), stop=(k == NFC - 1)),
                       reads=[wd.res, mT.res], writes=[psr[bank]])
                resid_add(nn, tb, bank)

    def retention(j):
        tr.barrier()
        rc = Carver(arena, R_BASE, ARENA_BYTES)
        Wq = [rc.buf([8, 256], BF16) for _ in range(2)]
        Wk = [rc.buf([8, 256], BF16) for _ in range(2)]
        Wv = rc.buf([8, 512], BF16)
        Wg = rc.buf([8, 512], BF16)
        Wo = rc.buf([4, 1024], BF16)
        qT = rc.buf([2, S], BF16)
        kT = rc.buf([2, S], BF16)
        yT = [rc.buf([4, 512], BF16) for _ in range(2)]
        cs = [[rc.buf([512], F32) for _ in range(2)] for _ in range(2)]
        tmp = [rc.buf([512], F32) for _ in range(4)]
        vc = [rc.buf([512], BF16) for _ in range(2)]
        sgc = [rc.buf([512], BF16) for _ in range(2)]
        sT = [rc.buf([128], BF16) for _ in range(2)]
        kdc = [rc.buf([256], BF16) for _ in range(2)]
        S32 = rc.buf([2, 512], F32)
        Sbf = [rc.buf([2, 512], BF16) for _ in range(2)]
        yc = [rc.buf([512], BF16) for _ in range(2)]
        junk = rc.buf([512], BF16)
        egb = rc.buf([512], F32)
        st = rc.buf([8], F32)
        sqs = [rc.buf([512], BF16) for _ in range(2)]
        rstd = rc.buf([512], F32)
        pstate["allowed"] = list(range(7))
        gam = [1.0 - 2.0 ** (-5.0 - h) for h in range(4)]

        def filler(n=1):
            for _ in range(n * RFILL):
                nc.tensor.matmul(PS[:, 7, 0:FILLN], lhsT=onesb.ap, rhs=hT[:, 0, 0:FILLN], start=True, stop=True, skip_group_check=True)

        def load_qk(h):
            tr.dma(out=Wq[h % 2].ap, in_=s_rq[j][h].rearrange("p (c n) -> p c n", c=8), writes=[Wq[h % 2].res])
            tr.dma(out=Wk[h % 2].ap, in_=s_rk[j][h].rearrange("p (c n) -> p c n", c=8), writes=[Wk[h % 2].res])

        load_qk(0)
        for tb in range(NTB):
            norm_block(tb, j, sqs, rstd, lambda c: hT[:, c, tbs(tb)], lambda c: hr[c][tb])
        csn = 0
        for h in range(4):
            tr.dma(out=Wv.ap, in_=s_rv[j][h].rearrange("p (c n) -> p c n", c=8), writes=[Wv.res])
            tr.dma(out=Wg.ap, in_=s_rg[j][h].rearrange("p (c n) -> p c n", c=8), writes=[Wg.res])
            tr.dma(out=Wo.ap, in_=s_ro[j][h].rearrange("p (e n) -> p e n", e=4), writes=[Wo.res])
            if h + 1 < 4:
                load_qk(h + 1)
            for tb in range(NTB):
                cb_, sb_ = cs[csn % 2]
                csn += 1
                tr.dma(out=cb_.ap, in_=c_cos[:, tbs(tb)], writes=[cb_.res])
                tr.dma(out=sb_.ap, in_=c_sin[:, tbs(tb)], writes=[sb_.res])
                for (W, dst) in ((Wq[h % 2], qT), (Wk[h % 2], kT)):
                    b1, b2 = psalloc(), psalloc()
                    for (bank, half) in ((b1, 0), (b2, 1)):
                        for c in range(NCH):
                            op("pe", lambda: nc.tensor.matmul(psb(bank), lhsT=W.ap[:, c, half * 128:(half + 1) * 128],
                                                              rhs=hT[:, c, tbs(tb)], start=(c == 0), stop=(c == NCH - 1)),
                               reads=[W.res, hr[c][tb]], writes=[psr[bank]])
                    t1, t2, t3, t4 = tmp
                    op("dve", lambda: nc.vector.tensor_tensor(out=t1.ap, in0=psb(b1), in1=cb_.ap, op=ALU.mult),
                       reads=[cb_.res], writes=[psr[b1], t1.res])
                    op("dve", lambda: nc.vector.tensor_tensor(out=t3.ap, in0=psb(b1), in1=sb_.ap, op=ALU.mult),
                       reads=[sb_.res], writes=[psr[b1], t3.res])
                    op("dve", lambda: nc.vector.tensor_tensor(out=t2.ap, in0=psb(b2), in1=sb_.ap, op=ALU.mult),
                       reads=[sb_.res], writes=[psr[b2], t2.res])
                    op("dve", lambda: nc.vector.tensor_tensor(out=t4.ap, in0=psb(b2), in1=cb_.ap, op=ALU.mult),
                       reads=[cb_.res], writes=[psr[b2], t4.res])
                    op("pool", lambda: nc.gpsimd.tensor_tensor(out=dst.ap[:, 0, tbs(tb)], in0=t1.ap, in1=t2.ap, op=ALU.subtract),
                       reads=[t1.res, t2.res], writes=[dst.res])
                    op("pool", lambda: nc.gpsimd.tensor_tensor(out=dst.ap[:, 1, tbs(tb)], in0=t3.ap, in1=t4.ap, op=ALU.add),
                       reads=[t3.res, t4.res], writes=[dst.res])
            def emit_A(c):
                tb = c // 4
                ts_ = slice(c * 128, (c + 1) * 128)
                v_, sg_ = vc[c % 2], sgc[c % 2]
                bv, bg = psalloc(), psalloc()
                for kc in range(NCH):
                    op("pe", lambda: nc.tensor.matmul(psb(bv), lhsT=hT[:, kc, ts_], rhs=Wv.ap[:, kc, :], start=(kc == 0), stop=(kc == 7)),
                       reads=[hr[kc][tb], Wv.res], writes=[psr[bv]])
                for kc in range(NCH):
                    op("pe", lambda: nc.tensor.matmul(psb(bg), lhsT=hT[:, kc, ts_], rhs=Wg.ap[:, kc, :], start=(kc == 0), stop=(kc == 7)),
                       reads=[hr[kc][tb], Wg.res], writes=[psr[bg]])
                op("act", lambda: nc.scalar.copy(out=v_.ap, in_=psb(bv)), writes=[psr[bv], v_.res])
                op("act", lambda: nc.scalar.activation(out=egb.ap, in_=psb(bg), func=AF.Exp, scale=-1.0), writes=[psr[bg], egb.res])
                op("act", lambda: nc.scalar.activation(out=egb.ap, in_=egb.ap, func=AF.Ln, bias=1.0), reads=[egb.res], writes=[egb.res])
                op("act", lambda: nc.scalar.activation(out=egb.ap, in_=egb.ap, func=AF.Exp, scale=-1.0), reads=[egb.res], writes=[egb.res])
                op("dve", lambda: nc.vector.tensor_tensor(out=sg_.ap, in0=psb(bg), in1=egb.ap, op=ALU.mult),
                   reads=[egb.res], writes=[psr[bg], sg_.res])

            emit_A(0)
            for c in range(16):
                tb = c // 4
                ts_ = slice(c * 128, (c + 1) * 128)
                v_, sg_, sT_, kd_, y_ = vc[c % 2], sgc[c % 2], sT[c % 2], kdc[c % 2], yc[c % 2]
                bs = psalloc()
                for dc in range(2):
                    op("pe", lambda: nc.tensor.matmul(PS[:, bs, 0:128], lhsT=kT.ap[:, dc, ts_], rhs=qT.ap[:, dc, ts_],
                                                      start=(dc == 0), stop=(dc == 1)),
                       reads=[kT.res, qT.res], writes=[psr[bs]])
                filler(2)
                op("dve", lambda: nc.vector.tensor_tensor(out=sT_.ap, in0=PS[:, bs, 0:128], in1=dm.ap[:, h, :], op=ALU.mult),
                   reads=[dm.res], writes=[psr[bs], sT_.res])
                bo = psalloc()
                sb_prev = Sbf[(c + 1) % 2]
                op("pe", lambda: nc.tensor.matmul(psb(bo), lhsT=sT_.ap, rhs=v_.ap, start=True, stop=(c == 0)),
                   reads=[sT_.res, v_.res], writes=[psr[bo]])
                if c > 0:
                    for dc in range(2):
                        op("pe", lambda: nc.tensor.matmul(psb(bo), lhsT=qT.ap[:, dc, ts_], rhs=sb_prev.ap[:, dc, :],
                                                          start=False, stop=(dc == 1)),
                           reads=[qT.res, sb_prev.res], writes=[psr[bo]])
                filler(2)
                if c < 15:
                    bt = psalloc()
                    for dc in range(2):
                        op("pe", lambda: nc.tensor.transpose(psb16(bt)[:, dc * 128:(dc + 1) * 128], kT.ap[:, dc, ts_], identb.ap),
                           reads=[kT.res, identb.res], writes=[psr[bt]])
                    op("act", lambda: nc.scalar.activation(out=kd_.ap, in_=psb16(bt)[:, 0:256], func=AF.Copy,
                                                           scale=kdecs.ap[:, h:h + 1]),
                       reads=[kdecs.res], writes=[psr[bt], kd_.res])
                    emit_A(c + 1)
                    sb_new = Sbf[c % 2]
                    for dc in range(2):
                        bu = psalloc()
                        op("pe", lambda: nc.tensor.matmul(psb(bu), lhsT=kd_.ap[:, dc * 128:(dc + 1) * 128], rhs=v_.ap, start=True, stop=True),
                           reads=[kd_.res, v_.res], writes=[psr[bu]])
                        if c == 0:
                            op("dve", lambda: nc.vector.tensor_copy(S32.ap[:, dc, :], psb(bu)), writes=[psr[bu], S32.res])
                        else:
                            op("dve", lambda: nc.vector.scalar_tensor_tensor(out=S32.ap[:, dc, :], in0=S32.ap[:, dc, :],
                                                                             scalar=float(gam[h] ** 128), in1=psb(bu),
                                                                             op0=ALU.mult, op1=ALU.add),
                               reads=[S32.res], writes=[psr[bu], S32.res])
                    op("pool", lambda: nc.gpsimd.tensor_copy(sb_new.ap, S32.ap), reads=[S32.res], writes=[sb_new.res])
                filler(2)
                op("act", lambda: nc.scalar.activation(out=junk.ap, in_=psb(bo), func=AF.Square, accum_out=st.ap[:, 0:1]),
                   writes=[psr[bo], junk.res, st.res])
                op("act", lambda: nc.scalar.activation(out=st.ap[:, 1:2], in_=st.ap[:, 0:1], func=AF.Ln, scale=1.0 / 512.0,
                                                       bias=epsv.ap[:, h:h + 1]),
                   reads=[st.res, epsv.res], writes=[st.res])
                op("act", lambda: nc.scalar.activation(out=st.ap[:, 2:3], in_=st.ap[:, 1:2], func=AF.Exp, scale=-0.5),
                   reads=[st.res], writes=[st.res])
                op("dve", lambda: nc.vector.scalar_tensor_tensor(out=y_.ap, in0=psb(bo), scalar=st.ap[:, 2:3], in1=sg_.ap,
                                                                 op0=ALU.mult, op1=ALU.mult),
                   reads=[st.res, sg_.res], writes=[psr[bo], y_.res])
                by = psalloc()
                for e in range(4):
                    op("pe", lambda: nc.tensor.transpose(psb16(by)[:, e * 128:(e + 1) * 128], y_.ap[:, e * 128:(e + 1) * 128], identb.ap),
                       reads=[y_.res, identb.res], writes=[psr[by]])
                filler(2)
                yt = yT[tb % 2]
                op("act", lambda: nc.scalar.copy(out=yt.ap[:, :, (c % 4) * 128:(c % 4 + 1) * 128],
                                                 in_=psb16(by)[:, 0:512].rearrange("p (e n) -> p e n", e=4)),
                   writes=[psr[by], yt.res])
                if c % 4 == 3:
                    for nn in range(NCH):
                        bank = psalloc()
                        for e in range(4):
                            op("pe", lambda: nc.tensor.matmul(psb(bank), lhsT=Wo.ap[:, e, nn * 128:(nn + 1) * 128], rhs=yt.ap[:, e, :],
                                                              start=(e == 0), stop=(e == 3)),
                               reads=[Wo.res, yt.res], writes=[psr[bank]])
                        resid_add(nn, tb, bank)

    def stick(j):
        tr.barrier()
        rc = Carver(arena, R_BASE, ARENA_BYTES)
        Wp = [rc.buf([8, 3, 128], BF16) for _ in range(2)]
        Wo = [rc.buf([2, 1024], BF16) for _ in range(2)]
        qsT = rc.buf([S], BF16)
        kT = rc.buf([S], BF16)
        vp = rc.buf([16, 128], BF16)
        oT = rc.buf([2, S], BF16)
        ebuf = [rc.buf([512], F32) for _ in range(4)]
        ecb = [rc.buf([512], F32) for _ in range(2)]
        spb = [rc.buf([512], BF16) for _ in range(4)]
        Rb = [rc.buf([512], BF16) for _ in range(2)]
        aTb = [rc.buf([512], BF16) for _ in range(4)]
        sqs = [rc.buf([512], BF16) for _ in range(3)]
        rstd = rc.buf([512], F32)
        pstate["allowed"] = list(range(8))

        def filler(n=1):
            for _ in range(n):
                nc.tensor.matmul(PS[:, 5, 0:FILLN], lhsT=onesb.ap, rhs=hT[:, 0, 0:FILLN], start=True, stop=True, skip_group_check=True)

        def load_w(p_):
            tr.dma(out=Wp[p_ % 2].ap, in_=s_sw[j][p_].rearrange("p (c t n) -> p c t n", c=8, t=3), writes=[Wp[p_ % 2].res])
            tr.dma(out=Wo[p_ % 2].ap[:64], in_=s_so[j][p_].rearrange("p (h n) -> p h n", h=2), writes=[Wo[p_ % 2].res])

        load_w(0)
        for tb in range(NTB):
            norm_block(tb, 2 + j, sqs, rstd, lambda c: hT[:, c, tbs(tb)], lambda c: hr[c][tb])
        cnt = {"e": 0, "sp": 0, "a": 0}
        for p_ in range(8):
            if p_ + 1 < 8:
                load_w(p_ + 1)
            W = Wp[p_ % 2]
            wo = Wo[p_ % 2]
            pstate["allowed"] = list(range(8))
            for tb in range(NTB):
                bq, bk = psalloc(), psalloc()
                for (bank, t3) in ((bq, 0), (bk, 1)):
                    for c in range(NCH):
                        op("pe", lambda: nc.tensor.matmul(psb(bank), lhsT=W.ap[:, c, t3, :], rhs=hT[:, c, tbs(tb)],
                                                          start=(c == 0), stop=(c == NCH - 1)),
                           reads=[W.res, hr[c][tb]], writes=[psr[bank]])
                op("act", lambda: nc.scalar.activation(out=qsT.ap[:, tbs(tb)], in_=psb(bq), func=AF.Copy, scale=0.125),
                   writes=[psr[bq], qsT.res])
                op("act", lambda: nc.scalar.copy(out=kT.ap[:, tbs(tb)], in_=psb(bk)), writes=[psr[bk], kT.res])
            for t4 in range(4):
                bank = psalloc()
                for tt in range(4):
                    t = t4 * 4 + tt
                    for c in range(NCH):
                        op("pe", lambda: nc.tensor.matmul(PS[:, bank, tt * 128:(tt + 1) * 128], lhsT=hT[:, c, t * 128:(t + 1) * 128],
                                                          rhs=W.ap[:, c, 2, :], start=(c == 0), stop=(c == NCH - 1)),
                           reads=[W.res, hr[c][t // 4]], writes=[psr[bank]])
                op("act", lambda: nc.scalar.copy(out=vp.ap[:, t4 * 4:(t4 + 1) * 4, :],
                                                 in_=PS[:, bank, :].rearrange("p (t n) -> p t n", t=4)),
                   writes=[psr[bank], vp.res])
            pstate["allowed"] = list(range(5))
            op("pe", lambda: nc.tensor.matmul(PS[:, 5, 0:FILLN], lhsT=onesb.ap, rhs=hT[:, 0, 0:FILLN], start=True, stop=True,
                                              skip_group_check=True),
               reads=[onesb.res], writes=[psr[5]])
            for qg in range(4):
                q0 = qg * 512
                for hh in range(2):
                    op("pool", lambda: nc.gpsimd.memset(Rb[hh].ap, 0.0), writes=[Rb[hh].res])
                nsteps = 4 * qg + 4
                zb = {}

                def emit_z(step, hh):
                    kb = 4 * qg + 3 - step
                    c0 = max(0, kb - 4 * qg) * 128
                    bank = psalloc()
                    ps_ = slice(hh * 64, hh * 64 + 64)
                    op("pe", lambda: nc.tensor.matmul(PS[:, bank, c0:512], lhsT=kT.ap[ps_, kb * 128:(kb + 1) * 128],
                                                      rhs=qsT.ap[ps_, q0 + c0:q0 + 512], start=True, stop=True),
                       reads=[kT.res, qsT.res], writes=[psr[bank]])
                    filler(FILL)
                    zb[(step, hh)] = bank

                for hh in range(2):
                    emit_z(0, hh)
                for step in range(nsteps):
                    kb = 4 * qg + 3 - step
                    diag = kb >= 4 * qg
                    c0 = max(0, kb - 4 * qg) * 128
                    N = 512 - c0
                    sps, ats, cbs, ebs = [], [], [], []
                    for hh in range(2):
                        bank = zb.pop((step, hh))
                        eb = ebuf[cnt["e"] % 4]
                        cnt["e"] += 1
                        ebs.append(eb)
                        sp_ = spb[cnt["sp"] % 4]
                        cnt["sp"] += 1
                        op("act", lambda: nc.scalar.activation(out=eb.ap[:, c0:512], in_=PS[:, bank, c0:512], func=AF.Exp),
                           writes=[psr[bank], eb.res])
                        op("act", lambda: nc.scalar.activation(out=sp_.ap[:, c0:512], in_=eb.ap[:, c0:512], func=AF.Ln, bias=1.0),
                           reads=[eb.res], writes=[sp_.res])
                        if diag:
                            op("dve", lambda: nc.vector.tensor_tensor(out=sp_.ap[:, c0:c0 + 128], in0=sp_.ap[:, c0:c0 + 128],
                                                                      in1=maskb.ap, op=ALU.mult),
                               reads=[sp_.res, maskb.res], writes=[sp_.res])
                        sps.append(sp_)
                    for hh in range(2):
                        sp_ = sps[hh]
                        bank = psalloc()
                        cbs.append(bank)
                        ps_ = slice(hh * 64, hh * 64 + 64)
                        op("pe", lambda: nc.tensor.matmul(PS[:, bank, c0:512], lhsT=tri.ap, rhs=sp_.ap[:, c0:512], start=True, stop=(step == 0)),
                           reads=[tri.res, sp_.res], writes=[psr[bank]])
                        filler(FILL)
                        if step > 0:
                            op("pe", lambda: nc.tensor.matmul(PS[:, bank, c0:512], lhsT=onesb.ap, rhs=Rb[hh].ap[:, c0:512],
                                                              start=False, stop=True),
                               reads=[onesb.res, Rb[hh].res], writes=[psr[bank]])
                            filler(FILL)
                    if step + 1 < nsteps:
                        for hh in range(2):
                            emit_z(step + 1, hh)
                    for hh in range(2):
                        sp_ = sps[hh]
                        bank = cbs[hh]
                        at = aTb[cnt["a"] % 4]
                        cnt["a"] += 1
                        ec = ecb[hh]
                        eb = ebs[hh]
                        op("act", lambda: nc.scalar.activation(out=ec.ap[:, c0:512], in_=PS[:, bank, c0:512], func=AF.Exp, scale=-1.0),
                           writes=[psr[bank], ec.res])
                        op("dve", lambda: nc.vector.tensor_tensor(out=at.ap[:, c0:512], in0=eb.ap[:, c0:512], in1=ec.ap[:, c0:512], op=ALU.mult),
                           reads=[eb.res, ec.res], writes=[at.res])
                        if diag:
                            op("dve", lambda: nc.vector.tensor_tensor(out=at.ap[:, c0:c0 + 128], in0=at.ap[:, c0:c0 + 128],
                                                                      in1=maskb.ap, op=ALU.mult),
                               reads=[at.res, maskb.res], writes=[at.res])
                        if step + 1 < nsteps:
                            op("pool", lambda: nc.gpsimd.tensor_tensor(out=Rb[hh].ap[:, c0:512], in0=Rb[hh].ap[:, c0:512],
                                                                       in1=sp_.ap[:, c0:512], op=ALU.add),
                               reads=[Rb[hh].res, sp_.res], writes=[Rb[hh].res])
                        ab = 6 + hh
                        op("pe", lambda: nc.tensor.matmul(PS[0:64, ab, c0:512], lhsT=vp.ap[:, kb, hh * 64:(hh + 1) * 64],
                                                          rhs=at.ap[:, c0:512], start=(step == 0), stop=(step == nsteps - 1),
                                                          skip_group_check=True),
                           reads=[vp.res, at.res], writes=[psr[ab]])
                        filler(FILL)
                for hh in range(2):
                    ab = 6 + hh
                    op("act", lambda: nc.scalar.copy(out=oT.ap[0:64, hh, q0:q0 + 512], in_=PS[0:64, ab, :]),
                       writes=[psr[ab], oT.res])
            pstate["allowed"] = list(range(6))
            for tb in range(NTB):
                for nn in range(NCH):
                    bank = psalloc()
                    for hh in range(2):
                        op("pe", lambda: nc.tensor.matmul(psb(bank), lhsT=wo.ap[0:64, hh, nn * 128:(nn + 1) * 128],
                                                          rhs=oT.ap[0:64, hh, tbs(tb)], start=(hh == 0), stop=(hh == 1)),
                           reads=[wo.res, oT.res], writes=[psr[bank]])
                    resid_add(nn, tb, bank)

    def body(b):
        load_x(b)
        for sl in sublayers:
            if sl[0] == "ret":
                retention(sl[1])
            elif sl[0] == "sb":
                stick(sl[1])
            else:
                ffn(sl[1])
        store_out(b)

    reset_sync()
    if nseq == 1:
        body(0)
        reset_sync()
    else:
        with nc.Fori(0, nseq, hint_back_edge=True) as b:
            body(b)
            reset_sync()
    return nc


def make_consts():
    bf = ml_dtypes.bfloat16
    c = {}
    c["c_identb"] = np.eye(128, dtype=np.float32).astype(bf)
    c["c_identf"] = np.eye(128, dtype=np.float32)
    jj = np.arange(128)
    c["c_tri"] = (jj[:, None] >= jj[None, :]).astype(np.float32).astype(bf)
    c["c_mask"] = (jj[:, None] < jj[None, :]).astype(np.float32).astype(bf)
    dm = np.zeros((128, 4, 128), np.float64)
    kd = np.zeros((128, 4), np.float64)
    ev = np.zeros((128, 4), np.float64)
    for h in range(4):
        g = 1.0 - 2.0 ** (-5.0 - h)
        k = jj.astype(np.float64)
        dm[:, h, :] = np.where(jj[None, :] >= jj[:, None], (g ** (-(k + 1.0)))[:, None] / 16.0, 0.0)
        kd[:, h] = g ** (127.0 - k) / 16.0
        ev[:, h] = EPS * g ** (-2.0 * (k + 1.0))
    c["c_dm"] = dm.reshape(128, 512).astype(np.float32)
    c["c_kdecs"] = kd.astype(np.float32)
    c["c_epsv"] = ev.astype(np.float32)
    pos = np.arange(S, dtype=np.float32)
    inv = (np.float32(10000.0) ** (-np.arange(0, 256, 2, dtype=np.float32) / np.float32(256))).astype(np.float32)
    ang = (pos[:, None] * inv[None, :]).astype(np.float32)
    c["c_cos"] = np.ascontiguousarray(np.cos(ang).T.astype(np.float32))
    c["c_sin"] = np.ascontiguousarray(np.sin(ang).T.astype(np.float32))
    return c


def layout_params(inp):
    f = lambda a: np.asarray(a, dtype=np.float32)
    gl = [f(inp["ret_norm"])[0], f(inp["ret_norm"])[1], f(inp["sb_norm"])[0], f(inp["sb_norm"])[1]]
    gl += [f(inp["ffn_norm"])[l] for l in range(4)] + [f(inp["final_norm"])]
    gains = np.stack([g.reshape(8, 128).T for g in gl], axis=1).reshape(128, 72)
    cwv = f(inp["ffn_conv_w"])
    cw = cwv.reshape(4, 3, 44, 128).transpose(3, 0, 2, 1).reshape(128, 4 * 44 * 3)
    cbv = f(inp["ffn_conv_b"])
    cb = cbv.reshape(4, 44, 128).transpose(2, 0, 1).reshape(128, 4 * 44)
    return {"c_gains": np.ascontiguousarray(gains), "c_cw": np.ascontiguousarray(cw), "c_cb": np.ascontiguousarray(cb)}


FULL = [("ret", 0), ("ffn", 0), ("sb", 0), ("ffn", 1), ("ret", 1), ("ffn", 2), ("sb", 1), ("ffn", 3)]


def run(inputs, nseq, sublayers, xs_per_core, trace=False):
    nc = build_program(nseq, sublayers)
    common = {}
    common.update(make_consts())
    common.update(layout_params(inputs))
    for sl in set(sublayers):
        names = {"ret": ("ret_w_in", "ret_w_out"), "sb": ("sb_w_in", "sb_w_out"), "ffn": ("ffn_w_up", "ffn_w_down")}[sl[0]]
        for k in names:
            common["%s_%d" % (k, sl[1])] = np.ascontiguousarray(np.asarray(inputs[k][sl[1]], dtype=np.float32))
    in_maps = []
    for xs in xs_per_core:
        m = dict(common)
        m["x"] = np.ascontiguousarray(xs)
        in_maps.append(m)
    res = run_bass_kernel_spmd(nc, in_maps, core_ids=list(range(len(xs_per_core))), trace=trace)
    return res


def kernel(**inputs):
    x = np.asarray(inputs["x"], dtype=np.float32)
    B = x.shape[0]
    per = B // NCORES
    xs = [x[i * per:(i + 1) * per] for i in range(NCORES)]
    res = run(inputs, per, FULL, xs)
    out = np.concatenate([np.asarray(r["out"], dtype=np.float32) for r in res.results], axis=0)
    return out
```

```python
import math
import numpy as np
import ml_dtypes
import concourse.bass as bass
import concourse.mybir as mybir
from concourse.bass_utils import run_bass_kernel_spmd

F32 = mybir.dt.float32
BF16 = mybir.dt.bfloat16
AF = mybir.ActivationFunctionType
ALU = mybir.AluOpType

D = 1024
S = 2048
NCH = 8
NTB = 4
DFF = 2816
NFC = 22
EPS = 1e-6
NCORES = 8
import os
FILL = int(os.environ.get('SB_FILL', '1'))
FILLN = int(os.environ.get('SB_FILLN', '256'))
RFILL = int(os.environ.get('RET_FILL', '8'))


class Res:
    __slots__ = ("w", "r")
    registry = []

    def __init__(self):
        self.w = None
        self.r = {}
        Res.registry.append(self)


class Buf:
    __slots__ = ("ap", "res")

    def __init__(self, ap):
        self.ap = ap
        self.res = Res()


class Eng:
    def __init__(self, name, h, sem, raw):
        self.name = name
        self.h = h
        self.sem = sem
        self.cnt = 0
        self.waited = {}
        self.raw = raw


class Tr:
    def __init__(self, nc):
        self.nc = nc
        self.engs = {}
        for name, h, raw in (("pe", nc.tensor, False), ("act", nc.scalar, True),
                             ("dve", nc.vector, True), ("pool", nc.gpsimd, True),
                             ("sp", nc.sync, False)):
            self.engs[name] = Eng(name, h, nc.alloc_semaphore("sem_" + name), raw)
        self.ring = [nc.alloc_semaphore("dring%d" % i) for i in range(16)]
        self.ring_n = 0
        self.ring_val = [0] * 16

    def _collect(self, e, reads, writes, extra=()):
        deps = {}

        def add(tok, same_ok):
            if tok is None:
                return
            sem, val = tok
            if sem is e.sem and not same_ok:
                return
            k = id(sem)
            if e.waited.get(k, 0) >= val:
                return
            if k not in deps or deps[k][1] < val:
                deps[k] = tok

        for r in reads:
            add(r.w, e.raw)
        for w in writes:
            add(w.w, False)
            for tok in w.r.values():
                add(tok, False)
        for tok in extra:
            add(tok, False)
        return list(deps.values())

    def op(self, en, fn, reads=(), writes=()):
        e = self.engs[en]
        toks = self._collect(e, reads, writes)
        for tok in toks[:-1]:
            e.h.wait_ge(tok[0], tok[1])
        ins = fn()
        if toks:
            ins._wait_ge(toks[-1][0], toks[-1][1])
        for tok in toks:
            e.waited[id(tok[0])] = tok[1]
        e.cnt += 1
        ins.then_inc(e.sem, 1)
        me = (e.sem, e.cnt)
        for r in reads:
            r.r[id(e.sem)] = me
        for w in writes:
            w.w = me
            w.r = {}
        return ins

    def dma(self, out, in_, reads=(), writes=(), en="sp"):
        e = self.engs[en]
        i = self.ring_n % 16
        self.ring_n += 1
        sem = self.ring[i]
        prev = (sem, self.ring_val[i]) if self.ring_val[i] > 0 else None
        self.ring_val[i] += 16
        toks = self._collect(e, reads, writes, extra=(prev,) if prev else ())
        for tok in toks:
            e.h.wait_ge(tok[0], tok[1])
            e.waited[id(tok[0])] = tok[1]
        e.h.dma_start(out=out, in_=in_).then_inc(sem, 16)
        me = (sem, self.ring_val[i])
        for r in reads:
            r.r[id(sem)] = me
        for w in writes:
            w.w = me
            w.r = {}

    def all_tokens(self):
        toks = [(e.sem, e.cnt) for e in self.engs.values() if e.cnt > 0]
        toks += [(self.ring[i], self.ring_val[i]) for i in range(16) if self.ring_val[i] > 0]
        return toks

    def barrier(self, names=("pe", "act", "dve", "pool", "sp")):
        toks = self.all_tokens()
        for n in names:
            e = self.engs[n]
            for sem, val in toks:
                if sem is e.sem:
                    continue
                if e.waited.get(id(sem), 0) >= val:
                    continue
                e.h.wait_ge(sem, val)
                e.waited[id(sem)] = val


class Carver:
    def __init__(self, arena, base, limit):
        self.a = arena
        self.off = base
        self.limit = limit

    def take(self, shape, dt):
        n = 1
        for s_ in shape:
            n *= s_
        nbytes = n * (4 if dt is F32 else 2)
        start = self.off
        self.off += (nbytes + 63) // 64 * 64
        assert self.off <= self.limit, ("SBUF carve overflow", self.off, self.limit)
        ap = self.a[:, start // 2: start // 2 + nbytes // 2]
        if dt is F32:
            ap = ap.bitcast(F32)
        if len(shape) == 2:
            ap = ap.rearrange("p (a b) -> p a b", a=shape[0])
        elif len(shape) == 3:
            ap = ap.rearrange("p (a b c) -> p a b c", a=shape[0], b=shape[1])
        return ap

    def buf(self, shape, dt):
        return Buf(self.take(shape, dt))


ARENA_BYTES = 212000
R_BASE = 65536 + 32768 + 8192


def build_program(nseq, sublayers, do_prep=True):
    nc = bass.Bass("TRN2", target_bir_lowering=False)
    Res.registry = []
    tr = Tr(nc)
    op = tr.op

    def reset_sync():
        tr.barrier()
        nc.all_engine_barrier()
        for e in tr.engs.values():
            e.h.sem_clear(e.sem)
        for sem in tr.ring:
            nc.sync.sem_clear(sem)
        nc.all_engine_barrier()
        for e in tr.engs.values():
            e.cnt = 0
            e.waited = {}
        tr.ring_n = 0
        tr.ring_val = [0] * 16
        for r in Res.registry:
            r.w = None
            r.r = {}

    def din(name, shape, dt=F32):
        return nc.dram_tensor(name, list(shape), dt, kind="ExternalInput").ap()

    x_d = din("x", [nseq, S, D])
    out_d = nc.dram_tensor("out", [nseq, S, D], F32, kind="ExternalOutput").ap()
    used = set(sublayers)
    ret_w_in = {j: din("ret_w_in_%d" % j, [D, 6144]) for j in range(2) if ("ret", j) in used}
    ret_w_out = {j: din("ret_w_out_%d" % j, [2048, D]) for j in range(2) if ("ret", j) in used}
    sb_w_in = {j: din("sb_w_in_%d" % j, [D, 3072]) for j in range(2) if ("sb", j) in used}
    sb_w_out = {j: din("sb_w_out_%d" % j, [D, D]) for j in range(2) if ("sb", j) in used}
    ffn_w_up = {l: din("ffn_w_up_%d" % l, [D, 2 * DFF]) for l in range(4) if ("ffn", l) in used}
    ffn_w_down = {l: din("ffn_w_down_%d" % l, [DFF, D]) for l in range(4) if ("ffn", l) in used}
    c_gains = din("c_gains", [128, 9 * 8])
    c_cw = din("c_cw", [128, 4 * 44 * 3])
    c_cb = din("c_cb", [128, 4 * 44])
    c_identb = din("c_identb", [128, 128], BF16)
    c_identf = din("c_identf", [128, 128])
    c_tri = din("c_tri", [128, 128], BF16)
    c_mask = din("c_mask", [128, 128], BF16)
    c_dm = din("c_dm", [128, 4 * 128])
    c_kdecs = din("c_kdecs", [128, 4])
    c_epsv = din("c_epsv", [128, 4])
    c_cos = din("c_cos", [128, S])
    c_sin = din("c_sin", [128, S])

    def dscr(name, shape):
        return nc.dram_tensor(name, list(shape), BF16).ap()

    s_rq = [dscr("s_rq%d" % j, [4, 128, 2048]) for j in range(2)]
    s_rk = [dscr("s_rk%d" % j, [4, 128, 2048]) for j in range(2)]
    s_rv = [dscr("s_rv%d" % j, [4, 128, 4096]) for j in range(2)]
    s_rg = [dscr("s_rg%d" % j, [4, 128, 4096]) for j in range(2)]
    s_ro = [dscr("s_ro%d" % j, [4, 128, 4096]) for j in range(2)]
    s_sw = [dscr("s_sw%d" % j, [8, 128, 3072]) for j in range(2)]
    s_so = [dscr("s_so%d" % j, [8, 64, 2048]) for j in range(2)]
    s_fu = [dscr("s_fu%d" % l, [22, 128, 2048]) for l in range(4)]
    s_fd = [dscr("s_fd%d" % l, [128, 22 * 1024]) for l in range(4)]

    arena = nc.alloc_sbuf_tensor("arena", [128, ARENA_BYTES // 2], BF16)
    pc = Carver(arena, 0, R_BASE)
    xT = pc.take([NCH, S], F32)
    hT = pc.take([NCH, S], BF16)
    xr = [[Res() for _ in range(NTB)] for _ in range(NCH)]
    hr = [[Res() for _ in range(NTB)] for _ in range(NCH)]
    identb = pc.buf([128], BF16)
    identf = pc.buf([128], F32)
    onesb = pc.buf([128], BF16)
    tri = pc.buf([128], BF16)
    maskb = pc.buf([128], BF16)
    dm = pc.buf([4, 128], F32)
    kdecs = pc.buf([4], F32)
    epsv = pc.buf([4], F32)
    gains = pc.buf([72], F32)
    g32 = pc.buf([72], F32)
    cw = pc.buf([4 * 44 * 3], F32)
    cb = pc.buf([4 * 44], F32)
    epsc = pc.buf([8], F32)

    PS = nc.alloc_psum_tensor("ps", [128, 8, 512], F32)
    psr = [Res() for _ in range(8)]
    pstate = {"i": 0, "allowed": list(range(8))}

    def psalloc():
        a = pstate["allowed"]
        pstate["i"] = (pstate["i"] + 1) % len(a)
        return a[pstate["i"]]

    def psb(b):
        return PS[:, b, :]

    def psb16(b):
        return PS[:, b, :].bitcast(BF16)

    for b_, src in ((identb, c_identb), (identf, c_identf), (tri, c_tri), (maskb, c_mask),
                    (kdecs, c_kdecs), (epsv, c_epsv), (gains, c_gains), (cw, c_cw), (cb, c_cb)):
        tr.dma(out=b_.ap, in_=src, writes=[b_.res])
    tr.dma(out=dm.ap, in_=c_dm.rearrange("p (h c) -> p h c", h=4), writes=[dm.res])
    op("dve", lambda: nc.vector.memset(onesb.ap, 1.0), writes=[onesb.res])
    op("dve", lambda: nc.vector.memset(epsc.ap, 1024.0 * EPS), writes=[epsc.res])
    op("dve", lambda: nc.vector.tensor_scalar(out=g32.ap, in0=gains.ap, scalar1=32.0, scalar2=None,
                                               op0=ALU.mult), reads=[gains.res], writes=[g32.res])

    cast_rr = {"i": 0}

    def cast(out, in_, reads, writes):
        k = cast_rr["i"] % 3
        cast_rr["i"] += 1
        if k == 0:
            op("act", lambda: nc.scalar.copy(out=out, in_=in_), reads=reads, writes=writes)
        else:
            op("dve", lambda: nc.vector.tensor_copy(out, in_), reads=reads, writes=writes)

    used = set(sublayers)
    if do_prep:
        rc = Carver(arena, R_BASE, ARENA_BYTES)
        stg = [rc.buf([4096], F32) for _ in range(3)]
        bst = [rc.buf([4096], BF16) for _ in range(3)]
        pi = {"i": 0}

        def piece(loads, n_el, store_fn, cast_views=None, parts=128):
            k = pi["i"] % 3
            pi["i"] += 1
            sb_, bb_ = stg[k], bst[k]
            for (dst_fn, src) in loads:
                tr.dma(out=dst_fn(sb_.ap), in_=src, writes=[sb_.res])
            if cast_views is None:
                cast(bb_.ap[:parts, :n_el], sb_.ap[:parts, :n_el], [sb_.res], [bb_.res])
            else:
                o_, i_ = cast_views(bb_.ap, sb_.ap)
                cast(o_, i_, [sb_.res], [bb_.res])
            store_fn(bb_)

        for j in range(2):
            if ("ret", j) in used:
                w = ret_w_in[j].rearrange("(c p) n -> p c n", p=128)
                for h in range(4):
                    for (scr, c0, ns) in ((s_rq[j], h * 256, 256), (s_rk[j], 1024 + h * 256, 256),
                                          (s_rv[j], 2048 + h * 512, 512), (s_rg[j], 4096 + h * 512, 512)):
                        ne = 8 * ns
                        piece([(lambda a, ns=ns, ne=ne: a[:, :ne].rearrange("p (c n) -> p c n", c=8),
                                w[:, :, c0:c0 + ns])], ne,
                              lambda bb, scr=scr, h=h, ne=ne: tr.dma(out=scr[h], in_=bb.ap[:, :ne], reads=[bb.res]))
                    wo = ret_w_out[j][h * 512:(h + 1) * 512, :].rearrange("(e p) n -> p e n", p=128)
                    piece([(lambda a: a.rearrange("p (e n) -> p e n", e=4), wo)], 4096,
                          lambda bb, j=j, h=h: tr.dma(out=s_ro[j][h], in_=bb.ap, reads=[bb.res]))
            if ("sb", j) in used:
                w = sb_w_in[j].rearrange("(c p) n -> p c n", p=128)
                for p_ in range(8):
                    loads = []
                    for t3 in range(3):
                        loads.append((lambda a, t3=t3: a[:, :3072].rearrange("p (c t n) -> p c t n", c=8, t=3)[:, :, t3, :],
                                      w[:, :, t3 * 1024 + p_ * 128: t3 * 1024 + (p_ + 1) * 128]))
                    piece(loads, 3072,
                          lambda bb, j=j, p_=p_: tr.dma(out=s_sw[j][p_], in_=bb.ap[:, :3072], reads=[bb.res]))
                    wo = sb_w_out[j][p_ * 128:(p_ + 1) * 128, :].rearrange("(hh d) n -> d hh n", d=64)
                    piece([(lambda a: a[:64, :2048].rearrange("p (hh n) -> p hh n", hh=2), wo)], 2048,
                          lambda bb, j=j, p_=p_: tr.dma(out=s_so[j][p_], in_=bb.ap[:64, :2048], reads=[bb.res]),
                          parts=64)
        for l in range(4):
            if ("ffn", l) in used:
                w = ffn_w_up[l].rearrange("(c p) n -> p c n", p=128)
                for g in range(11):
                    loads = []
                    for gv in range(2):
                        loads.append((lambda a, gv=gv: a.rearrange("p (c gv n) -> p c gv n", c=8, gv=2)[:, :, gv, :],
                                      w[:, :, gv * DFF + g * 256: gv * DFF + (g + 1) * 256]))

                    def cviews(bb, sb_):
                        o_ = bb.rearrange("p (pr cg n) -> p pr cg n", pr=2, n=128)
                        i_ = sb_.rearrange("p (cg pr n) -> p pr cg n", pr=2, n=128)
                        return o_, i_

                    def store(bb, l=l, g=g):
                        tr.dma(out=s_fu[l][2 * g:2 * g + 2].rearrange("s p n -> p s n"),
                               in_=bb.ap.rearrange("p (s n) -> p s n", s=2), reads=[bb.res])

                    piece(loads, 4096, store, cast_views=cviews)
                wd = ffn_w_down[l].rearrange("(k p) n -> p k n", p=128)
                for q in range(6):
                    k0 = q * 4
                    nk = min(4, 22 - k0)
                    ne = nk * 1024
                    piece([(lambda a, nk=nk, ne=ne: a[:, :ne].rearrange("p (k n) -> p k n", k=nk), wd[:, k0:k0 + nk, :])], ne,
                          lambda bb, l=l, k0=k0, ne=ne: tr.dma(out=s_fd[l][:, k0 * 1024:k0 * 1024 + ne], in_=bb.ap[:, :ne],
                                                               reads=[bb.res]))
        tr.barrier()

    def tbs(tb):
        return slice(tb * 512, (tb + 1) * 512)

    def norm_block(tb, gi, sqs, rstd, dst_fn, dst_res):
        bank = psalloc()
        for c in range(NCH):
            sqb = sqs[c % len(sqs)]
            op("act", lambda: nc.scalar.activation(out=sqb.ap, in_=xT[:, c, tbs(tb)], func=AF.Square),
               reads=[xr[c][tb]], writes=[sqb.res])
            op("pe", lambda: nc.tensor.matmul(psb(bank), lhsT=onesb.ap, rhs=sqb.ap, start=(c == 0), stop=(c == NCH - 1)),
               reads=[sqb.res, onesb.res], writes=[psr[bank]])
        op("act", lambda: nc.scalar.activation(out=rstd.ap, in_=psb(bank), func=AF.Ln, bias=epsc.ap[:, 0:1]),
           reads=[epsc.res], writes=[psr[bank], rstd.res])
        op("act", lambda: nc.scalar.activation(out=rstd.ap, in_=rstd.ap, func=AF.Exp, scale=-0.5),
           reads=[rstd.res], writes=[rstd.res])
        for c in range(NCH):
            en = "dve"
            eh = nc.vector
            op(en, lambda: eh.scalar_tensor_tensor(out=dst_fn(c), in0=xT[:, c, tbs(tb)],
                                                   scalar=g32.ap[:, gi * 8 + c: gi * 8 + c + 1], in1=rstd.ap,
                                                   op0=ALU.mult, op1=ALU.mult),
               reads=[xr[c][tb], rstd.res, g32.res], writes=[dst_res(c)])

    def resid_add(n, tb, bank):
        op("dve", lambda: nc.vector.tensor_tensor(out=xT[:, n, tbs(tb)], in0=psb(bank), in1=xT[:, n, tbs(tb)], op=ALU.add),
           reads=[xr[n][tb]], writes=[psr[bank], xr[n][tb]])

    def load_x(b):
        tr.barrier()
        rc = Carver(arena, R_BASE, ARENA_BYTES)
        st = [rc.buf([1024], F32) for _ in range(2)]
        pstate["allowed"] = list(range(8))
        for t in range(16):
            sb_ = st[t % 2]
            tr.dma(out=sb_.ap, in_=x_d[b, t * 128:(t + 1) * 128, :], writes=[sb_.res])
            for half in range(2):
                bank = psalloc()
                for cc in range(4):
                    c = half * 4 + cc
                    op("pe", lambda: nc.tensor.transpose(PS[:, bank, cc * 128:(cc + 1) * 128], sb_.ap[:, c * 128:(c + 1) * 128], identf.ap),
                       reads=[sb_.res, identf.res], writes=[psr[bank]])
                tb = t // 4
                dst = xT[:, half * 4:(half + 1) * 4, t * 128:(t + 1) * 128]
                src = PS[:, bank, :].rearrange("p (c n) -> p c n", c=4)
                wr = [psr[bank]] + [xr[half * 4 + cc][tb] for cc in range(4)]
                if half == 0:
                    op("act", lambda: nc.scalar.copy(out=dst, in_=src), writes=wr)
                else:
                    op("dve", lambda: nc.vector.tensor_copy(dst, src), writes=wr)

    def store_out(b):
        tr.barrier()
        rc = Carver(arena, R_BASE, ARENA_BYTES)
        sqs = [rc.buf([512], BF16) for _ in range(3)]
        rstd = rc.buf([512], F32)
        yn = rc.buf([NCH, 512], F32)
        ynr = [Res() for _ in range(NCH)]
        ost = [rc.buf([1024], F32) for _ in range(2)]
        pstate["allowed"] = list(range(8))
        for tb in range(NTB):
            norm_block(tb, 8, sqs, rstd, lambda c: yn.ap[:, c, :], lambda c: ynr[c])
            for tt in range(4):
                t = tb * 4 + tt
                ob = ost[t % 2]
                for half in range(2):
                    bank = psalloc()
                    for cc in range(4):
                        c = half * 4 + cc
                        op("pe", lambda: nc.tensor.transpose(PS[:, bank, cc * 128:(cc + 1) * 128], yn.ap[:, c, tt * 128:(tt + 1) * 128], identf.ap),
                           reads=[ynr[c], identf.res], writes=[psr[bank]])
                    if half == 0:
                        op("act", lambda: nc.scalar.copy(out=ob.ap[:, 0:512], in_=psb(bank)), writes=[psr[bank], ob.res])
                    else:
                        op("dve", lambda: nc.vector.tensor_copy(ob.ap[:, 512:1024], psb(bank)), writes=[psr[bank], ob.res])
                tr.dma(out=out_d[b, t * 128:(t + 1) * 128, :], in_=ob.ap, reads=[ob.res])

    def ffn(l):
        tr.barrier()
        rc = Carver(arena, R_BASE, ARENA_BYTES)
        wd = rc.buf([NFC, 1024], BF16)
        wups = [rc.buf([8, 2, 128], BF16) for _ in range(3)]
        mT = rc.buf([NFC, 512], BF16)
        sqs = [rc.buf([512], BF16) for _ in range(3)]
        rstd = rc.buf([512], F32)
        U = [[rc.buf([514], F32) for _ in range(2)] for _ in range(2)]
        ACC = [[rc.buf([512], F32) for _ in range(2)] for _ in range(2)]
        H = rc.buf([44, 2], F32)
        pstate["allowed"] = list(range(8))
        op("dve", lambda: nc.vector.memset(H.ap, 0.0), writes=[H.res])
        seq = [(tb, i) for tb in range(NTB) for i in range(NFC)]
        issued = {"n": 0}

        def ensure(upto):
            while issued["n"] <= min(upto, len(seq) - 1):
                n = issued["n"]
                tb_, i_ = seq[n]
                wb = wups[n % 3]
                tr.dma(out=wb.ap, in_=s_fu[l][i_].rearrange("p (c g n) -> p c g n", c=8, g=2), writes=[wb.res])
                if n == 1:
                    tr.dma(out=wd.ap, in_=s_fd[l].rearrange("p (k n) -> p k n", k=NFC), writes=[wd.res])
                issued["n"] += 1

        cwl = lambda i, gv, k: cw.ap[:, ((l * 44 + gv * 22 + i) * 3 + k):((l * 44 + gv * 22 + i) * 3 + k + 1)]
        cbl = lambda i, gv: cb.ap[:, (l * 44 + gv * 22 + i):(l * 44 + gv * 22 + i + 1)]
        n = 0
        norm_block(0, 4 + l, sqs, rstd, lambda c: hT[:, c, tbs(0)], lambda c: hr[c][0])
        for tb in range(NTB):
            for i in range(NFC):
                ensure(n + 2)
                wb = wups[n % 3]
                par = n % 2
                n += 1
                banks = []
                for gv in range(2):
                    bank = psalloc()
                    banks.append(bank)
                    for c in range(NCH):
                        op("pe", lambda: nc.tensor.matmul(psb(bank), lhsT=wb.ap[:, c, gv, :], rhs=hT[:, c, tbs(tb)],
                                                          start=(c == 0), stop=(c == NCH - 1)),
                           reads=[wb.res, hr[c][tb]], writes=[psr[bank]])
                for gv in range(2):
                    bank = banks[gv]
                    u = U[gv][par]
                    acc = ACC[gv][par]
                    hcol = H.ap[:, gv * 22 + i, :]
                    op("act", lambda: nc.scalar.copy(out=u.ap[:, 0:2], in_=hcol), reads=[H.res], writes=[u.res])
                    op("act", lambda: nc.scalar.copy(out=u.ap[:, 2:514], in_=psb(bank)), writes=[psr[bank], u.res])
                    op("act", lambda: nc.scalar.copy(out=hcol, in_=u.ap[:, 512:514]), reads=[u.res], writes=[H.res])
                    e2 = "dve"
                    h2 = nc.vector
                    op("act", lambda: nc.scalar.activation(out=acc.ap, in_=psb(bank), func=AF.Identity,
                                                           scale=cwl(i, gv, 2), bias=cbl(i, gv)),
                       reads=[cw.res, cb.res], writes=[psr[bank], acc.res])
                    op(e2, lambda: h2.scalar_tensor_tensor(out=acc.ap, in0=u.ap[:, 1:513], scalar=cwl(i, gv, 1), in1=acc.ap,
                                                           op0=ALU.mult, op1=ALU.add),
                       reads=[u.res, acc.res, cw.res], writes=[acc.res])
                    op(e2, lambda: h2.scalar_tensor_tensor(out=acc.ap, in0=u.ap[:, 0:512], scalar=cwl(i, gv, 0), in1=acc.ap,
                                                           op0=ALU.mult, op1=ALU.add),
                       reads=[u.res, acc.res, cw.res], writes=[acc.res])
                ag, av = ACC[0][par], ACC[1][par]
                op("act", lambda: nc.scalar.activation(out=ag.ap, in_=ag.ap, func=AF.Silu), reads=[ag.res], writes=[ag.res])
                op("pool", lambda: nc.gpsimd.tensor_tensor(out=mT.ap[:, i, :], in0=ag.ap, in1=av.ap, op=ALU.mult),
                   reads=[ag.res, av.res], writes=[mT.res])
            if tb + 1 < NTB:
                norm_block(tb + 1, 4 + l, sqs, rstd, lambda c: hT[:, c, tbs(tb + 1)], lambda c: hr[c][tb + 1])
            for nn in range(NCH):
                bank = psalloc()
                for k in range(NFC):
                    op("pe", lambda: nc.tensor.matmul(psb(bank), lhsT=wd.ap[:, k, nn * 128:(nn + 1) * 128], rhs=mT.ap[:, k, :],
                                                      start=(k == 0), stop=(k == NFC - 1)),
                       reads=[wd.res, mT.res], writes=[psr[bank]])
                resid_add(nn, tb, bank)

    def retention(j):
        tr.barrier()
        rc = Carver(arena, R_BASE, ARENA_BYTES)
        Wq = [rc.buf([8, 256], BF16) for _ in range(2)]
        Wk = [rc.buf([8, 256], BF16) for _ in range(2)]
        Wv = rc.buf([8, 512], BF16)
        Wg = rc.buf([8, 512], BF16)
        Wo = rc.buf([4, 1024], BF16)
        qT = rc.buf([2, S], BF16)
        kT = rc.buf([2, S], BF16)
        yT = [rc.buf([4, 512], BF16) for _ in range(2)]
        cs = [[rc.buf([512], F32) for _ in range(2)] for _ in range(2)]
        tmp = [rc.buf([512], F32) for _ in range(4)]
        vc = [rc.buf([512], BF16) for _ in range(2)]
        sgc = [rc.buf([512], BF16) for _ in range(2)]
        sT = [rc.buf([128], BF16) for _ in range(2)]
        kdc = [rc.buf([256], BF16) for _ in range(2)]
        S32 = rc.buf([2, 512], F32)
        Sbf = [rc.buf([2, 512], BF16) for _ in range(2)]
        yc = [rc.buf([512], BF16) for _ in range(2)]
        junk = rc.buf([512], BF16)
        egb = rc.buf([512], F32)
        st = rc.buf([8], F32)
        sqs = [rc.buf([512], BF16) for _ in range(2)]
        rstd = rc.buf([512], F32)
        pstate["allowed"] = list(range(7))
        gam = [1.0 - 2.0 ** (-5.0 - h) for h in range(4)]

        def filler(n=1):
            for _ in range(n * RFILL):
                nc.tensor.matmul(PS[:, 7, 0:FILLN], lhsT=onesb.ap, rhs=hT[:, 0, 0:FILLN], start=True, stop=True, skip_group_check=True)

        def load_qk(h):
            tr.dma(out=Wq[h % 2].ap, in_=s_rq[j][h].rearrange("p (c n) -> p c n", c=8), writes=[Wq[h % 2].res])
            tr.dma(out=Wk[h % 2].ap, in_=s_rk[j][h].rearrange("p (c n) -> p c n", c=8), writes=[Wk[h % 2].res])

        load_qk(0)
        for tb in range(NTB):
            norm_block(tb, j, sqs, rstd, lambda c: hT[:, c, tbs(tb)], lambda c: hr[c][tb])
        csn = 0
        for h in range(4):
            tr.dma(out=Wv.ap, in_=s_rv[j][h].rearrange("p (c n) -> p c n", c=8), writes=[Wv.res])
            tr.dma(out=Wg.ap, in_=s_rg[j][h].rearrange("p (c n) -> p c n", c=8), writes=[Wg.res])
            tr.dma(out=Wo.ap, in_=s_ro[j][h].rearrange("p (e n) -> p e n", e=4), writes=[Wo.res])
            if h + 1 < 4:
                load_qk(h + 1)
            for tb in range(NTB):
                cb_, sb_ = cs[csn % 2]
                csn += 1
                tr.dma(out=cb_.ap, in_=c_cos[:, tbs(tb)], writes=[cb_.res])
                tr.dma(out=sb_.ap, in_=c_sin[:, tbs(tb)], writes=[sb_.res])
                for (W, dst) in ((Wq[h % 2], qT), (Wk[h % 2], kT)):
                    b1, b2 = psalloc(), psalloc()
                    for (bank, half) in ((b1, 0), (b2, 1)):
                        for c in range(NCH):
                            op("pe", lambda: nc.tensor.matmul(psb(bank), lhsT=W.ap[:, c, half * 128:(half + 1) * 128],
                                                              rhs=hT[:, c, tbs(tb)], start=(c == 0), stop=(c == NCH - 1)),
                               reads=[W.res, hr[c][tb]], writes=[psr[bank]])
                    t1, t2, t3, t4 = tmp
                    op("dve", lambda: nc.vector.tensor_tensor(out=t1.ap, in0=psb(b1), in1=cb_.ap, op=ALU.mult),
                       reads=[cb_.res], writes=[psr[b1], t1.res])
                    op("dve", lambda: nc.vector.tensor_tensor(out=t3.ap, in0=psb(b1), in1=sb_.ap, op=ALU.mult),
                       reads=[sb_.res], writes=[psr[b1], t3.res])
                    op("dve", lambda: nc.vector.tensor_tensor(out=t2.ap, in0=psb(b2), in1=sb_.ap, op=ALU.mult),
                       reads=[sb_.res], writes=[psr[b2], t2.res])
                    op("dve", lambda: nc.vector.tensor_tensor(out=t4.ap, in0=psb(b2), in1=cb_.ap, op=ALU.mult),
                       reads=[cb_.res], writes=[psr[b2], t4.res])
                    op("pool", lambda: nc.gpsimd.tensor_tensor(out=dst.ap[:, 0, tbs(tb)], in0=t1.ap, in1=t2.ap, op=ALU.subtract),
                       reads=[t1.res, t2.res], writes=[dst.res])
                    op("pool", lambda: nc.gpsimd.tensor_tensor(out=dst.ap[:, 1, tbs(tb)], in0=t3.ap, in1=t4.ap, op=ALU.add),
                       reads=[t3.res, t4.res], writes=[dst.res])
            abanks = {}

            def emit_A(c):
                tb = c // 4
                ts_ = slice(c * 128, (c + 1) * 128)
                bv, bg = psalloc(), psalloc()
                abanks[c] = (bv, bg)
                for kc in range(NCH):
                    op("pe", lambda: nc.tensor.matmul(psb(bv), lhsT=hT[:, kc, ts_], rhs=Wv.ap[:, kc, :], start=(kc == 0), stop=(kc == 7)),
                       reads=[hr[kc][tb], Wv.res], writes=[psr[bv]])
                for kc in range(NCH):
                    op("pe", lambda: nc.tensor.matmul(psb(bg), lhsT=hT[:, kc, ts_], rhs=Wg.ap[:, kc, :], start=(kc == 0), stop=(kc == 7)),
                       reads=[hr[kc][tb], Wg.res], writes=[psr[bg]])

            def emit_A_evac(c):
                v_, sg_ = vc[c % 2], sgc[c % 2]
                bv, bg = abanks.pop(c)
                op("act", lambda: nc.scalar.copy(out=v_.ap, in_=psb(bv)), writes=[psr[bv], v_.res])
                op("act", lambda: nc.scalar.activation(out=egb.ap, in_=psb(bg), func=AF.Exp, scale=-1.0), writes=[psr[bg], egb.res])
                op("act", lambda: nc.scalar.activation(out=egb.ap, in_=egb.ap, func=AF.Ln, bias=1.0), reads=[egb.res], writes=[egb.res])
                op("act", lambda: nc.scalar.activation(out=egb.ap, in_=egb.ap, func=AF.Exp, scale=-1.0), reads=[egb.res], writes=[egb.res])
                op("dve", lambda: nc.vector.tensor_tensor(out=sg_.ap, in0=psb(bg), in1=egb.ap, op=ALU.mult),
                   reads=[egb.res], writes=[psr[bg], sg_.res])

            emit_A(0)
            emit_A_evac(0)
            for c in range(16):
                tb = c // 4
                ts_ = slice(c * 128, (c + 1) * 128)
                v_, sg_, sT_, kd_, y_ = vc[c % 2], sgc[c % 2], sT[c % 2], kdc[c % 2], yc[c % 2]
                bs = psalloc()
                for dc in range(2):
                    op("pe", lambda: nc.tensor.matmul(PS[:, bs, 0:128], lhsT=kT.ap[:, dc, ts_], rhs=qT.ap[:, dc, ts_],
                                                      start=(dc == 0), stop=(dc == 1)),
                       reads=[kT.res, qT.res], writes=[psr[bs]])
                op("dve", lambda: nc.vector.tensor_tensor(out=sT_.ap, in0=PS[:, bs, 0:128], in1=dm.ap[:, h, :], op=ALU.mult),
                   reads=[dm.res], writes=[psr[bs], sT_.res])
                bo = psalloc()
                sb_prev = Sbf[(c + 1) % 2]
                op("pe", lambda: nc.tensor.matmul(psb(bo), lhsT=sT_.ap, rhs=v_.ap, start=True, stop=(c == 0)),
                   reads=[sT_.res, v_.res], writes=[psr[bo]])
                if c > 0:
                    for dc in range(2):
                        op("pe", lambda: nc.tensor.matmul(psb(bo), lhsT=qT.ap[:, dc, ts_], rhs=sb_prev.ap[:, dc, :],
                                                          start=False, stop=(dc == 1)),
                           reads=[qT.res, sb_prev.res], writes=[psr[bo]])
                if c < 15:
                    bt = psalloc()
                    for dc in range(2):
                        op("pe", lambda: nc.tensor.transpose(psb16(bt)[:, dc * 128:(dc + 1) * 128], kT.ap[:, dc, ts_], identb.ap),
                           reads=[kT.res, identb.res], writes=[psr[bt]])
                    op("act", lambda: nc.scalar.activation(out=kd_.ap, in_=psb16(bt)[:, 0:256], func=AF.Copy,
                                                           scale=kdecs.ap[:, h:h + 1]),
                       reads=[kdecs.res], writes=[psr[bt], kd_.res])
                    emit_A(c + 1)
                    sb_new = Sbf[c % 2]
                    for dc in range(2):
                        bu = psalloc()
                        op("pe", lambda: nc.tensor.matmul(psb(bu), lhsT=kd_.ap[:, dc * 128:(dc + 1) * 128], rhs=v_.ap, start=True, stop=True),
                           reads=[kd_.res, v_.res], writes=[psr[bu]])
                        if c == 0:
                            op("dve", lambda: nc.vector.tensor_copy(S32.ap[:, dc, :], psb(bu)), writes=[psr[bu], S32.res])
                        else:
                            op("dve", lambda: nc.vector.scalar_tensor_tensor(out=S32.ap[:, dc, :], in0=S32.ap[:, dc, :],
                                                                             scalar=float(gam[h] ** 128), in1=psb(bu),
                                                                             op0=ALU.mult, op1=ALU.add),
                               reads=[S32.res], writes=[psr[bu], S32.res])
                    op("dve", lambda: nc.vector.tensor_copy(sb_new.ap, S32.ap), reads=[S32.res], writes=[sb_new.res])
                op("act", lambda: nc.scalar.activation(out=junk.ap, in_=psb(bo), func=AF.Square, accum_out=st.ap[:, 0:1]),
                   writes=[psr[bo], junk.res, st.res])
                op("act", lambda: nc.scalar.activation(out=st.ap[:, 1:2], in_=st.ap[:, 0:1], func=AF.Ln, scale=1.0 / 512.0,
                                                       bias=epsv.ap[:, h:h + 1]),
                   reads=[st.res, epsv.res], writes=[st.res])
                op("act", lambda: nc.scalar.activation(out=st.ap[:, 2:3], in_=st.ap[:, 1:2], func=AF.Exp, scale=-0.5),
                   reads=[st.res], writes=[st.res])
                op("dve", lambda: nc.vector.scalar_tensor_tensor(out=y_.ap, in0=psb(bo), scalar=st.ap[:, 2:3], in1=sg_.ap,
                                                                 op0=ALU.mult, op1=ALU.mult),
                   reads=[st.res, sg_.res], writes=[psr[bo], y_.res])
                for _ in range(RFILL):
                    nc.tensor.matmul(PS[:, 7, :], lhsT=onesb.ap, rhs=hT[:, 0, 0:512], start=True, stop=True, skip_group_check=True)
                by = psalloc()
                for e in range(4):
                    op("pe", lambda: nc.tensor.transpose(psb16(by)[:, e * 128:(e + 1) * 128], y_.ap[:, e * 128:(e + 1) * 128], identb.ap),
                       reads=[y_.res, identb.res], writes=[psr[by]])
                yt = yT[tb % 2]
                op("act", lambda: nc.scalar.copy(out=yt.ap[:, :, (c % 4) * 128:(c % 4 + 1) * 128],
                                                 in_=psb16(by)[:, 0:512].rearrange("p (e n) -> p e n", e=4)),
                   writes=[psr[by], yt.res])
                if c < 15:
                    emit_A_evac(c + 1)
                if c % 4 == 3:
                    for nn in range(NCH):
                        bank = psalloc()
                        for e in range(4):
                            op("pe", lambda: nc.tensor.matmul(psb(bank), lhsT=Wo.ap[:, e, nn * 128:(nn + 1) * 128], rhs=yt.ap[:, e, :],
                                                              start=(e == 0), stop=(e == 3)),
                               reads=[Wo.res, yt.res], writes=[psr[bank]])
                        resid_add(nn, tb, bank)

    def stick(j):
        tr.barrier()
        rc = Carver(arena, R_BASE, ARENA_BYTES)
        Wp = [rc.buf([8, 3, 128], BF16) for _ in range(2)]
        Wo = [rc.buf([2, 1024], BF16) for _ in range(2)]
        qsT = rc.buf([S], BF16)
        kT = rc.buf([S], BF16)
        vp = rc.buf([16, 128], BF16)
        oT = rc.buf([2, S], BF16)
        ebuf = [rc.buf([512], F32) for _ in range(4)]
        ecb = [rc.buf([512], F32) for _ in range(2)]
        spb = [rc.buf([512], BF16) for _ in range(4)]
        Rb = [rc.buf([512], BF16) for _ in range(2)]
        aTb = [rc.buf([512], BF16) for _ in range(4)]
        sqs = [rc.buf([512], BF16) for _ in range(3)]
        rstd = rc.buf([512], F32)
        pstate["allowed"] = list(range(8))

        def filler(n=1):
            for _ in range(n):
                nc.tensor.matmul(PS[:, 5, 0:FILLN], lhsT=onesb.ap, rhs=hT[:, 0, 0:FILLN], start=True, stop=True, skip_group_check=True)

        def load_w(p_):
            tr.dma(out=Wp[p_ % 2].ap, in_=s_sw[j][p_].rearrange("p (c t n) -> p c t n", c=8, t=3), writes=[Wp[p_ % 2].res])
            tr.dma(out=Wo[p_ % 2].ap[:64], in_=s_so[j][p_].rearrange("p (h n) -> p h n", h=2), writes=[Wo[p_ % 2].res])

        load_w(0)
        for tb in range(NTB):
            norm_block(tb, 2 + j, sqs, rstd, lambda c: hT[:, c, tbs(tb)], lambda c: hr[c][tb])
        cnt = {"e": 0, "sp": 0, "a": 0}
        for p_ in range(8):
            if p_ + 1 < 8:
                load_w(p_ + 1)
            W = Wp[p_ % 2]
            wo = Wo[p_ % 2]
            pstate["allowed"] = list(range(8))
            for tb in range(NTB):
                bq, bk = psalloc(), psalloc()
                for (bank, t3) in ((bq, 0), (bk, 1)):
                    for c in range(NCH):
                        op("pe", lambda: nc.tensor.matmul(psb(bank), lhsT=W.ap[:, c, t3, :], rhs=hT[:, c, tbs(tb)],
                                                          start=(c == 0), stop=(c == NCH - 1)),
                           reads=[W.res, hr[c][tb]], writes=[psr[bank]])
                op("act", lambda: nc.scalar.activation(out=qsT.ap[:, tbs(tb)], in_=psb(bq), func=AF.Copy, scale=0.125),
                   writes=[psr[bq], qsT.res])
                op("act", lambda: nc.scalar.copy(out=kT.ap[:, tbs(tb)], in_=psb(bk)), writes=[psr[bk], kT.res])
            for t4 in range(4):
                bank = psalloc()
                for tt in range(4):
                    t = t4 * 4 + tt
                    for c in range(NCH):
                        op("pe", lambda: nc.tensor.matmul(PS[:, bank, tt * 128:(tt + 1) * 128], lhsT=hT[:, c, t * 128:(t + 1) * 128],
                                                          rhs=W.ap[:, c, 2, :], start=(c == 0), stop=(c == NCH - 1)),
                           reads=[W.res, hr[c][t // 4]], writes=[psr[bank]])
                op("act", lambda: nc.scalar.copy(out=vp.ap[:, t4 * 4:(t4 + 1) * 4, :],
                                                 in_=PS[:, bank, :].rearrange("p (t n) -> p t n", t=4)),
                   writes=[psr[bank], vp.res])
            pstate["allowed"] = list(range(5))
            op("pe", lambda: nc.tensor.matmul(PS[:, 5, 0:FILLN], lhsT=onesb.ap, rhs=hT[:, 0, 0:FILLN], start=True, stop=True,
                                              skip_group_check=True),
               reads=[onesb.res], writes=[psr[5]])
            for qg in range(4):
                q0 = qg * 512
                for hh in range(2):
                    op("pool", lambda: nc.gpsimd.memset(Rb[hh].ap, 0.0), writes=[Rb[hh].res])
                nsteps = 4 * qg + 4
                zb = {}

                def emit_z(step, hh):
                    kb = 4 * qg + 3 - step
                    c0 = max(0, kb - 4 * qg) * 128
                    bank = psalloc()
                    ps_ = slice(hh * 64, hh * 64 + 64)
                    op("pe", lambda: nc.tensor.matmul(PS[:, bank, c0:512], lhsT=kT.ap[ps_, kb * 128:(kb + 1) * 128],
                                                      rhs=qsT.ap[ps_, q0 + c0:q0 + 512], start=True, stop=True),
                       reads=[kT.res, qsT.res], writes=[psr[bank]])
                    filler(FILL)
                    zb[(step, hh)] = bank

                for hh in range(2):
                    emit_z(0, hh)
                for step in range(nsteps):
                    kb = 4 * qg + 3 - step
                    diag = kb >= 4 * qg
                    c0 = max(0, kb - 4 * qg) * 128
                    N = 512 - c0
                    sps, ats, cbs, ebs = [], [], [], []
                    for hh in range(2):
                        bank = zb.pop((step, hh))
                        eb = ebuf[cnt["e"] % 4]
                        cnt["e"] += 1
                        ebs.append(eb)
                        sp_ = spb[cnt["sp"] % 4]
                        cnt["sp"] += 1
                        op("act", lambda: nc.scalar.activation(out=eb.ap[:, c0:512], in_=PS[:, bank, c0:512], func=AF.Exp),
                           writes=[psr[bank], eb.res])
                        op("act", lambda: nc.scalar.activation(out=sp_.ap[:, c0:512], in_=eb.ap[:, c0:512], func=AF.Ln, bias=1.0),
                           reads=[eb.res], writes=[sp_.res])
                        if diag:
                            op("dve", lambda: nc.vector.tensor_tensor(out=sp_.ap[:, c0:c0 + 128], in0=sp_.ap[:, c0:c0 + 128],
                                                                      in1=maskb.ap, op=ALU.mult),
                               reads=[sp_.res, maskb.res], writes=[sp_.res])
                        sps.append(sp_)
                    for hh in range(2):
                        sp_ = sps[hh]
                        bank = psalloc()
                        cbs.append(bank)
                        ps_ = slice(hh * 64, hh * 64 + 64)
                        op("pe", lambda: nc.tensor.matmul(PS[:, bank, c0:512], lhsT=tri.ap, rhs=sp_.ap[:, c0:512], start=True, stop=(step == 0)),
                           reads=[tri.res, sp_.res], writes=[psr[bank]])
                        filler(FILL)
                        if step > 0:
                            op("pe", lambda: nc.tensor.matmul(PS[:, bank, c0:512], lhsT=onesb.ap, rhs=Rb[hh].ap[:, c0:512],
                                                              start=False, stop=True),
                               reads=[onesb.res, Rb[hh].res], writes=[psr[bank]])
                            filler(FILL)
                    if step + 1 < nsteps:
                        for hh in range(2):
                            emit_z(step + 1, hh)
                    for hh in range(2):
                        sp_ = sps[hh]
                        bank = cbs[hh]
                        at = aTb[cnt["a"] % 4]
                        cnt["a"] += 1
                        ec = ecb[hh]
                        eb = ebs[hh]
                        op("act", lambda: nc.scalar.activation(out=ec.ap[:, c0:512], in_=PS[:, bank, c0:512], func=AF.Exp, scale=-1.0),
                           writes=[psr[bank], ec.res])
                        op("dve", lambda: nc.vector.tensor_tensor(out=at.ap[:, c0:512], in0=eb.ap[:, c0:512], in1=ec.ap[:, c0:512], op=ALU.mult),
                           reads=[eb.res, ec.res], writes=[at.res])
                        if diag:
                            op("dve", lambda: nc.vector.tensor_tensor(out=at.ap[:, c0:c0 + 128], in0=at.ap[:, c0:c0 + 128],
                                                                      in1=maskb.ap, op=ALU.mult),
                               reads=[at.res, maskb.res], writes=[at.res])
                        if step + 1 < nsteps:
                            op("pool", lambda: nc.gpsimd.tensor_tensor(out=Rb[hh].ap[:, c0:512], in0=Rb[hh].ap[:, c0:512],
                                                                       in1=sp_.ap[:, c0:512], op=ALU.add),
                               reads=[Rb[hh].res, sp_.res], writes=[Rb[hh].res])
                        ab = 6 + hh
                        op("pe", lambda: nc.tensor.matmul(PS[0:64, ab, c0:512], lhsT=vp.ap[:, kb, hh * 64:(hh + 1) * 64],
                                                          rhs=at.ap[:, c0:512], start=(step == 0), stop=(step == nsteps - 1),
                                                          skip_group_check=True),
                           reads=[vp.res, at.res], writes=[psr[ab]])
                        filler(FILL)
                for hh in range(2):
                    ab = 6 + hh
                    op("act", lambda: nc.scalar.copy(out=oT.ap[0:64, hh, q0:q0 + 512], in_=PS[0:64, ab, :]),
                       writes=[psr[ab], oT.res])
            pstate["allowed"] = list(range(6))
            for tb in range(NTB):
                for nn in range(NCH):
                    bank = psalloc()
                    for hh in range(2):
                        op("pe", lambda: nc.tensor.matmul(psb(bank), lhsT=wo.ap[0:64, hh, nn * 128:(nn + 1) * 128],
                                                          rhs=oT.ap[0:64, hh, tbs(tb)], start=(hh == 0), stop=(hh == 1)),
                           reads=[wo.res, oT.res], writes=[psr[bank]])
                    resid_add(nn, tb, bank)

    def body(b):
        load_x(b)
        for sl in sublayers:
            if sl[0] == "ret":
                retention(sl[1])
            elif sl[0] == "sb":
                stick(sl[1])
            else:
                ffn(sl[1])
        store_out(b)

    reset_sync()
    if nseq == 1:
        body(0)
        reset_sync()
    else:
        with nc.Fori(0, nseq, hint_back_edge=True) as b:
            body(b)
            reset_sync()
    return nc


def make_consts():
    bf = ml_dtypes.bfloat16
    c = {}
    c["c_identb"] = np.eye(128, dtype=np.float32).astype(bf)
    c["c_identf"] = np.eye(128, dtype=np.float32)
    jj = np.arange(128)
    c["c_tri"] = (jj[:, None] >= jj[None, :]).astype(np.float32).astype(bf)
    c["c_mask"] = (jj[:, None] < jj[None, :]).astype(np.float32).astype(bf)
    dm = np.zeros((128, 4, 128), np.float64)
    kd = np.zeros((128, 4), np.float64)
    ev = np.zeros((128, 4), np.float64)
    for h in range(4):
        g = 1.0 - 2.0 ** (-5.0 - h)
        k = jj.astype(np.float64)
        dm[:, h, :] = np.where(jj[None, :] >= jj[:, None], (g ** (-(k + 1.0)))[:, None] / 16.0, 0.0)
        kd[:, h] = g ** (127.0 - k) / 16.0
        ev[:, h] = EPS * g ** (-2.0 * (k + 1.0))
    c["c_dm"] = dm.reshape(128, 512).astype(np.float32)
    c["c_kdecs"] = kd.astype(np.float32)
    c["c_epsv"] = ev.astype(np.float32)
    pos = np.arange(S, dtype=np.float32)
    inv = (np.float32(10000.0) ** (-np.arange(0, 256, 2, dtype=np.float32) / np.float32(256))).astype(np.float32)
    ang = (pos[:, None] * inv[None, :]).astype(np.float32)
    c["c_cos"] = np.ascontiguousarray(np.cos(ang).T.astype(np.float32))
    c["c_sin"] = np.ascontiguousarray(np.sin(ang).T.astype(np.float32))
    return c


def layout_params(inp):
    f = lambda a: np.asarray(a, dtype=np.float32)
    gl = [f(inp["ret_norm"])[0], f(inp["ret_norm"])[1], f(inp["sb_norm"])[0], f(inp["sb_norm"])[1]]
    gl += [f(inp["ffn_norm"])[l] for l in range(4)] + [f(inp["final_norm"])]
    gains = np.stack([g.reshape(8, 128).T for g in gl], axis=1).reshape(128, 72)
    cwv = f(inp["ffn_conv_w"])
    cw = cwv.reshape(4, 3, 44, 128).transpose(3, 0, 2, 1).reshape(128, 4 * 44 * 3)
    cbv = f(inp["ffn_conv_b"])
    cb = cbv.reshape(4, 44, 128).transpose(2, 0, 1).reshape(128, 4 * 44)
    return {"c_gains": np.ascontiguousarray(gains), "c_cw": np.ascontiguousarray(cw), "c_cb": np.ascontiguousarray(cb)}


FULL = [("ret", 0), ("ffn", 0), ("sb", 0), ("ffn", 1), ("ret", 1), ("ffn", 2), ("sb", 1), ("ffn", 3)]


def run(inputs, nseq, sublayers, xs_per_core, trace=False):
    nc = build_program(nseq, sublayers)
    common = {}
    common.update(make_consts())
    common.update(layout_params(inputs))
    for sl in set(sublayers):
        names = {"ret": ("ret_w_in", "ret_w_out"), "sb": ("sb_w_in", "sb_w_out"), "ffn": ("ffn_w_up", "ffn_w_down")}[sl[0]]
        for k in names:
            common["%s_%d" % (k, sl[1])] = np.ascontiguousarray(np.asarray(inputs[k][sl[1]], dtype=np.float32))
    in_maps = []
    for xs in xs_per_core:
        m = dict(common)
        m["x"] = np.ascontiguousarray(xs)
        in_maps.append(m)
    res = run_bass_kernel_spmd(nc, in_maps, core_ids=list(range(len(xs_per_core))), trace=trace)
    return res


def kernel(**inputs):
    x = np.asarray(inputs["x"], dtype=np.float32)
    B = x.shape[0]
    per = B // NCORES
    xs = [x[i * per:(i + 1) * per] for i in range(NCORES)]
    res = run(inputs, per, FULL, xs)
    out = np.concatenate([np.asarray(r["out"], dtype=np.float32) for r in res.results], axis=0)
    return out
```
